# Optimizing a Trainium2 kernel written in Bass

```python
import jax, jax.numpy as jnp
from jax import lax
import numpy as np

D_MODEL = 1024
BATCH = 4
SEQ = 4096
DEPTH = 4

D_MIX = D_MODEL
D_RG = D_MIX // 2
RG_BLOCKS = 8
RG_BLOCK_W = D_RG // RG_BLOCKS
CONV_W = 4
RG_C = 8.0
GLA_HEADS = 4
GLA_VD = D_MIX - D_RG
GLA_KD = GLA_VD // 2
GLA_DK = GLA_KD // GLA_HEADS
GLA_DV = GLA_VD // GLA_HEADS
GLA_RANK = 16
GLA_TAU = 16.0
GLA_CHUNK = 64
D_IN = 2 * D_RG + 2 * GLA_KD + 2 * GLA_VD + GLA_RANK
D_FF = 2816
N_EXPERTS = 8
TOP_K = 2
N_DENSE = (DEPTH + 1) // 2
N_MOE = DEPTH // 2
EPS = 1e-6

kernel_name = "hybrid_rglru_gla_moe_adaln_trunk"


def rms_norm(x, g):
    x32 = x.astype(jnp.float32)
    y = x32 * lax.rsqrt(jnp.mean(jnp.square(x32), axis=-1, keepdims=True) + EPS)
    return (y * g.astype(jnp.float32)).astype(x.dtype)


def modulate(h, shift, scale):
    return h * (1.0 + scale) + shift


def causal_depthwise_conv(u, w, b):
    s = u.shape[1]
    up = jnp.pad(u, ((0, 0), (CONV_W - 1, 0), (0, 0)))
    out = b
    for tap in range(CONV_W):
        out = out + up[:, tap:tap + s] * w[tap]
    return out


def _linear_combine(c1, c2):
    a1, b1 = c1
    a2, b2 = c2
    return a1 * a2, a2 * b1 + b2


def rg_lru(u, wa, ba, wx, bx, lam):
    bsz, s, d = u.shape
    ub = u.reshape(bsz, s, RG_BLOCKS, RG_BLOCK_W)
    r = jax.nn.sigmoid((jnp.einsum('bsnd,nde->bsne', ub, wa).reshape(bsz, s, d) + ba).astype(jnp.float32))
    i = jax.nn.sigmoid((jnp.einsum('bsnd,nde->bsne', ub, wx).reshape(bsz, s, d) + bx).astype(jnp.float32))
    log_a = -RG_C * r * jax.nn.softplus(-lam.astype(jnp.float32))
    a = jnp.exp(log_a)
    mult = jnp.sqrt(-jnp.expm1(2.0 * log_a))
    first = (jnp.arange(s) == 0)[None, :, None]
    mult = jnp.where(first, 1.0, mult)
    b_term = mult * i * u.astype(jnp.float32)
    _, h = lax.associative_scan(_linear_combine, (a, b_term), axis=1)
    return h.astype(u.dtype)


def gla_chunked(q, k, v, log_f):
    out_dtype = v.dtype
    bsz, s, nh, dk = q.shape
    dv = v.shape[-1]
    n = s // GLA_CHUNK
    q = q.astype(jnp.float32) * (dk ** -0.5)
    k = k.astype(jnp.float32)
    v = v.astype(jnp.float32)
    q = q.reshape(bsz, n, GLA_CHUNK, nh, dk)
    k = k.reshape(bsz, n, GLA_CHUNK, nh, dk)
    v = v.reshape(bsz, n, GLA_CHUNK, nh, dv)
    b = jnp.cumsum(log_f.astype(jnp.float32).reshape(bsz, n, GLA_CHUNK, nh, dk), axis=2)
    b_last = b[:, :, -1]
    b_ref = b[:, :, GLA_CHUNK // 2 - 1:GLA_CHUNK // 2]
    q_loc = q * jnp.exp(b - b_ref)
    k_loc = k * jnp.exp(b_ref - b)
    scores = jnp.einsum('bnihd,bnjhd->bnhij', q_loc, k_loc)
    causal = jnp.tril(jnp.ones((GLA_CHUNK, GLA_CHUNK), dtype=bool))
    scores = jnp.where(causal, scores, 0.0)
    o_intra = jnp.einsum('bnhij,bnjhv->bnihv', scores, v)
    k_end = k * jnp.exp(b_last[:, :, None] - b)
    u_chunk = jnp.einsum('bnjhd,bnjhv->bnhdv', k_end, v)
    decay = jnp.exp(b_last)

    def step(state, inp):
        dec, upd = inp
        return dec[..., None] * state + upd, state

    init = jnp.zeros((bsz, nh, dk, dv), jnp.float32)
    _, states = lax.scan(step, init, (jnp.moveaxis(decay, 1, 0), jnp.moveaxis(u_chunk, 1, 0)))
    states = jnp.moveaxis(states, 0, 1)
    o_inter = jnp.einsum('bnihd,bnhdv->bnihv', q * jnp.exp(b), states)
    o = (o_intra + o_inter).reshape(bsz, s, nh, dv)
    return o.astype(out_dtype)


def gla_branch(q, k, v, g, f_low, wg2, bg, norm_g):
    bsz, s, _ = q.shape
    log_f = jax.nn.log_sigmoid((f_low @ wg2 + bg).astype(jnp.float32)) / GLA_TAU
    o = gla_chunked(q.reshape(bsz, s, GLA_HEADS, GLA_DK),
                    k.reshape(bsz, s, GLA_HEADS, GLA_DK),
                    v.reshape(bsz, s, GLA_HEADS, GLA_DV),
                    log_f.reshape(bsz, s, GLA_HEADS, GLA_DK))
    o = rms_norm(o, norm_g)
    return o.reshape(bsz, s, GLA_VD) * jax.nn.silu(g)


def hybrid_mixer(h, w_in, conv_w, conv_b, wa, ba, wx, bx, lam, wg2, bg, gla_g, w_out):
    z = h @ w_in
    splits = np.cumsum([D_RG, D_RG, GLA_KD, GLA_KD, GLA_VD, GLA_VD]).tolist()
    rg_x, rg_gate, q, k, v, g, f_low = jnp.split(z, splits, axis=-1)
    u = causal_depthwise_conv(rg_x, conv_w, conv_b)
    rg_out = rg_lru(u, wa, ba, wx, bx, lam) * jax.nn.gelu(rg_gate)
    gla_out = gla_branch(q, k, v, g, f_low, wg2, bg, gla_g)
    return jnp.concatenate([rg_out, gla_out], axis=-1) @ w_out


def swiglu(h, w1, w3, w2):
    return (jax.nn.silu(h @ w1) * (h @ w3)) @ w2


def moe_swiglu(h, router_w, w1, w3, w2):
    logits = (h @ router_w).astype(jnp.float32)
    top_v, top_i = lax.top_k(logits, TOP_K)
    probs = jax.nn.softmax(top_v, axis=-1)
    gates = jnp.sum(jax.nn.one_hot(top_i, N_EXPERTS, dtype=jnp.float32) * probs[..., None], axis=-2)
    gates = gates.astype(h.dtype)
    y = jnp.zeros_like(h)
    for e in range(N_EXPERTS):
        y = y + gates[..., e:e + 1] * swiglu(h, w1[e], w3[e], w2[e])
    return y


def setup_inputs(seed: int = 0) -> dict:
    key = jax.random.key(seed)
    ks = jax.random.split(key, 32)

    def nrm(k, shape, scale):
        return scale * jax.random.normal(k, shape, jnp.float32)

    a0 = jax.random.uniform(ks[13], (DEPTH, D_RG), jnp.float32, 0.9, 0.999)
    a_root = a0 ** (1.0 / RG_C)
    rg_lambda = jnp.log(a_root) - jnp.log1p(-a_root)
    return {
        "x": nrm(ks[0], (BATCH, SEQ, D_MODEL), 1.0),
        "c": nrm(ks[1], (BATCH, D_MODEL), 1.0),
        "ada_w": nrm(ks[2], (DEPTH, D_MODEL, 6 * D_MODEL), 0.5 * D_MODEL ** -0.5),
        "ada_b": nrm(ks[3], (DEPTH, 6 * D_MODEL), 0.01),
        "norm_mix_g": 1.0 + nrm(ks[4], (DEPTH, D_MODEL), 0.05),
        "norm_ffn_g": 1.0 + nrm(ks[5], (DEPTH, D_MODEL), 0.05),
        "w_in": nrm(ks[6], (DEPTH, D_MODEL, D_IN), D_MODEL ** -0.5),
        "rg_conv_w": nrm(ks[7], (DEPTH, CONV_W, D_RG), CONV_W ** -0.5),
        "rg_conv_b": nrm(ks[8], (DEPTH, D_RG), 0.01),
        "rg_wa": nrm(ks[9], (DEPTH, RG_BLOCKS, RG_BLOCK_W, RG_BLOCK_W), RG_BLOCK_W ** -0.5),
        "rg_ba": nrm(ks[10], (DEPTH, D_RG), 0.01),
        "rg_wx": nrm(ks[11], (DEPTH, RG_BLOCKS, RG_BLOCK_W, RG_BLOCK_W), RG_BLOCK_W ** -0.5),
        "rg_bx": nrm(ks[12], (DEPTH, D_RG), 0.01),
        "rg_lambda": rg_lambda,
        "gla_wg2": nrm(ks[14], (DEPTH, GLA_RANK, GLA_KD), GLA_RANK ** -0.5),
        "gla_bg": nrm(ks[15], (DEPTH, GLA_KD), 0.1),
        "gla_norm_g": 1.0 + nrm(ks[16], (DEPTH, GLA_DV), 0.05),
        "w_out": nrm(ks[17], (DEPTH, D_MIX, D_MODEL), D_MIX ** -0.5),
        "ffn_w1": nrm(ks[18], (N_DENSE, D_MODEL, D_FF), D_MODEL ** -0.5),
        "ffn_w3": nrm(ks[19], (N_DENSE, D_MODEL, D_FF), D_MODEL ** -0.5),
        "ffn_w2": nrm(ks[20], (N_DENSE, D_FF, D_MODEL), D_FF ** -0.5),
        "router_w": nrm(ks[21], (N_MOE, D_MODEL, N_EXPERTS), D_MODEL ** -0.5),
        "moe_w1": nrm(ks[22], (N_MOE, N_EXPERTS, D_MODEL, D_FF), D_MODEL ** -0.5),
        "moe_w3": nrm(ks[23], (N_MOE, N_EXPERTS, D_MODEL, D_FF), D_MODEL ** -0.5),
        "moe_w2": nrm(ks[24], (N_MOE, N_EXPERTS, D_FF, D_MODEL), D_FF ** -0.5),
        "final_g": 1.0 + nrm(ks[25], (D_MODEL,), 0.05),
    }


def reference(x, c, ada_w, ada_b, norm_mix_g, norm_ffn_g, w_in, rg_conv_w, rg_conv_b,
              rg_wa, rg_ba, rg_wx, rg_bx, rg_lambda, gla_wg2, gla_bg, gla_norm_g, w_out,
              ffn_w1, ffn_w3, ffn_w2, router_w, moe_w1, moe_w3, moe_w2, final_g):
    c_act = jax.nn.silu(c)
    for layer in range(DEPTH):
        mod = c_act @ ada_w[layer] + ada_b[layer]
        sh_m, sc_m, gt_m, sh_f, sc_f, gt_f = [m[:, None, :] for m in jnp.split(mod, 6, axis=-1)]
        h = modulate(rms_norm(x, norm_mix_g[layer]), sh_m, sc_m)
        mix = hybrid_mixer(h, w_in[layer], rg_conv_w[layer], rg_conv_b[layer],
                           rg_wa[layer], rg_ba[layer], rg_wx[layer], rg_bx[layer], rg_lambda[layer],
                           gla_wg2[layer], gla_bg[layer], gla_norm_g[layer], w_out[layer])
        x = x + gt_m * mix
        h = modulate(rms_norm(x, norm_ffn_g[layer]), sh_f, sc_f)
        j = layer // 2
        if layer % 2 == 0:
            f = swiglu(h, ffn_w1[j], ffn_w3[j], ffn_w2[j])
        else:
            f = moe_swiglu(h, router_w[j], moe_w1[j], moe_w3[j], moe_w2[j])
        x = x + gt_f * f
    return rms_norm(x, final_g)
```

```python
import numpy as np
from contextlib import ExitStack
import concourse.bass as bass
import concourse.mybir as mybir
from concourse.bass_utils import run_bass_kernel_spmd

F32 = mybir.dt.float32
BF16 = mybir.dt.bfloat16
AF = mybir.ActivationFunctionType
ALU = mybir.AluOpType
AX = mybir.AxisListType

D = 1024
S = 4096
NT = 512
NB = S // NT
DFF = 2816
NE = 8
DIN = 2576
DEPTH = 4
EPS = 1e-6
LV = 99
NV = 16 + DEPTH * LV
GROUPS = [(0, 4), (4, 4), (8, 4), (12, 4), (16, 4), (20, 2)]


class Res:
    __slots__ = ("name", "w", "r", "dsem", "dcnt", "children", "parent")

    def __init__(self, name):
        self.name = name
        self.w = None
        self.r = {}
        self.dsem = {}
        self.dcnt = {}
        self.children = []
        self.parent = None


class Eng:
    def __init__(self, name, e, sem):
        self.name, self.e, self.sem, self.cnt, self.known = name, e, sem, 0, {}


class _FirstWait:
    def __init__(self, e, wait):
        self.e, self.wait, self.done = e, wait, wait is None

    def _w(self, ins):
        if not self.done:
            ins._wait_ge(self.wait[0], self.wait[1])
            self.done = True
        return ins

    def matmul(self, *a, **k):
        return self._w(self.e.matmul(*a, **k))

    def transpose(self, *a, **k):
        return self._w(self.e.transpose(*a, **k))


class Prog:
    def __init__(self, nc, es):
        self.nc, self.es = nc, es
        self.pe = Eng("pe", nc.tensor, es.enter_context(nc.semaphore("s_pe")))
        self.act = Eng("act", nc.scalar, es.enter_context(nc.semaphore("s_act")))
        self.dve = Eng("dve", nc.vector, es.enter_context(nc.semaphore("s_dve")))
        self.pool = Eng("pool", nc.gpsimd, es.enter_context(nc.semaphore("s_pool")))
        self.sp = Eng("sp", nc.sync, es.enter_context(nc.semaphore("s_sp")))
        self.nres = 0

    def res(self, name):
        return Res(name)

    def _need(self, eng, reads, writes):
        def expand(rs):
            out = []
            for r in rs:
                out.append(r)
                out.extend(r.children)
                if r.parent is not None:
                    out.append(r.parent)
            return out

        rd, wr = {}, {}

        def add(dct, tok):
            if tok is None:
                return
            s, v = tok
            if dct.get(id(s), (None, 0))[1] < v:
                dct[id(s)] = (s, v)

        for r in expand(reads):
            add(rd, r.w)
        for w in expand(writes):
            add(wr, w.w)
            for s, v in w.r.values():
                add(wr, (s, v))
        need_r, need_w = [], []
        for k, (s, v) in rd.items():
            if eng is self.pe and s is self.pe.sem:
                continue
            if eng.known.get(k, 0) < v:
                need_r.append((s, v))
        for k, (s, v) in wr.items():
            if eng is self.pe and s is self.pe.sem:
                continue
            if k in rd and rd[k][1] >= v:
                continue
            if eng.known.get(k, 0) < v:
                need_w.append((s, v))
        return need_r, need_w

    def _deps(self, eng, reads, writes):
        need_r, need_w = self._need(eng, reads, writes)
        for s, v in need_r + need_w:
            if eng.known.get(id(s), 0) < v:
                eng.e.wait_ge(s, v)
                eng.known[id(s)] = v

    def op(self, eng, reads, writes, fn):
        need_r, need_w = self._need(eng, reads, writes)
        embed = None
        if eng is self.pe:
            standalone = need_r + need_w[:-1]
            if need_w:
                embed = need_w[-1]
        else:
            allw = need_r + need_w
            standalone = allw[:-1]
            if allw:
                embed = allw[-1]
        for s, v in standalone:
            if eng.known.get(id(s), 0) < v:
                eng.e.wait_ge(s, v)
                eng.known[id(s)] = v
        if eng is self.pe:
            prox = _FirstWait(eng.e, embed)
            ins = fn(prox)
            if embed is not None and not prox.done:
                raise RuntimeError("embedded wait not consumed")
        else:
            ins = fn(eng.e)
            if embed is not None:
                ins._wait_ge(embed[0], embed[1])
        if embed is not None:
            eng.known[id(embed[0])] = max(eng.known.get(id(embed[0]), 0), embed[1])
        eng.cnt += 1
        ins.then_inc(eng.sem, 1)
        tok = (eng.sem, eng.cnt)
        for r in reads:
            r.r[id(eng.sem)] = tok
        for w in writes:
            w.w = tok
            w.r = {}
        return ins

    def dma(self, q, out_res, in_res, fn):
        self._deps(q, [in_res], [out_res])
        kq = "sw" if q is self.pool else "hw"
        if kq not in out_res.dsem:
            self.nres += 1
            out_res.dsem[kq] = self.es.enter_context(self.nc.semaphore("d%d" % self.nres))
            out_res.dcnt[kq] = 0
        ins = fn(q.e)
        out_res.dcnt[kq] += 16
        dsem = out_res.dsem[kq]
        ins.then_inc(dsem, 16)
        tok = (dsem, out_res.dcnt[kq])
        in_res.r[id(dsem)] = tok
        out_res.w = tok
        out_res.r = {}
        return ins

    def wait_all(self, q, ress):
        self._deps(q, ress, [])


def build(nlayers=DEPTH):
    nc = bass.Bass("TRN2", target_bir_lowering=False)

    def din(name, shape):
        return nc.dram_tensor(name, shape, F32, kind="ExternalInput").ap()

    x_d = din("x", [S, D])
    vec_d = din("vecs", [128, NV])
    ada_w_d = din("ada_w", [DEPTH, D, 6 * D])
    w_in_d = din("w_in", [DEPTH, D, DIN])
    wa_d = din("rg_wa", [DEPTH, 8, 64, 64])
    wx_d = din("rg_wx", [DEPTH, 8, 64, 64])
    wg2_d = din("gla_wg2", [DEPTH, 16, 256])
    w_out_d = din("w_out", [DEPTH, D, D])
    fw1_d = din("ffn_w1", [2, D, DFF])
    fw3_d = din("ffn_w3", [2, D, DFF])
    fw2_d = din("ffn_w2", [2, DFF, D])
    rw_d = din("router_w", [2, D, NE])
    mw1_d = din("moe_w1", [2, NE, D, DFF])
    mw3_d = din("moe_w3", [2, NE, D, DFF])
    mw2_d = din("moe_w2", [2, NE, DFF, D])
    out_d = nc.dram_tensor("out", [S, D], F32, kind="ExternalOutput").ap()
    xs_d = nc.dram_tensor("xs", [NB, 128, 8, NT], F32, kind="Internal").ap()
    wsc_l = [nc.dram_tensor("wsc%d" % l, [7 + (144 if l % 2 == 1 else 18), 128, 4096], BF16, kind="Internal").ap() for l in range(DEPTH)]

    with ExitStack() as es:
        P = Prog(nc, es)
        pe, act, dve, pool, sp = P.pe, P.act, P.dve, P.pool, P.sp

        class T:
            def __init__(self, name, shape, dt, psum=False):
                if psum:
                    self.t = es.enter_context(nc.psum_tensor(name, shape, dt))
                else:
                    self.t = es.enter_context(nc.sbuf_tensor(name, shape, dt))
                self.r = P.res(name)

            def __getitem__(self, k):
                return self.t[k]

        def sb(name, shape, dt=F32):
            return T(name, shape, dt)

        dummy = P.res("dram_in")

        identF = sb("identF", [128, 128])
        identB = sb("identB", [128, 128], BF16)
        onesB = sb("onesB", [128, 128], BF16)
        maskT = sb("maskT", [128, 128])
        rowm = sb("rowm", [128, 2])
        resetm = sb("resetm", [128, NT])
        selE = sb("selE", [8, NE, 128])
        vec = sb("vec", [128, NV])
        cact2 = sb("cact2", [128, 8, 2])
        modall = sb("modall", [128, DEPTH, 48])
        lvec = sb("lvec", [128, 40])
        WaBD = sb("WaBD", [128, 4, 128], BF16)
        WxBD = sb("WxBD", [128, 4, 128], BF16)
        wg2b = sb("wg2b", [16, 256], BF16)
        flw = sb("flw", [128, 8, 16], BF16)
        rwb = sb("rwb", [128, 8, NE], BF16)
        halo = sb("halo", [128, 4, 3])
        hst = sb("hst", [128, 4])
        Sst = [sb("Sst%d" % p, [128, 128]) for p in range(2)]
        xT = sb("xT", [128, 8, NT])
        xTc = [P.res("xTc%d" % c) for c in range(8)]
        for r_ in xTc:
            r_.parent = xT.r
        xT.r.children = list(xTc)
        tmpY = [sb("tmpY%d" % i, [128, NT]) for i in range(2)]
        hT = sb("hT", [128, 8, NT], BF16)
        hgT = sb("hgT", [128, 8, NT], BF16)
        rstd = sb("rstd", [128, NT])
        tmpx = [sb("tmpx%d" % i, [128, NT]) for i in range(1)]
        xin = [sb("xin%d" % i, [128, D]) for i in range(1)]
        KS = [sb("KS%d" % i, [128, 8, 512], BF16) for i in range(5)]
        FS = [sb("FS%d" % i, [128, 4, D], BF16) for i in range(3)]
        gT = [sb("gT%d" % i, [128, 4, NT], BF16) for i in range(2)]
        sl = [sb("sl%d" % i, [128, NT]) for i in range(2)]
        rgxh = sb("rgxh", [128, NT + 3])
        Ub = sb("Ub", [128, NT], BF16)
        mU, mR, mI, mA, mM, mGG, mG2 = [sb("m%s" % n, [128, NT]) for n in ("U", "R", "I", "A", "M", "GG", "G2")]
        mixo = sb("mixo", [128, 8, NT], BF16)
        flowT = sb("flowT", [16, NT], BF16)
        vtok = sb("vtok", [128, 4, 512], BF16)
        gsil = sb("gsil", [128, 4, NT])
        qT, kT, LF, Bc, EQ, EK = [sb("g%s" % n, [128, NT]) for n in ("q", "k", "LF", "Bc", "EQ", "EK")]
        qloc = sb("qloc", [128, NT], BF16)
        kl = [sb("kl%d" % i, [128, NT], BF16) for i in range(2)]
        klf = sb("klf", [128, NT], BF16)
        kltok = sb("kltok", [128, 4, 128], BF16)
        dsm = sb("dsm", [128, 6, 4])
        Sb = [sb("Sb%d" % i, [128, 128], BF16) for i in range(2)]
        scm = [sb("scm%d" % i, [128, 128], BF16) for i in range(2)]
        T1 = [sb("T1_%d" % i, [128, 128]) for i in range(2)]
        oT = sb("oT", [128, NT])
        sqo = sb("sqo", [128, NT], BF16)
        rso = sb("rso", [128, NT])
        LG = sb("LG", [128, 4, NE])
        L2 = sb("L2", [128, 4, NE])
        GT = sb("GT", [128, 4, NE])
        sm4 = sb("sm4", [128, 4, 4])
        GTT = sb("GTT", [8, NT])
        gbc = sb("gbc", [128, NT], BF16)
        PA = [T("psA%d" % i, [128, 512], F32, psum=True) for i in range(2)]
        PB = [T("psB%d" % i, [128, 512], F32, psum=True) for i in range(2)]
        PY = [T("psY%d" % i, [128, 512], F32, psum=True) for i in range(2)]
        PM = T("psM", [128, 512], F32, psum=True)
        PT = T("psT", [128, 1024], BF16, psum=True)
        ctr = {"A": 0, "B": 0, "Y": 0, "KS": 0, "FS": 0, "gT": 0, "sl": 0, "tmpx": 0, "xin": 0, "tmpY": 0, "Y3": 0}

        def nxt(kind, arr):
            i = ctr[kind]
            ctr[kind] = i + 1
            return arr[i % len(arr)]

        def R(*ts):
            return [t.r for t in ts]

        imgs = {}
        wbres = [P.res("wb%d" % i) for i in range(8)]
        wbctr = [0]

        def load_slot(slot, key, blk, src_fn, kmajor):
            pat = "p (k f) -> p k f" if kmajor else "p (j d) -> p j d"
            kw = {"k": 8} if kmajor else {"j": 4}
            wsc_d = wsc_l[key[0]]
            if blk == 0:
                idx = sum(1 for kk in imgs if kk[0] == key[0])
                P.dma(pool, slot.r, dummy, src_fn)
                wr = wbres[wbctr[0] % 8]
                wbctr[0] += 1
                P.dma(sp, wr, slot.r, lambda e: e.dma_start(out=wsc_d[idx].rearrange(pat, **kw), in_=slot[:]))
                ir = P.res("img%d" % idx)
                ir.w = wr.w
                imgs[key] = (idx, ir)
            else:
                idx, ir = imgs[key]
                P.dma(sp, slot.r, ir, lambda e: e.dma_start(out=slot[:], in_=wsc_d[idx].rearrange(pat, **kw)))

        P.op(pool, [], R(identF), lambda e: e.memset(identF[:], 1.0))
        P.op(pool, R(identF), R(identF), lambda e: e.affine_select(out=identF[:], in_=identF[:], pattern=[[1, 128]], compare_op=ALU.is_equal, fill=0.0, base=0, channel_multiplier=-1))
        P.op(pool, [], R(maskT), lambda e: e.memset(maskT[:], 1.0))
        P.op(pool, R(maskT), R(maskT), lambda e: e.affine_select(out=maskT[:], in_=maskT[:], pattern=[[1, 128]], compare_op=ALU.is_ge, fill=0.0, base=0, channel_multiplier=-1))
        P.op(pool, [], R(rowm), lambda e: e.memset(rowm[:], 1.0))
        P.op(pool, R(rowm), R(rowm), lambda e: e.affine_select(out=rowm[:, 0:1], in_=rowm[:, 0:1], pattern=[[0, 1]], compare_op=ALU.is_ge, fill=0.0, base=63, channel_multiplier=-1))
        P.op(pool, R(rowm), R(rowm), lambda e: e.affine_select(out=rowm[:, 1:2], in_=rowm[:, 1:2], pattern=[[0, 1]], compare_op=ALU.is_ge, fill=0.0, base=-64, channel_multiplier=1))
        P.op(pool, [], R(selE), lambda e: e.memset(selE[:], 1.0))
        P.op(pool, R(selE), R(selE), lambda e: e.affine_select(out=selE[:], in_=selE[:], pattern=[[-1, NE], [0, 128]], compare_op=ALU.is_equal, fill=0.0, base=0, channel_multiplier=1))
        P.op(pool, [], R(onesB), lambda e: e.memset(onesB[:], 1.0))
        P.op(pool, [], R(resetm), lambda e: e.memset(resetm[:], 1.0))
        for t in range(NT // 128):
            P.op(pool, R(resetm), R(resetm), lambda e, t=t: e.memset(resetm[:, t * 128:t * 128 + 1], 0.0))
        P.op(pool, R(identF), R(identB), lambda e: e.tensor_copy(out=identB[:], in_=identF[:]))

        P.dma(sp, vec.r, dummy, lambda e: e.dma_start(out=vec[:], in_=vec_d[:, :]))
        P.op(act, R(vec), R(cact2), lambda e: e.activation(out=cact2[:, :, 0], in_=vec[:, 0:8], func=AF.Silu))
        P.op(act, R(vec), R(cact2), lambda e: e.activation(out=cact2[:, :, 1], in_=vec[:, 0:8], func=AF.Silu))

        for l in range(nlayers):
            vb = 16 + l * LV
            for pc in range(12):
                P.dma(sp, xT.r, dummy, lambda e, l=l, pc=pc: e.dma_start(
                    out=xT[:], in_=ada_w_d[l, :, pc * 512:(pc + 1) * 512].rearrange("(k p) f -> p k f", p=128)))

                def mm(e, pc=pc):
                    ins = None
                    for jj in range(4):
                        j = pc * 4 + jj
                        for k in range(8):
                            ins = e.matmul(PM[:, 2 * j:2 * j + 2], lhsT=xT[:, k, jj * 128:(jj + 1) * 128], rhs=cact2[:, k, :], start=(k == 0), stop=(k == 7))
                    return ins
                P.op(pe, R(xT, cact2), R(PM), mm)
            pmv = PM[:, 0:96].rearrange("p (j t) -> p j t", t=2)
            P.op(dve, R(PM, vec), R(modall), lambda e, l=l, vb=vb, pmv=pmv: e.tensor_tensor(
                out=modall[:, l, :], in0=pmv[:, :, 0], in1=vec[:, vb + 51:vb + 99], op=ALU.add))

        def evac_copy(dst_ap, dst_res, src_ap, src_res, eng=None):
            eng = eng or act
            if eng is act:
                P.op(act, [src_res], [dst_res], lambda e: e.copy(out=dst_ap, in_=src_ap))
            else:
                P.op(dve, [src_res], [dst_res], lambda e: e.tensor_copy(out=dst_ap, in_=src_ap))

        def proj(wslot, c0, width, bank, rhs_of_k=None, m_out=128):
            def f(e):
                ins = None
                for k in range(8):
                    ins = e.matmul(bank[0:width, :], lhsT=wslot[:, k, c0:c0 + width], rhs=hT[:, k, :], start=(k == 0), stop=(k == 7))
                return ins
            P.op(pe, R(wslot, hT), R(bank), f)

        def norm_mod(s1_ap, sh_ap):
            for c in range(8):
                P.op(act, R(xT), R(hgT), lambda e, c=c: e.activation(out=hgT[:, c, :], in_=xT[:, c, :], func=AF.Square))

            def f(e):
                ins = None
                for c in range(8):
                    ins = e.matmul(PM[:, :], lhsT=onesB[:], rhs=hgT[:, c, :], start=(c == 0), stop=(c == 7))
                return ins
            P.op(pe, R(onesB, hgT), R(PM), f)
            P.op(act, R(PM), R(rstd), lambda e: e.activation(out=rstd[:], in_=PM[:, :], func=AF.Ln, scale=1.0 / D, bias=EPS))
            P.op(act, R(rstd), R(rstd), lambda e: e.activation(out=rstd[:], in_=rstd[:], func=AF.Exp, scale=-0.5))
            for c in range(8):
                tx = nxt("tmpx", tmpx + tmpY)
                P.op(dve, R(xT, rstd, lvec), R(tx), lambda e, c=c, tx=tx: e.scalar_tensor_tensor(
                    out=tx[:], in0=xT[:, c, :], scalar=s1_ap[:, c:c + 1], in1=rstd[:], op0=ALU.mult, op1=ALU.mult))
                P.op(act, R(tx, modall), R(hT), lambda e, c=c, tx=tx: e.activation(
                    out=hT[:, c, :], in_=tx[:], func=AF.Identity, bias=sh_ap[:, c:c + 1]))

        def resid(bank, dc, gt_ap):
            if dc % 2 == 0:
                P.op(dve, [bank.r, modall.r, xTc[dc]], [xTc[dc]], lambda e: e.scalar_tensor_tensor(
                    out=xT[:, dc, :], in0=bank[:, :], scalar=gt_ap[:, dc:dc + 1], in1=xT[:, dc, :], op0=ALU.mult, op1=ALU.add))
            else:
                ty = nxt("tmpY", tmpY)
                P.op(act, [bank.r, modall.r], [ty.r], lambda e: e.activation(out=ty[:], in_=bank[:, :], func=AF.Identity, scale=gt_ap[:, dc:dc + 1]))
                P.op(pool, [ty.r, xTc[dc]], [xTc[dc]], lambda e: e.tensor_tensor(out=xT[:, dc, :], in0=xT[:, dc, :], in1=ty[:], op=ALU.add))

        steps = []

        def add_step(loads, compute):
            steps.append((loads, compute))

        for l in range(nlayers):
            vb = 16 + l * LV
            moe = (l % 2 == 1)
            jl = l // 2
            mod = lambda j0, l=l: modall[:, l, j0:j0 + 8]
            shm, gtm, shf, gtf = mod(0), mod(16), mod(24), mod(40)
            s1m, s1f, spv, sp2v = lvec[:, 0:8], lvec[:, 8:16], lvec[:, 16:20], lvec[:, 20:24]
            cw = lambda k, c, vb=vb: vec[:, vb + 16 + k * 4 + c:vb + 16 + k * 4 + c + 1]
            cb = lambda c, vb=vb: vec[:, vb + 32 + c:vb + 33 + c]
            ba = lambda c, vb=vb: vec[:, vb + 36 + c:vb + 37 + c]
            bx = lambda c, vb=vb: vec[:, vb + 40 + c:vb + 41 + c]
            bg = lambda p, vb=vb: vec[:, vb + 48 + p:vb + 49 + p]
            glag = vec[:, vb + 50:vb + 51]

            def layer_setup(l=l, vb=vb, moe=moe, jl=jl):
                P.op(dve, R(modall, vec), R(lvec), lambda e: e.scalar_tensor_tensor(
                    out=lvec[:, 0:8], in0=modall[:, l, 8:16], scalar=1.0, in1=vec[:, vb:vb + 8], op0=ALU.add, op1=ALU.mult))
                P.op(dve, R(modall, vec), R(lvec), lambda e: e.scalar_tensor_tensor(
                    out=lvec[:, 8:16], in0=modall[:, l, 32:40], scalar=1.0, in1=vec[:, vb + 8:vb + 16], op0=ALU.add, op1=ALU.mult))
                P.op(act, R(vec), R(lvec), lambda e: e.activation(out=lvec[:, 24:28], in_=vec[:, vb + 44:vb + 48], func=AF.Exp, scale=-1.0))
                P.op(act, R(lvec), R(lvec), lambda e: e.activation(out=lvec[:, 24:28], in_=lvec[:, 24:28], func=AF.Ln, bias=1.0))
                P.op(dve, R(lvec), R(lvec), lambda e: e.tensor_scalar(out=lvec[:, 16:20], in0=lvec[:, 24:28], scalar1=-8.0, scalar2=None, op0=ALU.mult))
                P.op(dve, R(lvec), R(lvec), lambda e: e.tensor_scalar(out=lvec[:, 20:24], in0=lvec[:, 24:28], scalar1=-16.0, scalar2=None, op0=ALU.mult))
                P.op(dve, [], R(WaBD), lambda e: e.memset(WaBD[:], 0.0))
                P.op(dve, [], R(WxBD), lambda e: e.memset(WxBD[:], 0.0))
                for n in range(8):
                    pb = (n % 2) * 64
                    P.dma(pool, WaBD.r, dummy, lambda e, n=n, pb=pb: e.dma_start(out=WaBD[pb:pb + 64, n // 2, pb:pb + 64], in_=wa_d[l, n, :, :]))
                    P.dma(pool, WxBD.r, dummy, lambda e, n=n, pb=pb: e.dma_start(out=WxBD[pb:pb + 64, n // 2, pb:pb + 64], in_=wx_d[l, n, :, :]))
                P.dma(pool, wg2b.r, dummy, lambda e: e.dma_start(out=wg2b[:], in_=wg2_d[l, :, :]))
                P.dma(pool, flw.r, dummy, lambda e: e.dma_start(out=flw[:], in_=w_in_d[l, :, 2560:2576].rearrange("(k p) f -> p k f", p=128)))
                if moe:
                    P.dma(pool, rwb.r, dummy, lambda e: e.dma_start(out=rwb[:], in_=rw_d[jl, :, :].rearrange("(k p) f -> p k f", p=128)))
                P.op(dve, [], R(halo), lambda e: e.memset(halo[:], 0.0))
                P.op(dve, [], R(hst), lambda e: e.memset(hst[:], 0.0))
                for p in range(2):
                    P.op(dve, [], R(Sst[p]), lambda e, p=p: e.memset(Sst[p][:], 0.0))

            for blk in range(NB):
                first_l, last_l = (l == 0), (l == nlayers - 1)
                slots = {}

                def ld_in(names, l=l, slots=slots, blk=blk):
                    def f():
                        for nm, c0 in names:
                            s = nxt("KS", KS)
                            slots[nm] = s
                            load_slot(s, (l, "in", nm), blk, lambda e, s=s, c0=c0: e.dma_start(
                                out=s[:], in_=w_in_d[l, :, c0:c0 + 512].rearrange("(k p) f -> p k f", p=128)), True)
                    return f

                def compA(l=l, blk=blk, slots=slots, first_l=first_l, shm=shm, s1m=s1m, cw=cw, cb=cb, ba=ba, bx=bx, spv=spv, sp2v=sp2v, layer_setup=layer_setup):
                    if blk == 0:
                        layer_setup()
                    if first_l:
                        for t in range(4):
                            xi = nxt("xin", xin)
                            r0 = blk * NT + t * 128
                            P.dma(sp, xi.r, dummy, lambda e, xi=xi, r0=r0: e.dma_start(out=xi[:], in_=x_d[r0:r0 + 128, :]))
                            for half in range(2):
                                def tr(e, xi=xi, half=half):
                                    ins = None
                                    for cc in range(4):
                                        c = half * 4 + cc
                                        ins = e.transpose(PM[:, cc * 128:(cc + 1) * 128], xi[:, c * 128:(c + 1) * 128], identF[:])
                                    return ins
                                P.op(pe, R(xi, identF), R(PM), tr)
                                P.op(dve, R(PM), R(xT), lambda e, t=t, half=half: e.tensor_copy(
                                    out=xT[:, half * 4:half * 4 + 4, t * 128:(t + 1) * 128], in_=PM[:, :].rearrange("p (c t) -> p c t", t=128)))
                    else:
                        P.dma(sp, xT.r, xsr[blk], lambda e: e.dma_start(out=xT[:], in_=xs_d[blk]))
                    norm_mod(s1m, shm)
                    P0, P1 = slots["rgx"], slots["rgg"]
                    for c in range(4):
                        bA = PA[0]
                        proj(P0, c * 128, 128, bA)
                        yield
                        P.op(dve, R(halo), R(rgxh), lambda e, c=c: e.tensor_copy(out=rgxh[:, 0:3], in_=halo[:, c, :]))
                        yield
                        evac_copy(rgxh[:, 3:NT + 3], rgxh.r, bA[:, :], bA.r)
                        yield
                        bB = PB[0]
                        proj(P1, c * 128, 128, bB)
                        yield
                        evac_copy(mGG[:], mGG.r, bB[:, :], bB.r)
                        yield
                        P.op(dve, R(rgxh, vec), R(mU), lambda e, c=c: e.tensor_scalar(
                            out=mU[:], in0=rgxh[:, 3:NT + 3], scalar1=cw(3, c), scalar2=cb(c), op0=ALU.mult, op1=ALU.add))
                        yield
                        for k in range(3):
                            P.op(dve, R(rgxh, vec, mU), R(mU), lambda e, c=c, k=k: e.scalar_tensor_tensor(
                                out=mU[:], in0=rgxh[:, k:k + NT], scalar=cw(k, c), in1=mU[:], op0=ALU.mult, op1=ALU.add))
                            yield
                        P.op(dve, R(rgxh), R(halo), lambda e, c=c: e.tensor_copy(out=halo[:, c, :], in_=rgxh[:, NT:NT + 3]))
                        yield
                        P.op(act, R(mU), R(Ub), lambda e: e.copy(out=Ub[:], in_=mU[:]))
                        yield
                        bA = PA[0]
                        P.op(pe, R(WaBD, Ub), R(bA), lambda e, c=c, bA=bA: e.matmul(bA[:, :], lhsT=WaBD[:, c, :], rhs=Ub[:], start=True, stop=True))
                        yield
                        bB = PB[0]
                        P.op(pe, R(WxBD, Ub), R(bB), lambda e, c=c, bB=bB: e.matmul(bB[:, :], lhsT=WxBD[:, c, :], rhs=Ub[:], start=True, stop=True))
                        yield
                        P.op(act, R(bA, vec), R(mR), lambda e, c=c, bA=bA: e.activation(out=mR[:], in_=bA[:, :], func=AF.Sigmoid, bias=ba(c)))
                        yield
                        P.op(act, R(bB, vec), R(mI), lambda e, c=c, bB=bB: e.activation(out=mI[:], in_=bB[:, :], func=AF.Sigmoid, bias=bx(c)))
                        yield
                        P.op(act, R(mR, lvec), R(mA), lambda e, c=c: e.activation(out=mA[:], in_=mR[:], func=AF.Exp, scale=spv[:, c:c + 1]))
                        yield
                        P.op(act, R(mR, lvec), R(mM), lambda e, c=c: e.activation(out=mM[:], in_=mR[:], func=AF.Exp, scale=sp2v[:, c:c + 1]))
                        yield
                        P.op(act, R(mM), R(mM), lambda e: e.activation(out=mM[:], in_=mM[:], func=AF.Sqrt, scale=-1.0, bias=1.0))
                        yield
                        if blk == 0:
                            P.op(dve, [], R(mM), lambda e: e.memset(mM[:, 0:1], 1.0))
                            yield
                        P.op(dve, R(mM, mI), R(mM), lambda e: e.tensor_tensor(out=mM[:], in0=mM[:], in1=mI[:], op=ALU.mult))
                        yield
                        P.op(dve, R(mM, mU), R(mM), lambda e: e.tensor_tensor(out=mM[:], in0=mM[:], in1=mU[:], op=ALU.mult))
                        yield
                        P.op(dve, R(mA, mM, hst), R(mR), lambda e, c=c: e.tensor_tensor_scan(
                            out=mR[:], data0=mA[:], data1=mM[:], initial=hst[:, c:c + 1], op0=ALU.mult, op1=ALU.add))
                        yield
                        P.op(dve, R(mR), R(hst), lambda e, c=c: e.tensor_copy(out=hst[:, c:c + 1], in_=mR[:, NT - 1:NT]))
                        yield
                        P.op(act, R(mGG), R(mG2), lambda e: e.activation(out=mG2[:], in_=mGG[:], func=AF.Square))
                        yield
                        P.op(dve, R(mG2), R(mG2), lambda e: e.tensor_scalar(out=mG2[:], in0=mG2[:], scalar1=0.044715, scalar2=1.0, op0=ALU.mult, op1=ALU.add))
                        yield
                        P.op(dve, R(mG2, mGG), R(mG2), lambda e: e.tensor_tensor(out=mG2[:], in0=mG2[:], in1=mGG[:], op=ALU.mult))
                        yield
                        P.op(act, R(mG2), R(mG2), lambda e: e.activation(out=mG2[:], in_=mG2[:], func=AF.Sigmoid, scale=1.5957691216057308))
                        yield
                        P.op(dve, R(mG2, mGG), R(mG2), lambda e: e.tensor_tensor(out=mG2[:], in0=mG2[:], in1=mGG[:], op=ALU.mult))
                        yield
                        P.op(dve, R(mR, mG2), R(mixo), lambda e, c=c: e.tensor_tensor(out=mixo[:, c, :], in0=mR[:], in1=mG2[:], op=ALU.mult))
                        yield

                def compB(l=l, blk=blk, slots=slots, bg=bg, glag=glag):
                    P2, P3, P4 = slots["qk"], slots["v"], slots["g"]
                    bA = PA[1]

                    def ff(e, bA=bA):
                        ins = None
                        for k in range(8):
                            ins = e.matmul(bA[0:16, :], lhsT=flw[:, k, :], rhs=hT[:, k, :], start=(k == 0), stop=(k == 7))
                        return ins
                    P.op(pe, R(flw, hT), R(bA), ff)
                    yield
                    evac_copy(flowT[:], flowT.r, bA[0:16, :], bA.r)
                    yield
                    for t in range(4):
                        bB = PB[1]

                        def fv(e, t=t, bB=bB):
                            ins = None
                            for k in range(8):
                                ins = e.matmul(bB[:, :], lhsT=hT[:, k, t * 128:(t + 1) * 128], rhs=P3[:, k, :], start=(k == 0), stop=(k == 7))
                            return ins
                        P.op(pe, R(P3, hT), R(bB), fv)
                        yield
                        evac_copy(vtok[:, t, :], vtok.r, bB[:, :], bB.r, eng=(act if t % 2 == 0 else dve))
                        yield
                    for hd in range(4):
                        bA = PA[1]
                        proj(P4, hd * 128, 128, bA)
                        yield
                        P.op(act, R(bA), R(gsil), lambda e, hd=hd, bA=bA: e.activation(out=gsil[:, hd, :], in_=bA[:, :], func=AF.Silu))
                        yield
                    for p in range(2):
                        bA = PA[1]
                        proj(P2, p * 128, 128, bA)
                        yield
                        evac_copy(qT[:], qT.r, bA[:, :], bA.r)
                        yield
                        bB = PB[1]
                        proj(P2, 256 + p * 128, 128, bB)
                        yield
                        evac_copy(kT[:], kT.r, bB[:, :], bB.r, eng=dve)
                        yield
                        bA = PA[1]
                        P.op(pe, R(wg2b, flowT), R(bA), lambda e, p=p, bA=bA: e.matmul(
                            bA[:, :], lhsT=wg2b[0:16, p * 128:(p + 1) * 128], rhs=flowT[0:16, :], start=True, stop=True))
                        yield
                        P.op(act, R(bA, vec), R(LF), lambda e, p=p, bA=bA: e.activation(out=LF[:], in_=bA[:, :], func=AF.Sigmoid, bias=bg(p)))
                        yield
                        P.op(act, R(LF), R(LF), lambda e: e.activation(out=LF[:], in_=LF[:], func=AF.Ln))
                        yield
                        P.op(dve, R(resetm, LF), R(Bc), lambda e: e.tensor_tensor_scan(
                            out=Bc[:], data0=resetm[:], data1=LF[:], initial=0.0, op0=ALU.mult, op1=ALU.add))
                        yield
                        Bv = Bc[:].rearrange("p (c t) -> p c t", t=128)
                        P.op(dve, R(Bc), R(EQ), lambda e, Bv=Bv: e.tensor_tensor(
                            out=EQ[:].rearrange("p (c t) -> p c t", t=128), in0=Bv, in1=Bv[:, :, 63:64].to_broadcast([128, 4, 128]), op=ALU.subtract))
                        yield
                        P.op(act, R(EQ), R(EK), lambda e: e.activation(out=EK[:], in_=EQ[:], func=AF.Exp, scale=-1.0 / 16))
                        yield
                        P.op(act, R(EQ), R(EQ), lambda e: e.activation(out=EQ[:], in_=EQ[:], func=AF.Exp, scale=1.0 / 16))
                        yield
                        P.op(dve, R(qT, EQ), R(qloc), lambda e: e.scalar_tensor_tensor(
                            out=qloc[:], in0=qT[:], scalar=0.125, in1=EQ[:], op0=ALU.mult, op1=ALU.mult))
                        yield
                        for hp in range(2):
                            P.op(dve, R(kT, EK, rowm), R(kl[hp]), lambda e, hp=hp: e.scalar_tensor_tensor(
                                out=kl[hp][:], in0=kT[:], scalar=rowm[:, hp:hp + 1], in1=EK[:], op0=ALU.mult, op1=ALU.mult))
                            yield
                        P.op(dve, R(kT, EK), R(klf), lambda e: e.tensor_tensor(out=klf[:], in0=kT[:], in1=EK[:], op=ALU.mult))
                        yield
                        P.op(act, R(Bc), R(dsm), lambda e, Bv=Bv: e.activation(out=dsm[:, 0, :], in_=Bv[:, :, 127], func=AF.Exp, scale=1.0 / 16))
                        yield
                        P.op(act, R(Bc), R(dsm), lambda e, Bv=Bv: e.activation(out=dsm[:, 1, :], in_=Bv[:, :, 63], func=AF.Exp, scale=1.0 / 16))
                        yield
                        P.op(dve, R(Bc), R(dsm), lambda e, Bv=Bv: e.tensor_tensor(out=dsm[:, 5, :], in0=Bv[:, :, 127], in1=Bv[:, :, 63], op=ALU.subtract))
                        yield
                        P.op(act, R(dsm), R(dsm), lambda e: e.activation(out=dsm[:, 2, :], in_=dsm[:, 5, :], func=AF.Exp, scale=1.0 / 16))
                        yield
                        for hp in range(2):
                            P.op(dve, R(dsm, rowm), R(dsm), lambda e, hp=hp: e.tensor_scalar(
                                out=dsm[:, 3 + hp, :], in0=dsm[:, 2, :], scalar1=rowm[:, hp:hp + 1], scalar2=None, op0=ALU.mult))
                            yield
                        def ftr(e):
                            ins = None
                            for t in range(4):
                                ins = e.transpose(PT[:, t * 128:(t + 1) * 128], klf[:, t * 128:(t + 1) * 128], identB[:])
                            return ins
                        P.op(pe, R(klf, identB), R(PT), ftr)
                        yield
                        P.op(act, R(PT), R(kltok), lambda e: e.copy(out=kltok[:].rearrange("p c t -> p (c t)"), in_=PT[:, 0:512]))
                        yield
                        bO = [nxt("Y", PY), nxt("Y", PY)]
                        S_ = Sst[p]
                        for t in range(4):
                            ts = slice(t * 128, (t + 1) * 128)
                            bU = PA[1]

                            def fu(e, t=t, bU=bU, p=p):
                                e.matmul(bU[:, 0:128], lhsT=kltok[:, t, :], rhs=vtok[:, t, (2 * p) * 128:(2 * p + 1) * 128], start=True, stop=True)
                                return e.matmul(bU[:, 128:256], lhsT=kltok[:, t, :], rhs=vtok[:, t, (2 * p + 1) * 128:(2 * p + 2) * 128], start=True, stop=True)
                            P.op(pe, R(kltok, vtok), R(bU), fu)
                            yield
                            bS = PB[1]

                            def fs(e, ts=ts, bS=bS):
                                e.matmul(bS[:, 0:128], lhsT=kl[0][:, ts], rhs=qloc[:, ts], start=True, stop=True)
                                return e.matmul(bS[:, 128:256], lhsT=kl[1][:, ts], rhs=qloc[:, ts], start=True, stop=True)
                            P.op(pe, R(kl[0], kl[1], qloc), R(bS), fs)
                            yield
                            for hp in range(2):
                                P.op(dve, R(bS, maskT), R(scm[hp]), lambda e, hp=hp, bS=bS: e.tensor_tensor(
                                    out=scm[hp][:], in0=bS[:, hp * 128:(hp + 1) * 128], in1=maskT[:], op=ALU.mult))
                                yield
                                P.op(dve, R(S_, dsm, rowm), R(Sb[hp]), lambda e, hp=hp, t=t, S_=S_: e.tensor_scalar(
                                    out=Sb[hp][:], in0=S_[:], scalar1=dsm[:, 1, t:t + 1], scalar2=rowm[:, hp:hp + 1], op0=ALU.mult, op1=ALU.mult))
                                yield
                            for hp in range(2):
                                h = 2 * p + hp

                                def fo(e, hp=hp, h=h, t=t, ts=ts):
                                    e.matmul(bO[hp][:, ts], lhsT=vtok[:, t, h * 128:(h + 1) * 128], rhs=scm[hp][:], start=True, stop=False)
                                    return e.matmul(bO[hp][:, ts], lhsT=Sb[hp][:], rhs=qloc[:, ts], start=False, stop=True)
                                P.op(pe, R(vtok, scm[hp], Sb[hp], qloc), R(bO[hp]), fo)
                                yield
                            P.op(dve, R(bU, dsm), R(T1[0]), lambda e, t=t, bU=bU: e.tensor_scalar(
                                out=T1[0][:], in0=bU[:, 0:128], scalar1=dsm[:, 3, t:t + 1], scalar2=None, op0=ALU.mult))
                            yield
                            P.op(dve, R(bU, dsm), R(T1[1]), lambda e, t=t, bU=bU: e.tensor_scalar(
                                out=T1[1][:], in0=bU[:, 128:256], scalar1=dsm[:, 4, t:t + 1], scalar2=None, op0=ALU.mult))
                            yield
                            P.op(dve, R(S_, dsm, T1[0]), R(S_), lambda e, t=t, S_=S_: e.scalar_tensor_tensor(
                                out=S_[:], in0=S_[:], scalar=dsm[:, 0, t:t + 1], in1=T1[0][:], op0=ALU.mult, op1=ALU.add))
                            yield
                            P.op(dve, R(S_, T1[1]), R(S_), lambda e, S_=S_: e.tensor_tensor(out=S_[:], in0=S_[:], in1=T1[1][:], op=ALU.add))
                            yield
                        for hp in range(2):
                            h = 2 * p + hp
                            b_ = bO[hp]
                            evac_copy(oT[:], oT.r, b_[:, :], b_.r)
                            yield
                            P.op(act, R(b_), R(sqo), lambda e, b_=b_: e.activation(out=sqo[:], in_=b_[:, :], func=AF.Square))
                            yield
                            P.op(pe, R(onesB, sqo), R(PM), lambda e: e.matmul(PM[:, :], lhsT=onesB[:], rhs=sqo[:], start=True, stop=True))
                            yield
                            P.op(act, R(PM), R(rso), lambda e: e.activation(out=rso[:], in_=PM[:, :], func=AF.Ln, scale=1.0 / 128, bias=EPS))
                            yield
                            P.op(act, R(rso), R(rso), lambda e: e.activation(out=rso[:], in_=rso[:], func=AF.Exp, scale=-0.5))
                            yield
                            P.op(dve, R(oT, rso), R(oT), lambda e: e.tensor_tensor(out=oT[:], in0=oT[:], in1=rso[:], op=ALU.mult))
                            yield
                            P.op(dve, R(oT, vec, gsil), R(mixo), lambda e, h=h: e.scalar_tensor_tensor(
                                out=mixo[:, 4 + h, :], in0=oT[:], scalar=glag, in1=gsil[:, h, :], op0=ALU.mult, op1=ALU.mult))
                            yield
                def compAB(compA=compA, compB=compB):
                    gens = [compA(), compB()]
                    while gens:
                        for g_ in list(gens):
                            try:
                                next(g_)
                            except StopIteration:
                                gens.remove(g_)
                add_step(ld_in([("rgx", 0), ("rgg", 512), ("qk", 1024), ("v", 1536), ("g", 2048)]), compAB)

                wo = {}

                def ldC(l=l, wo=wo, blk=blk):
                    for i in range(2):
                        s = nxt("FS", FS)
                        wo[i] = s
                        load_slot(s, (l, "out", i), blk, lambda e, s=s, i=i: e.dma_start(
                            out=s[:], in_=w_out_d[l, i * 512:(i + 1) * 512, :].rearrange("(j p) d -> p j d", p=128)), False)

                def compC(l=l, wo=wo, gtm=gtm, s1f=s1f, shf=shf, moe=moe):
                    for dc in range(8):
                        bY = nxt("Y", PY)

                        def f(e, dc=dc, bY=bY):
                            ins = None
                            for m in range(8):
                                ins = e.matmul(bY[:, :], lhsT=wo[m // 4][:, m % 4, dc * 128:(dc + 1) * 128], rhs=mixo[:, m, :], start=(m == 0), stop=(m == 7))
                            return ins
                        P.op(pe, R(wo[0], wo[1], mixo), R(bY), f)
                        resid(bY, dc, gtm)
                    norm_mod(s1f, shf)
                    if moe:
                        def fr(e):
                            ins = None
                            for t in range(4):
                                for k in range(8):
                                    ins = e.matmul(PM[:, t * NE:(t + 1) * NE], lhsT=hT[:, k, t * 128:(t + 1) * 128], rhs=rwb[:, k, :], start=(k == 0), stop=(k == 7))
                            return ins
                        P.op(pe, R(hT, rwb), R(PM), fr)
                        P.op(dve, R(PM), R(LG), lambda e: e.tensor_copy(out=LG[:].rearrange("p c t -> p (c t)"), in_=PM[:, 0:4 * NE]))
                        bc = lambda ap: ap.to_broadcast([128, 4, NE])
                        P.op(dve, R(LG), R(sm4), lambda e: e.tensor_reduce(out=sm4[:, :, 0:1], in_=LG[:], axis=AX.X, op=ALU.max))
                        P.op(dve, R(LG, sm4), R(L2), lambda e: e.tensor_tensor(out=L2[:], in0=LG[:], in1=bc(sm4[:, :, 0:1]), op=ALU.is_equal))
                        P.op(dve, R(L2, LG), R(L2), lambda e: e.scalar_tensor_tensor(out=L2[:], in0=L2[:], scalar=-1e30, in1=LG[:], op0=ALU.mult, op1=ALU.add))
                        P.op(dve, R(L2), R(sm4), lambda e: e.tensor_reduce(out=sm4[:, :, 1:2], in_=L2[:], axis=AX.X, op=ALU.max))
                        P.op(dve, R(LG, sm4), R(L2), lambda e: e.tensor_tensor(out=L2[:], in0=LG[:], in1=bc(sm4[:, :, 1:2]), op=ALU.is_ge))
                        P.op(dve, R(LG, sm4), R(GT), lambda e: e.tensor_tensor(out=GT[:], in0=LG[:], in1=bc(sm4[:, :, 0:1]), op=ALU.subtract))
                        P.op(act, R(GT), R(GT), lambda e: e.activation(out=GT[:], in_=GT[:], func=AF.Exp))
                        P.op(dve, R(GT, L2), R(GT), lambda e: e.tensor_tensor(out=GT[:], in0=GT[:], in1=L2[:], op=ALU.mult))
                        P.op(dve, R(GT), R(sm4), lambda e: e.tensor_reduce(out=sm4[:, :, 2:3], in_=GT[:], axis=AX.X, op=ALU.add))
                        P.op(dve, R(sm4), R(sm4), lambda e: e.reciprocal(out=sm4[:, :, 3:4], in_=sm4[:, :, 2:3]))
                        P.op(dve, R(GT, sm4), R(GT), lambda e: e.tensor_tensor(out=GT[:], in0=GT[:], in1=bc(sm4[:, :, 3:4]), op=ALU.mult))

                        def ft(e):
                            ins = None
                            for t in range(4):
                                ins = e.transpose(PM[0:NE, t * 128:(t + 1) * 128], GT[:, t, :], identF[:])
                            return ins
                        P.op(pe, R(GT, identF), R(PM), ft)
                        P.op(dve, R(PM), R(GTT), lambda e: e.tensor_copy(out=GTT[:], in_=PM[0:NE, :]))
                add_step(ldC, compC)

                if moe:
                    experts = [(mw1_d[jl, ex], mw3_d[jl, ex], mw2_d[jl, ex], ex) for ex in range(NE)]
                else:
                    experts = [(fw1_d[jl], fw3_d[jl], fw2_d[jl], None)]
                for (w1a, w3a, w2a, ex) in experts:
                    for gi, (f0, g) in enumerate(GROUPS):
                        ws = {}

                        def ldF(w1a=w1a, w3a=w3a, w2a=w2a, f0=f0, g=g, ws=ws, l=l, ex=ex, gi=gi, blk=blk):
                            c0, c1 = f0 * 128, (f0 + g) * 128
                            for nm, src in (("w1", w1a), ("w3", w3a)):
                                s = nxt("KS", KS)
                                ws[nm] = s
                                load_slot(s, (l, ex, gi, nm), blk, lambda e, s=s, src=src: e.dma_start(
                                    out=s[:, :, 0:g * 128], in_=src[:, c0:c1].rearrange("(k p) f -> p k f", p=128)), True)
                            s = nxt("FS", FS)
                            ws["w2"] = s
                            load_slot(s, (l, ex, gi, "w2"), blk, lambda e, s=s: e.dma_start(
                                out=s[:, 0:g, :], in_=w2a[c0:c1, :].rearrange("(j p) d -> p j d", p=128)), False)

                        def compF(ex=ex, gi=gi, g=g, ws=ws, gtf=gtf):
                            if ex is not None and gi == 0:
                                bA = nxt("A", PA)
                                P.op(pe, R(selE, GTT), R(bA), lambda e, bA=bA: e.matmul(bA[:, :], lhsT=selE[0:NE, ex, :], rhs=GTT[0:NE, :], start=True, stop=True))
                                evac_copy(gbc[:], gbc.r, bA[:, :], bA.r)
                                for k in range(8):
                                    P.op(dve, R(hT, gbc), R(hgT), lambda e, k=k: e.tensor_tensor(
                                        out=hgT[:, k, :], in0=hT[:, k, :], in1=gbc[:], op=ALU.mult))
                            h3 = hT if ex is None else hgT
                            gt_ = nxt("gT", gT)
                            for fi in range(g):
                                bA = nxt("A", PA)
                                proj(ws["w1"], fi * 128, 128, bA)
                                bB = nxt("B", PB)

                                def f3(e, fi=fi, bB=bB):
                                    ins = None
                                    for k in range(8):
                                        ins = e.matmul(bB[:, :], lhsT=ws["w3"][:, k, fi * 128:(fi + 1) * 128], rhs=h3[:, k, :], start=(k == 0), stop=(k == 7))
                                    return ins
                                P.op(pe, R(ws["w3"], h3), R(bB), f3)
                                s_ = nxt("sl", sl)
                                P.op(act, R(bA), R(s_), lambda e, bA=bA, s_=s_: e.activation(out=s_[:], in_=bA[:, :], func=AF.Silu))
                                P.op(dve, R(s_, bB), R(gt_), lambda e, fi=fi, bB=bB, s_=s_, gt_=gt_: e.tensor_tensor(
                                    out=gt_[:, fi, :], in0=s_[:], in1=bB[:, :], op=ALU.mult))
                            for dc in range(8):
                                bY = nxt("Y3", PY + [PM])

                                def f2(e, dc=dc, bY=bY, gt_=gt_):
                                    ins = None
                                    for fi in range(g):
                                        ins = e.matmul(bY[:, :], lhsT=ws["w2"][:, fi, dc * 128:(dc + 1) * 128], rhs=gt_[:, fi, :], start=(fi == 0), stop=(fi == g - 1))
                                    return ins
                                P.op(pe, R(ws["w2"], gt_), R(bY), f2)
                                resid(bY, dc, gtf)
                        add_step(ldF, compF)

                def compZ(l=l, blk=blk, last_l=last_l):
                    if not last_l:
                        P.dma(sp, xsr[blk], xT.r, lambda e: e.dma_start(out=xs_d[blk], in_=xT[:]))
                    else:
                        for c in range(8):
                            P.op(act, R(xT), R(hgT), lambda e, c=c: e.activation(out=hgT[:, c, :], in_=xT[:, c, :], func=AF.Square))

                        def f(e):
                            ins = None
                            for c in range(8):
                                ins = e.matmul(PM[:, :], lhsT=onesB[:], rhs=hgT[:, c, :], start=(c == 0), stop=(c == 7))
                            return ins
                        P.op(pe, R(onesB, hgT), R(PM), f)
                        P.op(act, R(PM), R(rstd), lambda e: e.activation(out=rstd[:], in_=PM[:, :], func=AF.Ln, scale=1.0 / D, bias=EPS))
                        P.op(act, R(rstd), R(rstd), lambda e: e.activation(out=rstd[:], in_=rstd[:], func=AF.Exp, scale=-0.5))
                        for c in range(8):
                            P.op(dve, R(xT, rstd, vec), R(xT), lambda e, c=c: e.scalar_tensor_tensor(
                                out=xT[:, c, :], in0=xT[:, c, :], scalar=vec[:, 8 + c:9 + c], in1=rstd[:], op0=ALU.mult, op1=ALU.mult))
                        for t in range(4):
                            xo = nxt("xin", xin)
                            for half in range(2):
                                bY = nxt("Y", PY)

                                def tr(e, t=t, half=half, bY=bY):
                                    ins = None
                                    for cc in range(4):
                                        c = half * 4 + cc
                                        ins = e.transpose(bY[:, cc * 128:(cc + 1) * 128], xT[:, c, t * 128:(t + 1) * 128], identF[:])
                                    return ins
                                P.op(pe, R(xT, identF), R(bY), tr)
                                evac_copy(xo[:, half * 512:(half + 1) * 512], xo.r, bY[:, :], bY.r, eng=(act if half == 0 else dve))
                            r0 = blk * NT + t * 128
                            P.dma(sp, outr, xo.r, lambda e, xo=xo, r0=r0: e.dma_start(out=out_d[r0:r0 + 128, :], in_=xo[:]))
                add_step(None, compZ)

        xsr = [P.res("xs%d" % b) for b in range(NB)]
        outr = P.res("out")
        if steps[0][0]:
            steps[0][0]()
        for i, (ld, comp) in enumerate(steps):
            if i + 1 < len(steps) and steps[i + 1][0]:
                steps[i + 1][0]()
            comp()
        P.wait_all(sp, [outr])
        sp.e.wait_ge(outr.dsem["hw"], outr.dcnt["hw"])
    return nc


def _col(v):
    v = np.asarray(v, np.float32).reshape(-1, 128)
    return v.T


def _pack_vecs(inp, b):
    cols = [_col(inp["c"][b]), _col(inp["final_g"])]
    for l in range(DEPTH):
        cols += [_col(inp["norm_mix_g"][l]), _col(inp["norm_ffn_g"][l])]
        cw = inp["rg_conv_w"][l]
        cols += [_col(cw[k]) for k in range(4)]
        cols += [_col(inp["rg_conv_b"][l]), _col(inp["rg_ba"][l]), _col(inp["rg_bx"][l]), _col(inp["rg_lambda"][l]),
                 _col(inp["gla_bg"][l]), _col(inp["gla_norm_g"][l]), _col(inp["ada_b"][l])]
    v = np.ascontiguousarray(np.concatenate(cols, axis=1), dtype=np.float32)
    assert v.shape == (128, NV), v.shape
    return v


_NC_CACHE = {}


def kernel(**inputs):
    inp = {k: np.asarray(v) for k, v in inputs.items()}
    if "nc" not in _NC_CACHE:
        _NC_CACHE["nc"] = build()
    nc = _NC_CACHE["nc"]
    shared = {k: np.ascontiguousarray(inp[k], dtype=np.float32) for k in
              ("ada_w", "w_in", "rg_wa", "rg_wx", "gla_wg2", "w_out", "ffn_w1", "ffn_w3", "ffn_w2",
               "router_w", "moe_w1", "moe_w3", "moe_w2")}
    B = inp["x"].shape[0]
    in_maps = []
    for b in range(B):
        m = dict(shared)
        m["x"] = np.ascontiguousarray(inp["x"][b], dtype=np.float32)
        m["vecs"] = _pack_vecs(inp, b)
        in_maps.append(m)
    res = run_bass_kernel_spmd(nc, in_maps, core_ids=list(range(B)))
    out = np.stack([np.asarray(res.results[b]["out"], dtype=np.float32) for b in range(B)], axis=0)
    return out
```

```python
import numpy as np
from contextlib import ExitStack
import concourse.bass as bass
import concourse.mybir as mybir
from concourse.bass_utils import run_bass_kernel_spmd

F32 = mybir.dt.float32
BF16 = mybir.dt.bfloat16
AF = mybir.ActivationFunctionType
ALU = mybir.AluOpType
AX = mybir.AxisListType

D = 1024
S = 4096
NT = 512
NB = S // NT
DFF = 2816
NE = 8
DIN = 2576
DEPTH = 4
EPS = 1e-6
LV = 99
NV = 16 + DEPTH * LV
GROUPS = [(0, 4), (4, 4), (8, 4), (12, 4), (16, 4), (20, 2)]


class Res:
    __slots__ = ("name", "w", "r", "dsem", "dcnt", "children", "parent")

    def __init__(self, name):
        self.name = name
        self.w = None
        self.r = {}
        self.dsem = {}
        self.dcnt = {}
        self.children = []
        self.parent = None


class Eng:
    def __init__(self, name, e, sem):
        self.name, self.e, self.sem, self.cnt, self.known = name, e, sem, 0, {}


class _FirstWait:
    def __init__(self, e, wait):
        self.e, self.wait, self.done = e, wait, wait is None

    def _w(self, ins):
        if not self.done:
            ins._wait_ge(self.wait[0], self.wait[1])
            self.done = True
        return ins

    def matmul(self, *a, **k):
        return self._w(self.e.matmul(*a, **k))

    def transpose(self, *a, **k):
        return self._w(self.e.transpose(*a, **k))


class Prog:
    def __init__(self, nc, es):
        self.nc, self.es = nc, es
        self.pe = Eng("pe", nc.tensor, es.enter_context(nc.semaphore("s_pe")))
        self.act = Eng("act", nc.scalar, es.enter_context(nc.semaphore("s_act")))
        self.dve = Eng("dve", nc.vector, es.enter_context(nc.semaphore("s_dve")))
        self.pool = Eng("pool", nc.gpsimd, es.enter_context(nc.semaphore("s_pool")))
        self.sp = Eng("sp", nc.sync, es.enter_context(nc.semaphore("s_sp")))
        self.nres = 0

    def res(self, name):
        return Res(name)

    def _need(self, eng, reads, writes):
        def expand(rs):
            out = []
            for r in rs:
                out.append(r)
                out.extend(r.children)
                if r.parent is not None:
                    out.append(r.parent)
            return out

        rd, wr = {}, {}

        def add(dct, tok):
            if tok is None:
                return
            s, v = tok
            if dct.get(id(s), (None, 0))[1] < v:
                dct[id(s)] = (s, v)

        for r in expand(reads):
            add(rd, r.w)
        for w in expand(writes):
            add(wr, w.w)
            for s, v in w.r.values():
                add(wr, (s, v))
        need_r, need_w = [], []
        for k, (s, v) in rd.items():
            if eng is self.pe and s is self.pe.sem:
                continue
            if eng.known.get(k, 0) < v:
                need_r.append((s, v))
        for k, (s, v) in wr.items():
            if eng is self.pe and s is self.pe.sem:
                continue
            if k in rd and rd[k][1] >= v:
                continue
            if eng.known.get(k, 0) < v:
                need_w.append((s, v))
        return need_r, need_w

    def _deps(self, eng, reads, writes):
        need_r, need_w = self._need(eng, reads, writes)
        for s, v in need_r + need_w:
            if eng.known.get(id(s), 0) < v:
                eng.e.wait_ge(s, v)
                eng.known[id(s)] = v

    def op(self, eng, reads, writes, fn):
        need_r, need_w = self._need(eng, reads, writes)
        embed = None
        if eng is self.pe:
            standalone = need_r + need_w[:-1]
            if need_w:
                embed = need_w[-1]
        else:
            allw = need_r + need_w
            standalone = allw[:-1]
            if allw:
                embed = allw[-1]
        for s, v in standalone:
            if eng.known.get(id(s), 0) < v:
                eng.e.wait_ge(s, v)
                eng.known[id(s)] = v
        if eng is self.pe:
            prox = _FirstWait(eng.e, embed)
            ins = fn(prox)
            if embed is not None and not prox.done:
                raise RuntimeError("embedded wait not consumed")
        else:
            ins = fn(eng.e)
            if embed is not None:
                ins._wait_ge(embed[0], embed[1])
        if embed is not None:
            eng.known[id(embed[0])] = max(eng.known.get(id(embed[0]), 0), embed[1])
        eng.cnt += 1
        ins.then_inc(eng.sem, 1)
        tok = (eng.sem, eng.cnt)
        for r in reads:
            r.r[id(eng.sem)] = tok
        for w in writes:
            w.w = tok
            w.r = {}
        return ins

    def dma(self, q, out_res, in_res, fn):
        self._deps(q, [in_res], [out_res])
        kq = "sw" if q is self.pool else "hw"
        if kq not in out_res.dsem:
            self.nres += 1
            out_res.dsem[kq] = self.es.enter_context(self.nc.semaphore("d%d" % self.nres))
            out_res.dcnt[kq] = 0
        ins = fn(q.e)
        out_res.dcnt[kq] += 16
        dsem = out_res.dsem[kq]
        ins.then_inc(dsem, 16)
        tok = (dsem, out_res.dcnt[kq])
        in_res.r[id(dsem)] = tok
        out_res.w = tok
        out_res.r = {}
        return ins

    def wait_all(self, q, ress):
        self._deps(q, ress, [])


def build(nlayers=DEPTH):
    nc = bass.Bass("TRN2", target_bir_lowering=False)

    def din(name, shape):
        return nc.dram_tensor(name, shape, F32, kind="ExternalInput").ap()

    x_d = din("x", [S, D])
    vec_d = din("vecs", [128, NV])
    ada_w_d = din("ada_w", [DEPTH, D, 6 * D])
    w_in_d = din("w_in", [DEPTH, D, DIN])
    wa_d = din("rg_wa", [DEPTH, 8, 64, 64])
    wx_d = din("rg_wx", [DEPTH, 8, 64, 64])
    wg2_d = din("gla_wg2", [DEPTH, 16, 256])
    w_out_d = din("w_out", [DEPTH, D, D])
    fw1_d = din("ffn_w1", [2, D, DFF])
    fw3_d = din("ffn_w3", [2, D, DFF])
    fw2_d = din("ffn_w2", [2, DFF, D])
    rw_d = din("router_w", [2, D, NE])
    mw1_d = din("moe_w1", [2, NE, D, DFF])
    mw3_d = din("moe_w3", [2, NE, D, DFF])
    mw2_d = din("moe_w2", [2, NE, DFF, D])
    out_d = nc.dram_tensor("out", [S, D], F32, kind="ExternalOutput").ap()
    xs_d = nc.dram_tensor("xs", [NB, 128, 8, NT], F32, kind="Internal").ap()
    wsc_l = [nc.dram_tensor("wsc%d" % l, [7 + (144 if l % 2 == 1 else 18), 128, 4096], BF16, kind="Internal").ap() for l in range(DEPTH)]

    with ExitStack() as es:
        P = Prog(nc, es)
        pe, act, dve, pool, sp = P.pe, P.act, P.dve, P.pool, P.sp

        class T:
            def __init__(self, name, shape, dt, psum=False):
                if psum:
                    self.t = es.enter_context(nc.psum_tensor(name, shape, dt))
                else:
                    self.t = es.enter_context(nc.sbuf_tensor(name, shape, dt))
                self.r = P.res(name)

            def __getitem__(self, k):
                return self.t[k]

        def sb(name, shape, dt=F32):
            return T(name, shape, dt)

        dummy = P.res("dram_in")

        identF = sb("identF", [128, 128])
        identB = sb("identB", [128, 128], BF16)
        onesB = sb("onesB", [128, 128], BF16)
        maskT = sb("maskT", [128, 128])
        rowm = sb("rowm", [128, 2])
        resetm = sb("resetm", [128, NT])
        selE = sb("selE", [8, NE, 128])
        vec = sb("vec", [128, NV])
        cact2 = sb("cact2", [128, 8, 2])
        modall = sb("modall", [128, DEPTH, 48])
        lvec = sb("lvec", [128, 40])
        WaBD = sb("WaBD", [128, 4, 128], BF16)
        WxBD = sb("WxBD", [128, 4, 128], BF16)
        wg2b = sb("wg2b", [16, 256], BF16)
        flw = sb("flw", [128, 8, 16], BF16)
        rwb = sb("rwb", [128, 8, NE], BF16)
        halo = sb("halo", [128, 4, 3])
        hst = sb("hst", [128, 4])
        Sst = [sb("Sst%d" % p, [128, 128]) for p in range(2)]
        xT = sb("xT", [128, 8, NT])
        xTc = [P.res("xTc%d" % c) for c in range(8)]
        for r_ in xTc:
            r_.parent = xT.r
        xT.r.children = list(xTc)
        tmpY = [sb("tmpY%d" % i, [128, NT]) for i in range(2)]
        hT = sb("hT", [128, 8, NT], BF16)
        hgT = sb("hgT", [128, 8, NT], BF16)
        rstd = sb("rstd", [128, NT])
        tmpx = [sb("tmpx%d" % i, [128, NT]) for i in range(1)]
        xin = [sb("xin%d" % i, [128, D]) for i in range(2)]
        KS = [sb("KS%d" % i, [128, 8, 512], BF16) for i in range(5)]
        FS = [sb("FS%d" % i, [128, 4, D], BF16) for i in range(3)]
        gT = [sb("gT%d" % i, [128, 4, NT], BF16) for i in range(2)]
        sl = [sb("sl%d" % i, [128, NT]) for i in range(2)]
        rgxh = sb("rgxh", [128, NT + 3])
        Ub = sb("Ub", [128, NT], BF16)
        mU, mR, mI, mA, mM, mGG, mG2 = [sb("m%s" % n, [128, NT]) for n in ("U", "R", "I", "A", "M", "GG", "G2")]
        mixo = sb("mixo", [128, 8, NT], BF16)
        flowT = sb("flowT", [16, NT], BF16)
        vtok = sb("vtok", [128, 4, 512], BF16)
        gsil = sb("gsil", [128, 4, NT])
        qT, kT, LF, Bc, EQ, EK = [sb("g%s" % n, [128, NT]) for n in ("q", "k", "LF", "Bc", "EQ", "EK")]
        qloc = sb("qloc", [128, NT], BF16)
        kl = [sb("kl%d" % i, [128, NT], BF16) for i in range(2)]
        klf = sb("klf", [128, NT], BF16)
        kltok = sb("kltok", [128, 4, 128], BF16)
        dsm = sb("dsm", [128, 6, 4])
        Sb = [sb("Sb%d" % i, [128, 128], BF16) for i in range(2)]
        scm = [sb("scm%d" % i, [128, 128], BF16) for i in range(2)]
        T1 = [sb("T1_%d" % i, [128, 128]) for i in range(2)]
        oT = sb("oT", [128, NT])
        sqo = sb("sqo", [128, NT], BF16)
        rso = sb("rso", [128, NT])
        LG = sb("LG", [128, 4, NE])
        L2 = sb("L2", [128, 4, NE])
        GT = sb("GT", [128, 4, NE])
        sm4 = sb("sm4", [128, 4, 4])
        GTT = sb("GTT", [8, NT])
        gbc = sb("gbc", [128, NT], BF16)
        PA = [T("psA%d" % i, [128, 512], F32, psum=True) for i in range(2)]
        PB = [T("psB%d" % i, [128, 512], F32, psum=True) for i in range(2)]
        PY = [T("psY%d" % i, [128, 512], F32, psum=True) for i in range(2)]
        PM = T("psM", [128, 512], F32, psum=True)
        PT = T("psT", [128, 1024], BF16, psum=True)
        ctr = {"A": 0, "B": 0, "Y": 0, "KS": 0, "FS": 0, "gT": 0, "sl": 0, "tmpx": 0, "xin": 0, "tmpY": 0, "Y3": 0}

        def nxt(kind, arr):
            i = ctr[kind]
            ctr[kind] = i + 1
            return arr[i % len(arr)]

        def R(*ts):
            return [t.r for t in ts]

        imgs = {}
        wbres = [P.res("wb%d" % i) for i in range(8)]
        wbctr = [0]

        def load_slot(slot, key, blk, src_fn, kmajor):
            pat = "p (k f) -> p k f" if kmajor else "p (j d) -> p j d"
            kw = {"k": 8} if kmajor else {"j": 4}
            wsc_d = wsc_l[key[0]]
            if blk == 0:
                idx = sum(1 for kk in imgs if kk[0] == key[0])
                P.dma(pool, slot.r, dummy, src_fn)
                wr = wbres[wbctr[0] % 8]
                wbctr[0] += 1
                P.dma(sp, wr, slot.r, lambda e: e.dma_start(out=wsc_d[idx].rearrange(pat, **kw), in_=slot[:]))
                ir = P.res("img%d" % idx)
                ir.w = wr.w
                imgs[key] = (idx, ir)
            else:
                idx, ir = imgs[key]
                P.dma(sp, slot.r, ir, lambda e: e.dma_start(out=slot[:], in_=wsc_d[idx].rearrange(pat, **kw)))

        P.op(pool, [], R(identF), lambda e: e.memset(identF[:], 1.0))
        P.op(pool, R(identF), R(identF), lambda e: e.affine_select(out=identF[:], in_=identF[:], pattern=[[1, 128]], compare_op=ALU.is_equal, fill=0.0, base=0, channel_multiplier=-1))
        P.op(pool, [], R(maskT), lambda e: e.memset(maskT[:], 1.0))
        P.op(pool, R(maskT), R(maskT), lambda e: e.affine_select(out=maskT[:], in_=maskT[:], pattern=[[1, 128]], compare_op=ALU.is_ge, fill=0.0, base=0, channel_multiplier=-1))
        P.op(pool, [], R(rowm), lambda e: e.memset(rowm[:], 1.0))
        P.op(pool, R(rowm), R(rowm), lambda e: e.affine_select(out=rowm[:, 0:1], in_=rowm[:, 0:1], pattern=[[0, 1]], compare_op=ALU.is_ge, fill=0.0, base=63, channel_multiplier=-1))
        P.op(pool, R(rowm), R(rowm), lambda e: e.affine_select(out=rowm[:, 1:2], in_=rowm[:, 1:2], pattern=[[0, 1]], compare_op=ALU.is_ge, fill=0.0, base=-64, channel_multiplier=1))
        P.op(pool, [], R(selE), lambda e: e.memset(selE[:], 1.0))
        P.op(pool, R(selE), R(selE), lambda e: e.affine_select(out=selE[:], in_=selE[:], pattern=[[-1, NE], [0, 128]], compare_op=ALU.is_equal, fill=0.0, base=0, channel_multiplier=1))
        P.op(pool, [], R(onesB), lambda e: e.memset(onesB[:], 1.0))
        P.op(pool, [], R(resetm), lambda e: e.memset(resetm[:], 1.0))
        for t in range(NT // 128):
            P.op(pool, R(resetm), R(resetm), lambda e, t=t: e.memset(resetm[:, t * 128:t * 128 + 1], 0.0))
        P.op(pool, R(identF), R(identB), lambda e: e.tensor_copy(out=identB[:], in_=identF[:]))

        P.dma(sp, vec.r, dummy, lambda e: e.dma_start(out=vec[:], in_=vec_d[:, :]))
        P.op(act, R(vec), R(cact2), lambda e: e.activation(out=cact2[:, :, 0], in_=vec[:, 0:8], func=AF.Silu))
        P.op(act, R(vec), R(cact2), lambda e: e.activation(out=cact2[:, :, 1], in_=vec[:, 0:8], func=AF.Silu))

        for l in range(nlayers):
            vb = 16 + l * LV
            for pc in range(12):
                P.dma(sp, xT.r, dummy, lambda e, l=l, pc=pc: e.dma_start(
                    out=xT[:], in_=ada_w_d[l, :, pc * 512:(pc + 1) * 512].rearrange("(k p) f -> p k f", p=128)))

                def mm(e, pc=pc):
                    ins = None
                    for jj in range(4):
                        j = pc * 4 + jj
                        for k in range(8):
                            ins = e.matmul(PM[:, 2 * j:2 * j + 2], lhsT=xT[:, k, jj * 128:(jj + 1) * 128], rhs=cact2[:, k, :], start=(k == 0), stop=(k == 7))
                    return ins
                P.op(pe, R(xT, cact2), R(PM), mm)
            pmv = PM[:, 0:96].rearrange("p (j t) -> p j t", t=2)
            P.op(dve, R(PM, vec), R(modall), lambda e, l=l, vb=vb, pmv=pmv: e.tensor_tensor(
                out=modall[:, l, :], in0=pmv[:, :, 0], in1=vec[:, vb + 51:vb + 99], op=ALU.add))

        def evac_copy(dst_ap, dst_res, src_ap, src_res, eng=None):
            eng = eng or act
            if eng is act:
                P.op(act, [src_res], [dst_res], lambda e: e.copy(out=dst_ap, in_=src_ap))
            else:
                P.op(dve, [src_res], [dst_res], lambda e: e.tensor_copy(out=dst_ap, in_=src_ap))

        def proj(wslot, c0, width, bank, rhs_of_k=None, m_out=128):
            def f(e):
                ins = None
                for k in range(8):
                    ins = e.matmul(bank[0:width, :], lhsT=wslot[:, k, c0:c0 + width], rhs=hT[:, k, :], start=(k == 0), stop=(k == 7))
                return ins
            P.op(pe, R(wslot, hT), R(bank), f)

        def norm_mod(s1_ap, sh_ap):
            for c in range(8):
                P.op(act, R(xT), R(hgT), lambda e, c=c: e.activation(out=hgT[:, c, :], in_=xT[:, c, :], func=AF.Square))

            def f(e):
                ins = None
                for c in range(8):
                    ins = e.matmul(PM[:, :], lhsT=onesB[:], rhs=hgT[:, c, :], start=(c == 0), stop=(c == 7))
                return ins
            P.op(pe, R(onesB, hgT), R(PM), f)
            P.op(act, R(PM), R(rstd), lambda e: e.activation(out=rstd[:], in_=PM[:, :], func=AF.Ln, scale=1.0 / D, bias=EPS))
            P.op(act, R(rstd), R(rstd), lambda e: e.activation(out=rstd[:], in_=rstd[:], func=AF.Exp, scale=-0.5))
            for c in range(8):
                tx = nxt("tmpx", tmpx + tmpY)
                P.op(dve, R(xT, rstd, lvec), R(tx), lambda e, c=c, tx=tx: e.scalar_tensor_tensor(
                    out=tx[:], in0=xT[:, c, :], scalar=s1_ap[:, c:c + 1], in1=rstd[:], op0=ALU.mult, op1=ALU.mult))
                P.op(act, R(tx, modall), R(hT), lambda e, c=c, tx=tx: e.activation(
                    out=hT[:, c, :], in_=tx[:], func=AF.Identity, bias=sh_ap[:, c:c + 1]))

        def resid(bank, dc, gt_ap):
            if dc % 2 == 0:
                P.op(dve, [bank.r, modall.r, xTc[dc]], [xTc[dc]], lambda e: e.scalar_tensor_tensor(
                    out=xT[:, dc, :], in0=bank[:, :], scalar=gt_ap[:, dc:dc + 1], in1=xT[:, dc, :], op0=ALU.mult, op1=ALU.add))
            else:
                ty = nxt("tmpY", tmpY)
                P.op(act, [bank.r, modall.r], [ty.r], lambda e: e.activation(out=ty[:], in_=bank[:, :], func=AF.Identity, scale=gt_ap[:, dc:dc + 1]))
                P.op(pool, [ty.r, xTc[dc]], [xTc[dc]], lambda e: e.tensor_tensor(out=xT[:, dc, :], in0=xT[:, dc, :], in1=ty[:], op=ALU.add))

        steps = []

        def add_step(loads, compute):
            steps.append((loads, compute))

        for l in range(nlayers):
            vb = 16 + l * LV
            moe = (l % 2 == 1)
            jl = l // 2
            mod = lambda j0, l=l: modall[:, l, j0:j0 + 8]
            shm, gtm, shf, gtf = mod(0), mod(16), mod(24), mod(40)
            s1m, s1f, spv, sp2v = lvec[:, 0:8], lvec[:, 8:16], lvec[:, 16:20], lvec[:, 20:24]
            cw = lambda k, c, vb=vb: vec[:, vb + 16 + k * 4 + c:vb + 16 + k * 4 + c + 1]
            cb = lambda c, vb=vb: vec[:, vb + 32 + c:vb + 33 + c]
            ba = lambda c, vb=vb: vec[:, vb + 36 + c:vb + 37 + c]
            bx = lambda c, vb=vb: vec[:, vb + 40 + c:vb + 41 + c]
            bg = lambda p, vb=vb: vec[:, vb + 48 + p:vb + 49 + p]
            glag = vec[:, vb + 50:vb + 51]

            def layer_setup(l=l, vb=vb, moe=moe, jl=jl):
                P.op(dve, R(modall, vec), R(lvec), lambda e: e.scalar_tensor_tensor(
                    out=lvec[:, 0:8], in0=modall[:, l, 8:16], scalar=1.0, in1=vec[:, vb:vb + 8], op0=ALU.add, op1=ALU.mult))
                P.op(dve, R(modall, vec), R(lvec), lambda e: e.scalar_tensor_tensor(
                    out=lvec[:, 8:16], in0=modall[:, l, 32:40], scalar=1.0, in1=vec[:, vb + 8:vb + 16], op0=ALU.add, op1=ALU.mult))
                P.op(act, R(vec), R(lvec), lambda e: e.activation(out=lvec[:, 24:28], in_=vec[:, vb + 44:vb + 48], func=AF.Exp, scale=-1.0))
                P.op(act, R(lvec), R(lvec), lambda e: e.activation(out=lvec[:, 24:28], in_=lvec[:, 24:28], func=AF.Ln, bias=1.0))
                P.op(dve, R(lvec), R(lvec), lambda e: e.tensor_scalar(out=lvec[:, 16:20], in0=lvec[:, 24:28], scalar1=-8.0, scalar2=None, op0=ALU.mult))
                P.op(dve, R(lvec), R(lvec), lambda e: e.tensor_scalar(out=lvec[:, 20:24], in0=lvec[:, 24:28], scalar1=-16.0, scalar2=None, op0=ALU.mult))
                P.op(dve, [], R(WaBD), lambda e: e.memset(WaBD[:], 0.0))
                P.op(dve, [], R(WxBD), lambda e: e.memset(WxBD[:], 0.0))
                for n in range(8):
                    pb = (n % 2) * 64
                    P.dma(pool, WaBD.r, dummy, lambda e, n=n, pb=pb: e.dma_start(out=WaBD[pb:pb + 64, n // 2, pb:pb + 64], in_=wa_d[l, n, :, :]))
                    P.dma(pool, WxBD.r, dummy, lambda e, n=n, pb=pb: e.dma_start(out=WxBD[pb:pb + 64, n // 2, pb:pb + 64], in_=wx_d[l, n, :, :]))
                P.dma(pool, wg2b.r, dummy, lambda e: e.dma_start(out=wg2b[:], in_=wg2_d[l, :, :]))
                P.dma(pool, flw.r, dummy, lambda e: e.dma_start(out=flw[:], in_=w_in_d[l, :, 2560:2576].rearrange("(k p) f -> p k f", p=128)))
                if moe:
                    P.dma(pool, rwb.r, dummy, lambda e: e.dma_start(out=rwb[:], in_=rw_d[jl, :, :].rearrange("(k p) f -> p k f", p=128)))
                P.op(dve, [], R(halo), lambda e: e.memset(halo[:], 0.0))
                P.op(dve, [], R(hst), lambda e: e.memset(hst[:], 0.0))
                for p in range(2):
                    P.op(dve, [], R(Sst[p]), lambda e, p=p: e.memset(Sst[p][:], 0.0))

            for blk in range(NB):
                first_l, last_l = (l == 0), (l == nlayers - 1)
                slots = {}

                def ld_in(names, l=l, slots=slots, blk=blk):
                    def f():
                        for nm, c0 in names:
                            s = nxt("KS", KS)
                            slots[nm] = s
                            load_slot(s, (l, "in", nm), blk, lambda e, s=s, c0=c0: e.dma_start(
                                out=s[:], in_=w_in_d[l, :, c0:c0 + 512].rearrange("(k p) f -> p k f", p=128)), True)
                    return f

                def compA(l=l, blk=blk, slots=slots, first_l=first_l, shm=shm, s1m=s1m, cw=cw, cb=cb, ba=ba, bx=bx, spv=spv, sp2v=sp2v, layer_setup=layer_setup):
                    if blk == 0:
                        layer_setup()
                    if first_l:
                        for t in range(4):
                            xi = nxt("xin", xin)
                            r0 = blk * NT + t * 128
                            P.dma(sp, xi.r, dummy, lambda e, xi=xi, r0=r0: e.dma_start(out=xi[:], in_=x_d[r0:r0 + 128, :]))
                            for half in range(2):
                                bT = nxt("Y", PY)

                                def tr(e, xi=xi, half=half, bT=bT):
                                    ins = None
                                    for cc in range(4):
                                        c = half * 4 + cc
                                        ins = e.transpose(bT[:, cc * 128:(cc + 1) * 128], xi[:, c * 128:(c + 1) * 128], identF[:])
                                    return ins
                                P.op(pe, R(xi, identF), R(bT), tr)
                                if half == 0:
                                    P.op(dve, R(bT), R(xT), lambda e, t=t, half=half, bT=bT: e.tensor_copy(
                                        out=xT[:, half * 4:half * 4 + 4, t * 128:(t + 1) * 128], in_=bT[:, :].rearrange("p (c t) -> p c t", t=128)))
                                else:
                                    P.op(act, R(bT), R(xT), lambda e, t=t, half=half, bT=bT: e.copy(
                                        out=xT[:, half * 4:half * 4 + 4, t * 128:(t + 1) * 128], in_=bT[:, :].rearrange("p (c t) -> p c t", t=128)))
                    else:
                        P.dma(sp, xT.r, xsr[blk], lambda e: e.dma_start(out=xT[:], in_=xs_d[blk]))
                    norm_mod(s1m, shm)
                    P0, P1 = slots["rgx"], slots["rgg"]
                    for c in range(4):
                        bA = PA[0]
                        proj(P0, c * 128, 128, bA)
                        yield
                        P.op(dve, R(halo), R(rgxh), lambda e, c=c: e.tensor_copy(out=rgxh[:, 0:3], in_=halo[:, c, :]))
                        yield
                        evac_copy(rgxh[:, 3:NT + 3], rgxh.r, bA[:, :], bA.r)
                        yield
                        bB = PB[0]
                        proj(P1, c * 128, 128, bB)
                        yield
                        evac_copy(mGG[:], mGG.r, bB[:, :], bB.r)
                        yield
                        P.op(dve, R(rgxh, vec), R(mU), lambda e, c=c: e.tensor_scalar(
                            out=mU[:], in0=rgxh[:, 3:NT + 3], scalar1=cw(3, c), scalar2=cb(c), op0=ALU.mult, op1=ALU.add))
                        yield
                        for k in range(3):
                            P.op(dve, R(rgxh, vec, mU), R(mU), lambda e, c=c, k=k: e.scalar_tensor_tensor(
                                out=mU[:], in0=rgxh[:, k:k + NT], scalar=cw(k, c), in1=mU[:], op0=ALU.mult, op1=ALU.add))
                            yield
                        P.op(dve, R(rgxh), R(halo), lambda e, c=c: e.tensor_copy(out=halo[:, c, :], in_=rgxh[:, NT:NT + 3]))
                        yield
                        P.op(act, R(mU), R(Ub), lambda e: e.copy(out=Ub[:], in_=mU[:]))
                        yield
                        bA = PA[0]
                        P.op(pe, R(WaBD, Ub), R(bA), lambda e, c=c, bA=bA: e.matmul(bA[:, :], lhsT=WaBD[:, c, :], rhs=Ub[:], start=True, stop=True))
                        yield
                        bB = PB[0]
                        P.op(pe, R(WxBD, Ub), R(bB), lambda e, c=c, bB=bB: e.matmul(bB[:, :], lhsT=WxBD[:, c, :], rhs=Ub[:], start=True, stop=True))
                        yield
                        P.op(act, R(bA, vec), R(mR), lambda e, c=c, bA=bA: e.activation(out=mR[:], in_=bA[:, :], func=AF.Sigmoid, bias=ba(c)))
                        yield
                        P.op(act, R(bB, vec), R(mI), lambda e, c=c, bB=bB: e.activation(out=mI[:], in_=bB[:, :], func=AF.Sigmoid, bias=bx(c)))
                        yield
                        P.op(act, R(mR, lvec), R(mA), lambda e, c=c: e.activation(out=mA[:], in_=mR[:], func=AF.Exp, scale=spv[:, c:c + 1]))
                        yield
                        P.op(act, R(mR, lvec), R(mM), lambda e, c=c: e.activation(out=mM[:], in_=mR[:], func=AF.Exp, scale=sp2v[:, c:c + 1]))
                        yield
                        P.op(act, R(mM), R(mM), lambda e: e.activation(out=mM[:], in_=mM[:], func=AF.Sqrt, scale=-1.0, bias=1.0))
                        yield
                        if blk == 0:
                            P.op(dve, [], R(mM), lambda e: e.memset(mM[:, 0:1], 1.0))
                            yield
                        P.op(dve, R(mM, mI), R(mM), lambda e: e.tensor_tensor(out=mM[:], in0=mM[:], in1=mI[:], op=ALU.mult))
                        yield
                        P.op(dve, R(mM, mU), R(mM), lambda e: e.tensor_tensor(out=mM[:], in0=mM[:], in1=mU[:], op=ALU.mult))
                        yield
                        P.op(dve, R(mA, mM, hst), R(mR), lambda e, c=c: e.tensor_tensor_scan(
                            out=mR[:], data0=mA[:], data1=mM[:], initial=hst[:, c:c + 1], op0=ALU.mult, op1=ALU.add))
                        yield
                        P.op(dve, R(mR), R(hst), lambda e, c=c: e.tensor_copy(out=hst[:, c:c + 1], in_=mR[:, NT - 1:NT]))
                        yield
                        P.op(act, R(mGG), R(mG2), lambda e: e.activation(out=mG2[:], in_=mGG[:], func=AF.Square))
                        yield
                        P.op(dve, R(mG2), R(mG2), lambda e: e.tensor_scalar(out=mG2[:], in0=mG2[:], scalar1=0.044715, scalar2=1.0, op0=ALU.mult, op1=ALU.add))
                        yield
                        P.op(dve, R(mG2, mGG), R(mG2), lambda e: e.tensor_tensor(out=mG2[:], in0=mG2[:], in1=mGG[:], op=ALU.mult))
                        yield
                        P.op(act, R(mG2), R(mG2), lambda e: e.activation(out=mG2[:], in_=mG2[:], func=AF.Sigmoid, scale=1.5957691216057308))
                        yield
                        P.op(dve, R(mG2, mGG), R(mG2), lambda e: e.tensor_tensor(out=mG2[:], in0=mG2[:], in1=mGG[:], op=ALU.mult))
                        yield
                        P.op(dve, R(mR, mG2), R(mixo), lambda e, c=c: e.tensor_tensor(out=mixo[:, c, :], in0=mR[:], in1=mG2[:], op=ALU.mult))
                        yield

                def compB(l=l, blk=blk, slots=slots, bg=bg, glag=glag):
                    P2, P3, P4 = slots["qk"], slots["v"], slots["g"]
                    bA = PA[1]

                    def ff(e, bA=bA):
                        ins = None
                        for k in range(8):
                            ins = e.matmul(bA[0:16, :], lhsT=flw[:, k, :], rhs=hT[:, k, :], start=(k == 0), stop=(k == 7))
                        return ins
                    P.op(pe, R(flw, hT), R(bA), ff)
                    yield
                    evac_copy(flowT[:], flowT.r, bA[0:16, :], bA.r)
                    yield
                    for t in range(4):
                        bB = PB[1]

                        def fv(e, t=t, bB=bB):
                            ins = None
                            for k in range(8):
                                ins = e.matmul(bB[:, :], lhsT=hT[:, k, t * 128:(t + 1) * 128], rhs=P3[:, k, :], start=(k == 0), stop=(k == 7))
                            return ins
                        P.op(pe, R(P3, hT), R(bB), fv)
                        yield
                        evac_copy(vtok[:, t, :], vtok.r, bB[:, :], bB.r, eng=(act if t % 2 == 0 else dve))
                        yield
                    for hd in range(4):
                        bA = PA[1]
                        proj(P4, hd * 128, 128, bA)
                        yield
                        P.op(act, R(bA), R(gsil), lambda e, hd=hd, bA=bA: e.activation(out=gsil[:, hd, :], in_=bA[:, :], func=AF.Silu))
                        yield
                    for p in range(2):
                        bA = PA[1]
                        proj(P2, p * 128, 128, bA)
                        yield
                        evac_copy(qT[:], qT.r, bA[:, :], bA.r)
                        yield
                        bB = PB[1]
                        proj(P2, 256 + p * 128, 128, bB)
                        yield
                        evac_copy(kT[:], kT.r, bB[:, :], bB.r, eng=dve)
                        yield
                        bA = PA[1]
                        P.op(pe, R(wg2b, flowT), R(bA), lambda e, p=p, bA=bA: e.matmul(
                            bA[:, :], lhsT=wg2b[0:16, p * 128:(p + 1) * 128], rhs=flowT[0:16, :], start=True, stop=True))
                        yield
                        P.op(act, R(bA, vec), R(LF), lambda e, p=p, bA=bA: e.activation(out=LF[:], in_=bA[:, :], func=AF.Sigmoid, bias=bg(p)))
                        yield
                        P.op(act, R(LF), R(LF), lambda e: e.activation(out=LF[:], in_=LF[:], func=AF.Ln))
                        yield
                        P.op(dve, R(resetm, LF), R(Bc), lambda e: e.tensor_tensor_scan(
                            out=Bc[:], data0=resetm[:], data1=LF[:], initial=0.0, op0=ALU.mult, op1=ALU.add))
                        yield
                        Bv = Bc[:].rearrange("p (c t) -> p c t", t=128)
                        P.op(dve, R(Bc), R(EQ), lambda e, Bv=Bv: e.tensor_tensor(
                            out=EQ[:].rearrange("p (c t) -> p c t", t=128), in0=Bv, in1=Bv[:, :, 63:64].to_broadcast([128, 4, 128]), op=ALU.subtract))
                        yield
                        P.op(act, R(EQ), R(EK), lambda e: e.activation(out=EK[:], in_=EQ[:], func=AF.Exp, scale=-1.0 / 16))
                        yield
                        P.op(act, R(EQ), R(EQ), lambda e: e.activation(out=EQ[:], in_=EQ[:], func=AF.Exp, scale=1.0 / 16))
                        yield
                        P.op(dve, R(qT, EQ), R(qloc), lambda e: e.scalar_tensor_tensor(
                            out=qloc[:], in0=qT[:], scalar=0.125, in1=EQ[:], op0=ALU.mult, op1=ALU.mult))
                        yield
                        for hp in range(2):
                            P.op(dve, R(kT, EK, rowm), R(kl[hp]), lambda e, hp=hp: e.scalar_tensor_tensor(
                                out=kl[hp][:], in0=kT[:], scalar=rowm[:, hp:hp + 1], in1=EK[:], op0=ALU.mult, op1=ALU.mult))
                            yield
                        P.op(dve, R(kT, EK), R(klf), lambda e: e.tensor_tensor(out=klf[:], in0=kT[:], in1=EK[:], op=ALU.mult))
                        yield
                        P.op(act, R(Bc), R(dsm), lambda e, Bv=Bv: e.activation(out=dsm[:, 0, :], in_=Bv[:, :, 127], func=AF.Exp, scale=1.0 / 16))
                        yield
                        P.op(act, R(Bc), R(dsm), lambda e, Bv=Bv: e.activation(out=dsm[:, 1, :], in_=Bv[:, :, 63], func=AF.Exp, scale=1.0 / 16))
                        yield
                        P.op(dve, R(Bc), R(dsm), lambda e, Bv=Bv: e.tensor_tensor(out=dsm[:, 5, :], in0=Bv[:, :, 127], in1=Bv[:, :, 63], op=ALU.subtract))
                        yield
                        P.op(act, R(dsm), R(dsm), lambda e: e.activation(out=dsm[:, 2, :], in_=dsm[:, 5, :], func=AF.Exp, scale=1.0 / 16))
                        yield
                        for hp in range(2):
                            P.op(dve, R(dsm, rowm), R(dsm), lambda e, hp=hp: e.tensor_scalar(
                                out=dsm[:, 3 + hp, :], in0=dsm[:, 2, :], scalar1=rowm[:, hp:hp + 1], scalar2=None, op0=ALU.mult))
                            yield
                        def ftr(e):
                            ins = None
                            for t in range(4):
                                ins = e.transpose(PT[:, t * 128:(t + 1) * 128], klf[:, t * 128:(t + 1) * 128], identB[:])
                            return ins
                        P.op(pe, R(klf, identB), R(PT), ftr)
                        yield
                        P.op(act, R(PT), R(kltok), lambda e: e.copy(out=kltok[:].rearrange("p c t -> p (c t)"), in_=PT[:, 0:512]))
                        yield
                        bO = [nxt("Y", PY), nxt("Y", PY)]
                        S_ = Sst[p]
                        for t in range(4):
                            ts = slice(t * 128, (t + 1) * 128)
                            bU = PA[1]

                            def fu(e, t=t, bU=bU, p=p):
                                e.matmul(bU[:, 0:128], lhsT=kltok[:, t, :], rhs=vtok[:, t, (2 * p) * 128:(2 * p + 1) * 128], start=True, stop=True)
                                return e.matmul(bU[:, 128:256], lhsT=kltok[:, t, :], rhs=vtok[:, t, (2 * p + 1) * 128:(2 * p + 2) * 128], start=True, stop=True)
                            P.op(pe, R(kltok, vtok), R(bU), fu)
                            yield
                            bS = PB[1]

                            def fs(e, ts=ts, bS=bS):
                                e.matmul(bS[:, 0:128], lhsT=kl[0][:, ts], rhs=qloc[:, ts], start=True, stop=True)
                                return e.matmul(bS[:, 128:256], lhsT=kl[1][:, ts], rhs=qloc[:, ts], start=True, stop=True)
                            P.op(pe, R(kl[0], kl[1], qloc), R(bS), fs)
                            yield
                            for hp in range(2):
                                P.op(dve, R(bS, maskT), R(scm[hp]), lambda e, hp=hp, bS=bS: e.tensor_tensor(
                                    out=scm[hp][:], in0=bS[:, hp * 128:(hp + 1) * 128], in1=maskT[:], op=ALU.mult))
                                yield
                                P.op(dve, R(S_, dsm, rowm), R(Sb[hp]), lambda e, hp=hp, t=t, S_=S_: e.tensor_scalar(
                                    out=Sb[hp][:], in0=S_[:], scalar1=dsm[:, 1, t:t + 1], scalar2=rowm[:, hp:hp + 1], op0=ALU.mult, op1=ALU.mult))
                                yield
                            for hp in range(2):
                                h = 2 * p + hp

                                def fo(e, hp=hp, h=h, t=t, ts=ts):
                                    e.matmul(bO[hp][:, ts], lhsT=vtok[:, t, h * 128:(h + 1) * 128], rhs=scm[hp][:], start=True, stop=False)
                                    return e.matmul(bO[hp][:, ts], lhsT=Sb[hp][:], rhs=qloc[:, ts], start=False, stop=True)
                                P.op(pe, R(vtok, scm[hp], Sb[hp], qloc), R(bO[hp]), fo)
                                yield
                            P.op(dve, R(bU, dsm), R(T1[0]), lambda e, t=t, bU=bU: e.tensor_scalar(
                                out=T1[0][:], in0=bU[:, 0:128], scalar1=dsm[:, 3, t:t + 1], scalar2=None, op0=ALU.mult))
                            yield
                            P.op(dve, R(bU, dsm), R(T1[1]), lambda e, t=t, bU=bU: e.tensor_scalar(
                                out=T1[1][:], in0=bU[:, 128:256], scalar1=dsm[:, 4, t:t + 1], scalar2=None, op0=ALU.mult))
                            yield
                            P.op(dve, R(S_, dsm, T1[0]), R(S_), lambda e, t=t, S_=S_: e.scalar_tensor_tensor(
                                out=S_[:], in0=S_[:], scalar=dsm[:, 0, t:t + 1], in1=T1[0][:], op0=ALU.mult, op1=ALU.add))
                            yield
                            P.op(dve, R(S_, T1[1]), R(S_), lambda e, S_=S_: e.tensor_tensor(out=S_[:], in0=S_[:], in1=T1[1][:], op=ALU.add))
                            yield
                        for hp in range(2):
                            h = 2 * p + hp
                            b_ = bO[hp]
                            evac_copy(oT[:], oT.r, b_[:, :], b_.r)
                            yield
                            P.op(act, R(b_), R(sqo), lambda e, b_=b_: e.activation(out=sqo[:], in_=b_[:, :], func=AF.Square))
                            yield
                            P.op(pe, R(onesB, sqo), R(PM), lambda e: e.matmul(PM[:, :], lhsT=onesB[:], rhs=sqo[:], start=True, stop=True))
                            yield
                            P.op(act, R(PM), R(rso), lambda e: e.activation(out=rso[:], in_=PM[:, :], func=AF.Ln, scale=1.0 / 128, bias=EPS))
                            yield
                            P.op(act, R(rso), R(rso), lambda e: e.activation(out=rso[:], in_=rso[:], func=AF.Exp, scale=-0.5))
                            yield
                            P.op(dve, R(oT, rso), R(oT), lambda e: e.tensor_tensor(out=oT[:], in0=oT[:], in1=rso[:], op=ALU.mult))
                            yield
                            P.op(dve, R(oT, vec, gsil), R(mixo), lambda e, h=h: e.scalar_tensor_tensor(
                                out=mixo[:, 4 + h, :], in0=oT[:], scalar=glag, in1=gsil[:, h, :], op0=ALU.mult, op1=ALU.mult))
                            yield
                def compAB(compA=compA, compB=compB):
                    gens = [compA(), compB()]
                    while gens:
                        for g_ in list(gens):
                            try:
                                next(g_)
                            except StopIteration:
                                gens.remove(g_)
                add_step(ld_in([("rgx", 0), ("rgg", 512), ("qk", 1024), ("v", 1536), ("g", 2048)]), compAB)

                wo = {}

                def ldC(l=l, wo=wo, blk=blk):
                    for i in range(2):
                        s = nxt("FS", FS)
                        wo[i] = s
                        load_slot(s, (l, "out", i), blk, lambda e, s=s, i=i: e.dma_start(
                            out=s[:], in_=w_out_d[l, i * 512:(i + 1) * 512, :].rearrange("(j p) d -> p j d", p=128)), False)

                def compC(l=l, wo=wo, gtm=gtm, s1f=s1f, shf=shf, moe=moe):
                    for dc in range(8):
                        bY = nxt("Y", PY)

                        def f(e, dc=dc, bY=bY):
                            ins = None
                            for m in range(8):
                                ins = e.matmul(bY[:, :], lhsT=wo[m // 4][:, m % 4, dc * 128:(dc + 1) * 128], rhs=mixo[:, m, :], start=(m == 0), stop=(m == 7))
                            return ins
                        P.op(pe, R(wo[0], wo[1], mixo), R(bY), f)
                        resid(bY, dc, gtm)
                    norm_mod(s1f, shf)
                    if moe:
                        def fr(e):
                            ins = None
                            for t in range(4):
                                for k in range(8):
                                    ins = e.matmul(PM[:, t * NE:(t + 1) * NE], lhsT=hT[:, k, t * 128:(t + 1) * 128], rhs=rwb[:, k, :], start=(k == 0), stop=(k == 7))
                            return ins
                        P.op(pe, R(hT, rwb), R(PM), fr)
                        P.op(dve, R(PM), R(LG), lambda e: e.tensor_copy(out=LG[:].rearrange("p c t -> p (c t)"), in_=PM[:, 0:4 * NE]))
                        bc = lambda ap: ap.to_broadcast([128, 4, NE])
                        P.op(dve, R(LG), R(sm4), lambda e: e.tensor_reduce(out=sm4[:, :, 0:1], in_=LG[:], axis=AX.X, op=ALU.max))
                        P.op(dve, R(LG, sm4), R(L2), lambda e: e.tensor_tensor(out=L2[:], in0=LG[:], in1=bc(sm4[:, :, 0:1]), op=ALU.is_equal))
                        P.op(dve, R(L2, LG), R(L2), lambda e: e.scalar_tensor_tensor(out=L2[:], in0=L2[:], scalar=-1e30, in1=LG[:], op0=ALU.mult, op1=ALU.add))
                        P.op(dve, R(L2), R(sm4), lambda e: e.tensor_reduce(out=sm4[:, :, 1:2], in_=L2[:], axis=AX.X, op=ALU.max))
                        P.op(dve, R(LG, sm4), R(L2), lambda e: e.tensor_tensor(out=L2[:], in0=LG[:], in1=bc(sm4[:, :, 1:2]), op=ALU.is_ge))
                        P.op(dve, R(LG, sm4), R(GT), lambda e: e.tensor_tensor(out=GT[:], in0=LG[:], in1=bc(sm4[:, :, 0:1]), op=ALU.subtract))
                        P.op(act, R(GT), R(GT), lambda e: e.activation(out=GT[:], in_=GT[:], func=AF.Exp))
                        P.op(dve, R(GT, L2), R(GT), lambda e: e.tensor_tensor(out=GT[:], in0=GT[:], in1=L2[:], op=ALU.mult))
                        P.op(dve, R(GT), R(sm4), lambda e: e.tensor_reduce(out=sm4[:, :, 2:3], in_=GT[:], axis=AX.X, op=ALU.add))
                        P.op(dve, R(sm4), R(sm4), lambda e: e.reciprocal(out=sm4[:, :, 3:4], in_=sm4[:, :, 2:3]))
                        P.op(dve, R(GT, sm4), R(GT), lambda e: e.tensor_tensor(out=GT[:], in0=GT[:], in1=bc(sm4[:, :, 3:4]), op=ALU.mult))

                        def ft(e):
                            ins = None
                            for t in range(4):
                                ins = e.transpose(PM[0:NE, t * 128:(t + 1) * 128], GT[:, t, :], identF[:])
                            return ins
                        P.op(pe, R(GT, identF), R(PM), ft)
                        P.op(dve, R(PM), R(GTT), lambda e: e.tensor_copy(out=GTT[:], in_=PM[0:NE, :]))
                add_step(ldC, compC)

                if moe:
                    experts = [(mw1_d[jl, ex], mw3_d[jl, ex], mw2_d[jl, ex], ex) for ex in range(NE)]
                else:
                    experts = [(fw1_d[jl], fw3_d[jl], fw2_d[jl], None)]
                for (w1a, w3a, w2a, ex) in experts:
                    for gi, (f0, g) in enumerate(GROUPS):
                        ws = {}

                        def ldF(w1a=w1a, w3a=w3a, w2a=w2a, f0=f0, g=g, ws=ws, l=l, ex=ex, gi=gi, blk=blk):
                            c0, c1 = f0 * 128, (f0 + g) * 128
                            for nm, src in (("w1", w1a), ("w3", w3a)):
                                s = nxt("KS", KS)
                                ws[nm] = s
                                load_slot(s, (l, ex, gi, nm), blk, lambda e, s=s, src=src: e.dma_start(
                                    out=s[:, :, 0:g * 128], in_=src[:, c0:c1].rearrange("(k p) f -> p k f", p=128)), True)
                            s = nxt("FS", FS)
                            ws["w2"] = s
                            load_slot(s, (l, ex, gi, "w2"), blk, lambda e, s=s: e.dma_start(
                                out=s[:, 0:g, :], in_=w2a[c0:c1, :].rearrange("(j p) d -> p j d", p=128)), False)

                        def compF(ex=ex, gi=gi, g=g, ws=ws, gtf=gtf):
                            if ex is not None and gi == 0:
                                bA = nxt("A", PA)
                                P.op(pe, R(selE, GTT), R(bA), lambda e, bA=bA: e.matmul(bA[:, :], lhsT=selE[0:NE, ex, :], rhs=GTT[0:NE, :], start=True, stop=True))
                                evac_copy(gbc[:], gbc.r, bA[:, :], bA.r)
                                for k in range(8):
                                    P.op(dve, R(hT, gbc), R(hgT), lambda e, k=k: e.tensor_tensor(
                                        out=hgT[:, k, :], in0=hT[:, k, :], in1=gbc[:], op=ALU.mult))
                            h3 = hT if ex is None else hgT
                            gt_ = nxt("gT", gT)
                            for fi in range(g):
                                bA = nxt("A", PA)
                                proj(ws["w1"], fi * 128, 128, bA)
                                bB = nxt("B", PB)

                                def f3(e, fi=fi, bB=bB):
                                    ins = None
                                    for k in range(8):
                                        ins = e.matmul(bB[:, :], lhsT=ws["w3"][:, k, fi * 128:(fi + 1) * 128], rhs=h3[:, k, :], start=(k == 0), stop=(k == 7))
                                    return ins
                                P.op(pe, R(ws["w3"], h3), R(bB), f3)
                                s_ = nxt("sl", sl)
                                P.op(act, R(bA), R(s_), lambda e, bA=bA, s_=s_: e.activation(out=s_[:], in_=bA[:, :], func=AF.Silu))
                                P.op(dve, R(s_, bB), R(gt_), lambda e, fi=fi, bB=bB, s_=s_, gt_=gt_: e.tensor_tensor(
                                    out=gt_[:, fi, :], in0=s_[:], in1=bB[:, :], op=ALU.mult))
                            for dc in range(8):
                                bY = nxt("Y3", PY + [PM])

                                def f2(e, dc=dc, bY=bY, gt_=gt_):
                                    ins = None
                                    for fi in range(g):
                                        ins = e.matmul(bY[:, :], lhsT=ws["w2"][:, fi, dc * 128:(dc + 1) * 128], rhs=gt_[:, fi, :], start=(fi == 0), stop=(fi == g - 1))
                                    return ins
                                P.op(pe, R(ws["w2"], gt_), R(bY), f2)
                                resid(bY, dc, gtf)
                        add_step(ldF, compF)

                def compZ(l=l, blk=blk, last_l=last_l):
                    if not last_l:
                        P.dma(sp, xsr[blk], xT.r, lambda e: e.dma_start(out=xs_d[blk], in_=xT[:]))
                    else:
                        for c in range(8):
                            P.op(act, R(xT), R(hgT), lambda e, c=c: e.activation(out=hgT[:, c, :], in_=xT[:, c, :], func=AF.Square))

                        def f(e):
                            ins = None
                            for c in range(8):
                                ins = e.matmul(PM[:, :], lhsT=onesB[:], rhs=hgT[:, c, :], start=(c == 0), stop=(c == 7))
                            return ins
                        P.op(pe, R(onesB, hgT), R(PM), f)
                        P.op(act, R(PM), R(rstd), lambda e: e.activation(out=rstd[:], in_=PM[:, :], func=AF.Ln, scale=1.0 / D, bias=EPS))
                        P.op(act, R(rstd), R(rstd), lambda e: e.activation(out=rstd[:], in_=rstd[:], func=AF.Exp, scale=-0.5))
                        for c in range(8):
                            P.op(dve, R(xT, rstd, vec), R(xT), lambda e, c=c: e.scalar_tensor_tensor(
                                out=xT[:, c, :], in0=xT[:, c, :], scalar=vec[:, 8 + c:9 + c], in1=rstd[:], op0=ALU.mult, op1=ALU.mult))
                        for t in range(4):
                            xo = nxt("xin", xin)
                            for half in range(2):
                                bY = nxt("Y", PY)

                                def tr(e, t=t, half=half, bY=bY):
                                    ins = None
                                    for cc in range(4):
                                        c = half * 4 + cc
                                        ins = e.transpose(bY[:, cc * 128:(cc + 1) * 128], xT[:, c, t * 128:(t + 1) * 128], identF[:])
                                    return ins
                                P.op(pe, R(xT, identF), R(bY), tr)
                                evac_copy(xo[:, half * 512:(half + 1) * 512], xo.r, bY[:, :], bY.r, eng=(act if half == 0 else dve))
                            r0 = blk * NT + t * 128
                            P.dma(sp, outr, xo.r, lambda e, xo=xo, r0=r0: e.dma_start(out=out_d[r0:r0 + 128, :], in_=xo[:]))
                add_step(None, compZ)

        xsr = [P.res("xs%d" % b) for b in range(NB)]
        outr = P.res("out")
        if steps[0][0]:
            steps[0][0]()
        for i, (ld, comp) in enumerate(steps):
            if i + 1 < len(steps) and steps[i + 1][0]:
                steps[i + 1][0]()
            comp()
        P.wait_all(sp, [outr])
        sp.e.wait_ge(outr.dsem["hw"], outr.dcnt["hw"])
    return nc


def _col(v):
    v = np.asarray(v, np.float32).reshape(-1, 128)
    return v.T


def _pack_vecs(inp, b):
    cols = [_col(inp["c"][b]), _col(inp["final_g"])]
    for l in range(DEPTH):
        cols += [_col(inp["norm_mix_g"][l]), _col(inp["norm_ffn_g"][l])]
        cw = inp["rg_conv_w"][l]
        cols += [_col(cw[k]) for k in range(4)]
        cols += [_col(inp["rg_conv_b"][l]), _col(inp["rg_ba"][l]), _col(inp["rg_bx"][l]), _col(inp["rg_lambda"][l]),
                 _col(inp["gla_bg"][l]), _col(inp["gla_norm_g"][l]), _col(inp["ada_b"][l])]
    v = np.ascontiguousarray(np.concatenate(cols, axis=1), dtype=np.float32)
    assert v.shape == (128, NV), v.shape
    return v


_NC_CACHE = {}


def kernel(**inputs):
    inp = {k: np.asarray(v) for k, v in inputs.items()}
    if "nc" not in _NC_CACHE:
        _NC_CACHE["nc"] = build()
    nc = _NC_CACHE["nc"]
    shared = {k: np.ascontiguousarray(inp[k], dtype=np.float32) for k in
              ("ada_w", "w_in", "rg_wa", "rg_wx", "gla_wg2", "w_out", "ffn_w1", "ffn_w3", "ffn_w2",
               "router_w", "moe_w1", "moe_w3", "moe_w2")}
    B = inp["x"].shape[0]
    in_maps = []
    for b in range(B):
        m = dict(shared)
        m["x"] = np.ascontiguousarray(inp["x"][b], dtype=np.float32)
        m["vecs"] = _pack_vecs(inp, b)
        in_maps.append(m)
    res = run_bass_kernel_spmd(nc, in_maps, core_ids=list(range(B)))
    out = np.stack([np.asarray(res.results[b]["out"], dtype=np.float32) for b in range(B)], axis=0)
    return out
```

```python
import numpy as np
from contextlib import ExitStack
import concourse.bass as bass
import concourse.mybir as mybir
from concourse.bass_utils import run_bass_kernel_spmd

F32 = mybir.dt.float32
BF16 = mybir.dt.bfloat16
AF = mybir.ActivationFunctionType
ALU = mybir.AluOpType
AX = mybir.AxisListType

D = 1024
S = 4096
NT = 512
NB = S // NT
DFF = 2816
NE = 8
DIN = 2576
DEPTH = 4
EPS = 1e-6
LV = 99
NV = 16 + DEPTH * LV
GROUPS = [(0, 4), (4, 4), (8, 4), (12, 4), (16, 4), (20, 2)]


class Res:
    __slots__ = ("name", "w", "r", "dsem", "dcnt", "children", "parent")

    def __init__(self, name):
        self.name = name
        self.w = None
        self.r = {}
        self.dsem = {}
        self.dcnt = {}
        self.children = []
        self.parent = None


class Eng:
    def __init__(self, name, e, sem):
        self.name, self.e, self.sem, self.cnt, self.known = name, e, sem, 0, {}


class _FirstWait:
    def __init__(self, e, wait):
        self.e, self.wait, self.done = e, wait, wait is None

    def _w(self, ins):
        if not self.done:
            ins._wait_ge(self.wait[0], self.wait[1])
            self.done = True
        return ins

    def matmul(self, *a, **k):
        return self._w(self.e.matmul(*a, **k))

    def transpose(self, *a, **k):
        return self._w(self.e.transpose(*a, **k))


class Prog:
    def __init__(self, nc, es):
        self.nc, self.es = nc, es
        self.pe = Eng("pe", nc.tensor, es.enter_context(nc.semaphore("s_pe")))
        self.act = Eng("act", nc.scalar, es.enter_context(nc.semaphore("s_act")))
        self.dve = Eng("dve", nc.vector, es.enter_context(nc.semaphore("s_dve")))
        self.pool = Eng("pool", nc.gpsimd, es.enter_context(nc.semaphore("s_pool")))
        self.sp = Eng("sp", nc.sync, es.enter_context(nc.semaphore("s_sp")))
        self.nres = 0

    def res(self, name):
        return Res(name)

    def _need(self, eng, reads, writes):
        def expand(rs):
            out = []
            for r in rs:
                out.append(r)
                out.extend(r.children)
                if r.parent is not None:
                    out.append(r.parent)
            return out

        rd, wr = {}, {}

        def add(dct, tok):
            if tok is None:
                return
            s, v = tok
            if dct.get(id(s), (None, 0))[1] < v:
                dct[id(s)] = (s, v)

        for r in expand(reads):
            add(rd, r.w)
        for w in expand(writes):
            add(wr, w.w)
            for s, v in w.r.values():
                add(wr, (s, v))
        need_r, need_w = [], []
        for k, (s, v) in rd.items():
            if eng is self.pe and s is self.pe.sem:
                continue
            if eng.known.get(k, 0) < v:
                need_r.append((s, v))
        for k, (s, v) in wr.items():
            if eng is self.pe and s is self.pe.sem:
                continue
            if k in rd and rd[k][1] >= v:
                continue
            if eng.known.get(k, 0) < v:
                need_w.append((s, v))
        return need_r, need_w

    def _deps(self, eng, reads, writes):
        need_r, need_w = self._need(eng, reads, writes)
        for s, v in need_r + need_w:
            if eng.known.get(id(s), 0) < v:
                eng.e.wait_ge(s, v)
                eng.known[id(s)] = v

    def op(self, eng, reads, writes, fn):
        need_r, need_w = self._need(eng, reads, writes)
        embed = None
        if eng is self.pe:
            standalone = need_r + need_w[:-1]
            if need_w:
                embed = need_w[-1]
        else:
            allw = need_r + need_w
            standalone = allw[:-1]
            if allw:
                embed = allw[-1]
        for s, v in standalone:
            if eng.known.get(id(s), 0) < v:
                eng.e.wait_ge(s, v)
                eng.known[id(s)] = v
        if eng is self.pe:
            prox = _FirstWait(eng.e, embed)
            ins = fn(prox)
            if embed is not None and not prox.done:
                raise RuntimeError("embedded wait not consumed")
        else:
            ins = fn(eng.e)
            if embed is not None:
                ins._wait_ge(embed[0], embed[1])
        if embed is not None:
            eng.known[id(embed[0])] = max(eng.known.get(id(embed[0]), 0), embed[1])
        eng.cnt += 1
        ins.then_inc(eng.sem, 1)
        tok = (eng.sem, eng.cnt)
        for r in reads:
            r.r[id(eng.sem)] = tok
        for w in writes:
            w.w = tok
            w.r = {}
        return ins

    def dma(self, q, out_res, in_res, fn):
        self._deps(q, [in_res], [out_res])
        kq = "sw" if q is self.pool else "hw"
        if kq not in out_res.dsem:
            self.nres += 1
            out_res.dsem[kq] = self.es.enter_context(self.nc.semaphore("d%d" % self.nres))
            out_res.dcnt[kq] = 0
        ins = fn(q.e)
        out_res.dcnt[kq] += 16
        dsem = out_res.dsem[kq]
        ins.then_inc(dsem, 16)
        tok = (dsem, out_res.dcnt[kq])
        in_res.r[id(dsem)] = tok
        out_res.w = tok
        out_res.r = {}
        return ins

    def wait_all(self, q, ress):
        self._deps(q, ress, [])


def build(nlayers=DEPTH):
    nc = bass.Bass("TRN2", target_bir_lowering=False)

    def din(name, shape):
        return nc.dram_tensor(name, shape, F32, kind="ExternalInput").ap()

    x_d = din("x", [S, D])
    vec_d = din("vecs", [128, NV])
    ada_w_d = din("ada_w", [DEPTH, D, 6 * D])
    w_in_d = din("w_in", [DEPTH, D, DIN])
    wa_d = din("rg_wa", [DEPTH, 8, 64, 64])
    wx_d = din("rg_wx", [DEPTH, 8, 64, 64])
    wg2_d = din("gla_wg2", [DEPTH, 16, 256])
    w_out_d = din("w_out", [DEPTH, D, D])
    fw1_d = din("ffn_w1", [2, D, DFF])
    fw3_d = din("ffn_w3", [2, D, DFF])
    fw2_d = din("ffn_w2", [2, DFF, D])
    rw_d = din("router_w", [2, D, NE])
    mw1_d = din("moe_w1", [2, NE, D, DFF])
    mw3_d = din("moe_w3", [2, NE, D, DFF])
    mw2_d = din("moe_w2", [2, NE, DFF, D])
    out_d = nc.dram_tensor("out", [S, D], F32, kind="ExternalOutput").ap()
    xs_d = nc.dram_tensor("xs", [NB, 128, 8, NT], F32, kind="Internal").ap()
    wsc_l = [nc.dram_tensor("wsc%d" % l, [7 + (144 if l % 2 == 1 else 18), 128, 4096], BF16, kind="Internal").ap() for l in range(DEPTH)]

    with ExitStack() as es:
        P = Prog(nc, es)
        pe, act, dve, pool, sp = P.pe, P.act, P.dve, P.pool, P.sp

        class T:
            def __init__(self, name, shape, dt, psum=False):
                if psum:
                    self.t = es.enter_context(nc.psum_tensor(name, shape, dt))
                else:
                    self.t = es.enter_context(nc.sbuf_tensor(name, shape, dt))
                self.r = P.res(name)

            def __getitem__(self, k):
                return self.t[k]

        def sb(name, shape, dt=F32):
            return T(name, shape, dt)

        dummy = P.res("dram_in")

        identF = sb("identF", [128, 128])
        identB = sb("identB", [128, 128], BF16)
        onesB = sb("onesB", [128, 128], BF16)
        maskT = sb("maskT", [128, 128])
        rowm = sb("rowm", [128, 2])
        resetm = sb("resetm", [128, NT])
        selE = sb("selE", [8, NE, 128])
        vec = sb("vec", [128, NV])
        cact2 = sb("cact2", [128, 8, 2])
        modall = sb("modall", [128, DEPTH, 48])
        lvec = sb("lvec", [128, 40])
        WaBD = sb("WaBD", [128, 4, 128], BF16)
        WxBD = sb("WxBD", [128, 4, 128], BF16)
        wg2b = sb("wg2b", [16, 256], BF16)
        flw = sb("flw", [128, 8, 16], BF16)
        rwb = sb("rwb", [128, 8, NE], BF16)
        halo = sb("halo", [128, 4, 3])
        hst = sb("hst", [128, 4])
        Sst = [sb("Sst%d" % p, [128, 128]) for p in range(2)]
        xT = sb("xT", [128, 8, NT])
        xTc = [P.res("xTc%d" % c) for c in range(8)]
        for r_ in xTc:
            r_.parent = xT.r
        xT.r.children = list(xTc)
        tmpY = [sb("tmpY%d" % i, [128, NT]) for i in range(2)]
        hT = sb("hT", [128, 8, NT], BF16)
        hgT = sb("hgT", [128, 8, NT], BF16)
        rstd = sb("rstd", [128, NT])
        tmpx = [sb("tmpx%d" % i, [128, NT]) for i in range(1)]
        xin = [sb("xin%d" % i, [128, D]) for i in range(2)]
        KS = [sb("KS%d" % i, [128, 8, 512], BF16) for i in range(5)]
        FS = [sb("FS%d" % i, [128, 4, D], BF16) for i in range(3)]
        gT = [sb("gT%d" % i, [128, 4, NT], BF16) for i in range(2)]
        sl = [sb("sl%d" % i, [128, NT]) for i in range(4)]
        rgxh = sb("rgxh", [128, NT + 3])
        Ub = sb("Ub", [128, NT], BF16)
        mU, mR, mI, mA, mM, mGG, mG2 = [sb("m%s" % n, [128, NT]) for n in ("U", "R", "I", "A", "M", "GG", "G2")]
        mixo = sb("mixo", [128, 8, NT], BF16)
        flowT = sb("flowT", [16, NT], BF16)
        vtok = sb("vtok", [128, 4, 512], BF16)
        gsil = sb("gsil", [128, 4, NT])
        qT, kT, LF, Bc, EQ, EK = [sb("g%s" % n, [128, NT]) for n in ("q", "k", "LF", "Bc", "EQ", "EK")]
        qloc = sb("qloc", [128, NT], BF16)
        kl = [sb("kl%d" % i, [128, NT], BF16) for i in range(2)]
        klf = sb("klf", [128, NT], BF16)
        kltok = sb("kltok", [128, 4, 128], BF16)
        dsm = sb("dsm", [128, 6, 4])
        Sb = [sb("Sb%d" % i, [128, 128], BF16) for i in range(2)]
        scm = [sb("scm%d" % i, [128, 128], BF16) for i in range(2)]
        T1 = [sb("T1_%d" % i, [128, 128]) for i in range(2)]
        oT = sb("oT", [128, NT])
        sqo = sb("sqo", [128, NT], BF16)
        rso = sb("rso", [128, NT])
        LG = sb("LG", [128, 4, NE])
        L2 = sb("L2", [128, 4, NE])
        GT = sb("GT", [128, 4, NE])
        sm4 = sb("sm4", [128, 4, 4])
        GTT = sb("GTT", [8, NT])
        gbc = sb("gbc", [128, NT], BF16)
        PA = [T("psA%d" % i, [128, 512], F32, psum=True) for i in range(2)]
        PB = [T("psB%d" % i, [128, 512], F32, psum=True) for i in range(2)]
        PY = [T("psY%d" % i, [128, 512], F32, psum=True) for i in range(2)]
        PM = T("psM", [128, 512], F32, psum=True)
        PT = T("psT", [128, 1024], BF16, psum=True)
        ctr = {"A": 0, "B": 0, "Y": 0, "KS": 0, "FS": 0, "gT": 0, "sl": 0, "tmpx": 0, "xin": 0, "tmpY": 0, "Y3": 0}

        def nxt(kind, arr):
            i = ctr[kind]
            ctr[kind] = i + 1
            return arr[i % len(arr)]

        def R(*ts):
            return [t.r for t in ts]

        imgs = {}
        wbres = [P.res("wb%d" % i) for i in range(8)]
        wbctr = [0]

        def load_slot(slot, key, blk, src_fn, kmajor):
            pat = "p (k f) -> p k f" if kmajor else "p (j d) -> p j d"
            kw = {"k": 8} if kmajor else {"j": 4}
            wsc_d = wsc_l[key[0]]
            if blk == 0:
                idx = sum(1 for kk in imgs if kk[0] == key[0])
                P.dma(pool, slot.r, dummy, src_fn)
                wr = wbres[wbctr[0] % 8]
                wbctr[0] += 1
                P.dma(sp, wr, slot.r, lambda e: e.dma_start(out=wsc_d[idx].rearrange(pat, **kw), in_=slot[:]))
                ir = P.res("img%d" % idx)
                ir.w = wr.w
                imgs[key] = (idx, ir)
            else:
                idx, ir = imgs[key]
                P.dma(sp, slot.r, ir, lambda e: e.dma_start(out=slot[:], in_=wsc_d[idx].rearrange(pat, **kw)))

        P.op(pool, [], R(identF), lambda e: e.memset(identF[:], 1.0))
        P.op(pool, R(identF), R(identF), lambda e: e.affine_select(out=identF[:], in_=identF[:], pattern=[[1, 128]], compare_op=ALU.is_equal, fill=0.0, base=0, channel_multiplier=-1))
        P.op(pool, [], R(maskT), lambda e: e.memset(maskT[:], 1.0))
        P.op(pool, R(maskT), R(maskT), lambda e: e.affine_select(out=maskT[:], in_=maskT[:], pattern=[[1, 128]], compare_op=ALU.is_ge, fill=0.0, base=0, channel_multiplier=-1))
        P.op(pool, [], R(rowm), lambda e: e.memset(rowm[:], 1.0))
        P.op(pool, R(rowm), R(rowm), lambda e: e.affine_select(out=rowm[:, 0:1], in_=rowm[:, 0:1], pattern=[[0, 1]], compare_op=ALU.is_ge, fill=0.0, base=63, channel_multiplier=-1))
        P.op(pool, R(rowm), R(rowm), lambda e: e.affine_select(out=rowm[:, 1:2], in_=rowm[:, 1:2], pattern=[[0, 1]], compare_op=ALU.is_ge, fill=0.0, base=-64, channel_multiplier=1))
        P.op(pool, [], R(selE), lambda e: e.memset(selE[:], 1.0))
        P.op(pool, R(selE), R(selE), lambda e: e.affine_select(out=selE[:], in_=selE[:], pattern=[[-1, NE], [0, 128]], compare_op=ALU.is_equal, fill=0.0, base=0, channel_multiplier=1))
        P.op(pool, [], R(onesB), lambda e: e.memset(onesB[:], 1.0))
        P.op(pool, [], R(resetm), lambda e: e.memset(resetm[:], 1.0))
        for t in range(NT // 128):
            P.op(pool, R(resetm), R(resetm), lambda e, t=t: e.memset(resetm[:, t * 128:t * 128 + 1], 0.0))
        P.op(pool, R(identF), R(identB), lambda e: e.tensor_copy(out=identB[:], in_=identF[:]))

        P.dma(sp, vec.r, dummy, lambda e: e.dma_start(out=vec[:], in_=vec_d[:, :]))
        P.op(act, R(vec), R(cact2), lambda e: e.activation(out=cact2[:, :, 0], in_=vec[:, 0:8], func=AF.Silu))
        P.op(act, R(vec), R(cact2), lambda e: e.activation(out=cact2[:, :, 1], in_=vec[:, 0:8], func=AF.Silu))

        for l in range(nlayers):
            vb = 16 + l * LV
            for pc in range(12):
                P.dma(sp, xT.r, dummy, lambda e, l=l, pc=pc: e.dma_start(
                    out=xT[:], in_=ada_w_d[l, :, pc * 512:(pc + 1) * 512].rearrange("(k p) f -> p k f", p=128)))

                def mm(e, pc=pc):
                    ins = None
                    for jj in range(4):
                        j = pc * 4 + jj
                        for k in range(8):
                            ins = e.matmul(PM[:, 2 * j:2 * j + 2], lhsT=xT[:, k, jj * 128:(jj + 1) * 128], rhs=cact2[:, k, :], start=(k == 0), stop=(k == 7))
                    return ins
                P.op(pe, R(xT, cact2), R(PM), mm)
            pmv = PM[:, 0:96].rearrange("p (j t) -> p j t", t=2)
            P.op(dve, R(PM, vec), R(modall), lambda e, l=l, vb=vb, pmv=pmv: e.tensor_tensor(
                out=modall[:, l, :], in0=pmv[:, :, 0], in1=vec[:, vb + 51:vb + 99], op=ALU.add))

        def evac_copy(dst_ap, dst_res, src_ap, src_res, eng=None):
            eng = eng or act
            if eng is act:
                P.op(act, [src_res], [dst_res], lambda e: e.copy(out=dst_ap, in_=src_ap))
            else:
                P.op(dve, [src_res], [dst_res], lambda e: e.tensor_copy(out=dst_ap, in_=src_ap))

        def proj(wslot, c0, width, bank, rhs_of_k=None, m_out=128):
            def f(e):
                ins = None
                for k in range(8):
                    ins = e.matmul(bank[0:width, :], lhsT=wslot[:, k, c0:c0 + width], rhs=hT[:, k, :], start=(k == 0), stop=(k == 7))
                return ins
            P.op(pe, R(wslot, hT), R(bank), f)

        def norm_mod(s1_ap, sh_ap):
            for c in range(8):
                P.op(act, R(xT), R(hgT), lambda e, c=c: e.activation(out=hgT[:, c, :], in_=xT[:, c, :], func=AF.Square))

            def f(e):
                ins = None
                for c in range(8):
                    ins = e.matmul(PM[:, :], lhsT=onesB[:], rhs=hgT[:, c, :], start=(c == 0), stop=(c == 7))
                return ins
            P.op(pe, R(onesB, hgT), R(PM), f)
            P.op(act, R(PM), R(rstd), lambda e: e.activation(out=rstd[:], in_=PM[:, :], func=AF.Ln, scale=1.0 / D, bias=EPS))
            P.op(act, R(rstd), R(rstd), lambda e: e.activation(out=rstd[:], in_=rstd[:], func=AF.Exp, scale=-0.5))
            for c in range(8):
                tx = nxt("tmpx", tmpx + tmpY)
                P.op(dve, R(xT, rstd, lvec), R(tx), lambda e, c=c, tx=tx: e.scalar_tensor_tensor(
                    out=tx[:], in0=xT[:, c, :], scalar=s1_ap[:, c:c + 1], in1=rstd[:], op0=ALU.mult, op1=ALU.mult))
                P.op(act, R(tx, modall), R(hT), lambda e, c=c, tx=tx: e.activation(
                    out=hT[:, c, :], in_=tx[:], func=AF.Identity, bias=sh_ap[:, c:c + 1]))

        def resid(bank, dc, gt_ap):
            if dc % 2 == 0:
                P.op(dve, [bank.r, modall.r, xTc[dc]], [xTc[dc]], lambda e: e.scalar_tensor_tensor(
                    out=xT[:, dc, :], in0=bank[:, :], scalar=gt_ap[:, dc:dc + 1], in1=xT[:, dc, :], op0=ALU.mult, op1=ALU.add))
            else:
                ty = nxt("tmpY", tmpY)
                P.op(act, [bank.r, modall.r], [ty.r], lambda e: e.activation(out=ty[:], in_=bank[:, :], func=AF.Identity, scale=gt_ap[:, dc:dc + 1]))
                P.op(pool, [ty.r, xTc[dc]], [xTc[dc]], lambda e: e.tensor_tensor(out=xT[:, dc, :], in0=xT[:, dc, :], in1=ty[:], op=ALU.add))

        steps = []

        def add_step(loads, compute):
            steps.append((loads, compute))

        for l in range(nlayers):
            vb = 16 + l * LV
            moe = (l % 2 == 1)
            jl = l // 2
            mod = lambda j0, l=l: modall[:, l, j0:j0 + 8]
            shm, gtm, shf, gtf = mod(0), mod(16), mod(24), mod(40)
            s1m, s1f, spv, sp2v = lvec[:, 0:8], lvec[:, 8:16], lvec[:, 16:20], lvec[:, 20:24]
            cw = lambda k, c, vb=vb: vec[:, vb + 16 + k * 4 + c:vb + 16 + k * 4 + c + 1]
            cb = lambda c, vb=vb: vec[:, vb + 32 + c:vb + 33 + c]
            ba = lambda c, vb=vb: vec[:, vb + 36 + c:vb + 37 + c]
            bx = lambda c, vb=vb: vec[:, vb + 40 + c:vb + 41 + c]
            bg = lambda p, vb=vb: vec[:, vb + 48 + p:vb + 49 + p]
            glag = vec[:, vb + 50:vb + 51]

            def layer_setup(l=l, vb=vb, moe=moe, jl=jl):
                P.op(dve, R(modall, vec), R(lvec), lambda e: e.scalar_tensor_tensor(
                    out=lvec[:, 0:8], in0=modall[:, l, 8:16], scalar=1.0, in1=vec[:, vb:vb + 8], op0=ALU.add, op1=ALU.mult))
                P.op(dve, R(modall, vec), R(lvec), lambda e: e.scalar_tensor_tensor(
                    out=lvec[:, 8:16], in0=modall[:, l, 32:40], scalar=1.0, in1=vec[:, vb + 8:vb + 16], op0=ALU.add, op1=ALU.mult))
                P.op(act, R(vec), R(lvec), lambda e: e.activation(out=lvec[:, 24:28], in_=vec[:, vb + 44:vb + 48], func=AF.Exp, scale=-1.0))
                P.op(act, R(lvec), R(lvec), lambda e: e.activation(out=lvec[:, 24:28], in_=lvec[:, 24:28], func=AF.Ln, bias=1.0))
                P.op(dve, R(lvec), R(lvec), lambda e: e.tensor_scalar(out=lvec[:, 16:20], in0=lvec[:, 24:28], scalar1=-8.0, scalar2=None, op0=ALU.mult))
                P.op(dve, R(lvec), R(lvec), lambda e: e.tensor_scalar(out=lvec[:, 20:24], in0=lvec[:, 24:28], scalar1=-16.0, scalar2=None, op0=ALU.mult))
                P.op(dve, [], R(WaBD), lambda e: e.memset(WaBD[:], 0.0))
                P.op(dve, [], R(WxBD), lambda e: e.memset(WxBD[:], 0.0))
                for n in range(8):
                    pb = (n % 2) * 64
                    P.dma(pool, WaBD.r, dummy, lambda e, n=n, pb=pb: e.dma_start(out=WaBD[pb:pb + 64, n // 2, pb:pb + 64], in_=wa_d[l, n, :, :]))
                    P.dma(pool, WxBD.r, dummy, lambda e, n=n, pb=pb: e.dma_start(out=WxBD[pb:pb + 64, n // 2, pb:pb + 64], in_=wx_d[l, n, :, :]))
                P.dma(pool, wg2b.r, dummy, lambda e: e.dma_start(out=wg2b[:], in_=wg2_d[l, :, :]))
                P.dma(pool, flw.r, dummy, lambda e: e.dma_start(out=flw[:], in_=w_in_d[l, :, 2560:2576].rearrange("(k p) f -> p k f", p=128)))
                if moe:
                    P.dma(pool, rwb.r, dummy, lambda e: e.dma_start(out=rwb[:], in_=rw_d[jl, :, :].rearrange("(k p) f -> p k f", p=128)))
                P.op(dve, [], R(halo), lambda e: e.memset(halo[:], 0.0))
                P.op(dve, [], R(hst), lambda e: e.memset(hst[:], 0.0))
                for p in range(2):
                    P.op(dve, [], R(Sst[p]), lambda e, p=p: e.memset(Sst[p][:], 0.0))

            for blk in range(NB):
                first_l, last_l = (l == 0), (l == nlayers - 1)
                slots = {}

                def ld_in(names, l=l, slots=slots, blk=blk):
                    def f():
                        for nm, c0 in names:
                            s = nxt("KS", KS)
                            slots[nm] = s
                            load_slot(s, (l, "in", nm), blk, lambda e, s=s, c0=c0: e.dma_start(
                                out=s[:], in_=w_in_d[l, :, c0:c0 + 512].rearrange("(k p) f -> p k f", p=128)), True)
                    return f

                def compA(l=l, blk=blk, slots=slots, first_l=first_l, shm=shm, s1m=s1m, cw=cw, cb=cb, ba=ba, bx=bx, spv=spv, sp2v=sp2v, layer_setup=layer_setup):
                    if blk == 0:
                        layer_setup()
                    if first_l:
                        for t in range(4):
                            xi = nxt("xin", xin)
                            r0 = blk * NT + t * 128
                            P.dma(sp, xi.r, dummy, lambda e, xi=xi, r0=r0: e.dma_start(out=xi[:], in_=x_d[r0:r0 + 128, :]))
                            for half in range(2):
                                bT = nxt("Y", PY)

                                def tr(e, xi=xi, half=half, bT=bT):
                                    ins = None
                                    for cc in range(4):
                                        c = half * 4 + cc
                                        ins = e.transpose(bT[:, cc * 128:(cc + 1) * 128], xi[:, c * 128:(c + 1) * 128], identF[:])
                                    return ins
                                P.op(pe, R(xi, identF), R(bT), tr)
                                if half == 0:
                                    P.op(dve, R(bT), R(xT), lambda e, t=t, half=half, bT=bT: e.tensor_copy(
                                        out=xT[:, half * 4:half * 4 + 4, t * 128:(t + 1) * 128], in_=bT[:, :].rearrange("p (c t) -> p c t", t=128)))
                                else:
                                    P.op(act, R(bT), R(xT), lambda e, t=t, half=half, bT=bT: e.copy(
                                        out=xT[:, half * 4:half * 4 + 4, t * 128:(t + 1) * 128], in_=bT[:, :].rearrange("p (c t) -> p c t", t=128)))
                    else:
                        P.dma(sp, xT.r, xsr[blk], lambda e: e.dma_start(out=xT[:], in_=xs_d[blk]))
                    norm_mod(s1m, shm)
                    P0, P1 = slots["rgx"], slots["rgg"]
                    for c in range(4):
                        bA = PA[0]
                        proj(P0, c * 128, 128, bA)
                        yield
                        P.op(dve, R(halo), R(rgxh), lambda e, c=c: e.tensor_copy(out=rgxh[:, 0:3], in_=halo[:, c, :]))
                        yield
                        evac_copy(rgxh[:, 3:NT + 3], rgxh.r, bA[:, :], bA.r)
                        yield
                        bB = PB[0]
                        proj(P1, c * 128, 128, bB)
                        yield
                        evac_copy(mGG[:], mGG.r, bB[:, :], bB.r)
                        yield
                        P.op(dve, R(rgxh, vec), R(mU), lambda e, c=c: e.tensor_scalar(
                            out=mU[:], in0=rgxh[:, 3:NT + 3], scalar1=cw(3, c), scalar2=cb(c), op0=ALU.mult, op1=ALU.add))
                        yield
                        for k in range(3):
                            P.op(dve, R(rgxh, vec, mU), R(mU), lambda e, c=c, k=k: e.scalar_tensor_tensor(
                                out=mU[:], in0=rgxh[:, k:k + NT], scalar=cw(k, c), in1=mU[:], op0=ALU.mult, op1=ALU.add))
                            yield
                        P.op(dve, R(rgxh), R(halo), lambda e, c=c: e.tensor_copy(out=halo[:, c, :], in_=rgxh[:, NT:NT + 3]))
                        yield
                        P.op(act, R(mU), R(Ub), lambda e: e.copy(out=Ub[:], in_=mU[:]))
                        yield
                        bA = PA[0]
                        P.op(pe, R(WaBD, Ub), R(bA), lambda e, c=c, bA=bA: e.matmul(bA[:, :], lhsT=WaBD[:, c, :], rhs=Ub[:], start=True, stop=True))
                        yield
                        bB = PB[0]
                        P.op(pe, R(WxBD, Ub), R(bB), lambda e, c=c, bB=bB: e.matmul(bB[:, :], lhsT=WxBD[:, c, :], rhs=Ub[:], start=True, stop=True))
                        yield
                        P.op(act, R(bA, vec), R(mR), lambda e, c=c, bA=bA: e.activation(out=mR[:], in_=bA[:, :], func=AF.Sigmoid, bias=ba(c)))
                        yield
                        P.op(act, R(bB, vec), R(mI), lambda e, c=c, bB=bB: e.activation(out=mI[:], in_=bB[:, :], func=AF.Sigmoid, bias=bx(c)))
                        yield
                        P.op(act, R(mR, lvec), R(mA), lambda e, c=c: e.activation(out=mA[:], in_=mR[:], func=AF.Exp, scale=spv[:, c:c + 1]))
                        yield
                        P.op(act, R(mR, lvec), R(mM), lambda e, c=c: e.activation(out=mM[:], in_=mR[:], func=AF.Exp, scale=sp2v[:, c:c + 1]))
                        yield
                        P.op(act, R(mM), R(mM), lambda e: e.activation(out=mM[:], in_=mM[:], func=AF.Sqrt, scale=-1.0, bias=1.0))
                        yield
                        if blk == 0:
                            P.op(dve, [], R(mM), lambda e: e.memset(mM[:, 0:1], 1.0))
                            yield
                        P.op(dve, R(mM, mI), R(mM), lambda e: e.tensor_tensor(out=mM[:], in0=mM[:], in1=mI[:], op=ALU.mult))
                        yield
                        P.op(dve, R(mM, mU), R(mM), lambda e: e.tensor_tensor(out=mM[:], in0=mM[:], in1=mU[:], op=ALU.mult))
                        yield
                        P.op(dve, R(mA, mM, hst), R(mR), lambda e, c=c: e.tensor_tensor_scan(
                            out=mR[:], data0=mA[:], data1=mM[:], initial=hst[:, c:c + 1], op0=ALU.mult, op1=ALU.add))
                        yield
                        P.op(dve, R(mR), R(hst), lambda e, c=c: e.tensor_copy(out=hst[:, c:c + 1], in_=mR[:, NT - 1:NT]))
                        yield
                        P.op(act, R(mGG), R(mG2), lambda e: e.activation(out=mG2[:], in_=mGG[:], func=AF.Square))
                        yield
                        P.op(dve, R(mG2), R(mG2), lambda e: e.tensor_scalar(out=mG2[:], in0=mG2[:], scalar1=0.044715, scalar2=1.0, op0=ALU.mult, op1=ALU.add))
                        yield
                        P.op(dve, R(mG2, mGG), R(mG2), lambda e: e.tensor_tensor(out=mG2[:], in0=mG2[:], in1=mGG[:], op=ALU.mult))
                        yield
                        P.op(act, R(mG2), R(mG2), lambda e: e.activation(out=mG2[:], in_=mG2[:], func=AF.Sigmoid, scale=1.5957691216057308))
                        yield
                        P.op(dve, R(mG2, mGG), R(mG2), lambda e: e.tensor_tensor(out=mG2[:], in0=mG2[:], in1=mGG[:], op=ALU.mult))
                        yield
                        P.op(dve, R(mR, mG2), R(mixo), lambda e, c=c: e.tensor_tensor(out=mixo[:, c, :], in0=mR[:], in1=mG2[:], op=ALU.mult))
                        yield

                def compB(l=l, blk=blk, slots=slots, bg=bg, glag=glag):
                    P2, P3, P4 = slots["qk"], slots["v"], slots["g"]
                    bA = PA[1]

                    def ff(e, bA=bA):
                        ins = None
                        for k in range(8):
                            ins = e.matmul(bA[0:16, :], lhsT=flw[:, k, :], rhs=hT[:, k, :], start=(k == 0), stop=(k == 7))
                        return ins
                    P.op(pe, R(flw, hT), R(bA), ff)
                    yield
                    evac_copy(flowT[:], flowT.r, bA[0:16, :], bA.r)
                    yield
                    for t in range(4):
                        bB = PB[1]

                        def fv(e, t=t, bB=bB):
                            ins = None
                            for k in range(8):
                                ins = e.matmul(bB[:, :], lhsT=hT[:, k, t * 128:(t + 1) * 128], rhs=P3[:, k, :], start=(k == 0), stop=(k == 7))
                            return ins
                        P.op(pe, R(P3, hT), R(bB), fv)
                        yield
                        evac_copy(vtok[:, t, :], vtok.r, bB[:, :], bB.r, eng=(act if t % 2 == 0 else dve))
                        yield
                    for hd in range(4):
                        bA = PA[1]
                        proj(P4, hd * 128, 128, bA)
                        yield
                        P.op(act, R(bA), R(gsil), lambda e, hd=hd, bA=bA: e.activation(out=gsil[:, hd, :], in_=bA[:, :], func=AF.Silu))
                        yield
                    for p in range(2):
                        bA = PA[1]
                        proj(P2, p * 128, 128, bA)
                        yield
                        evac_copy(qT[:], qT.r, bA[:, :], bA.r)
                        yield
                        bB = PB[1]
                        proj(P2, 256 + p * 128, 128, bB)
                        yield
                        evac_copy(kT[:], kT.r, bB[:, :], bB.r, eng=dve)
                        yield
                        bA = PA[1]
                        P.op(pe, R(wg2b, flowT), R(bA), lambda e, p=p, bA=bA: e.matmul(
                            bA[:, :], lhsT=wg2b[0:16, p * 128:(p + 1) * 128], rhs=flowT[0:16, :], start=True, stop=True))
                        yield
                        P.op(act, R(bA, vec), R(LF), lambda e, p=p, bA=bA: e.activation(out=LF[:], in_=bA[:, :], func=AF.Sigmoid, bias=bg(p)))
                        yield
                        P.op(act, R(LF), R(LF), lambda e: e.activation(out=LF[:], in_=LF[:], func=AF.Ln))
                        yield
                        P.op(dve, R(resetm, LF), R(Bc), lambda e: e.tensor_tensor_scan(
                            out=Bc[:], data0=resetm[:], data1=LF[:], initial=0.0, op0=ALU.mult, op1=ALU.add))
                        yield
                        Bv = Bc[:].rearrange("p (c t) -> p c t", t=128)
                        P.op(dve, R(Bc), R(EQ), lambda e, Bv=Bv: e.tensor_tensor(
                            out=EQ[:].rearrange("p (c t) -> p c t", t=128), in0=Bv, in1=Bv[:, :, 63:64].to_broadcast([128, 4, 128]), op=ALU.subtract))
                        yield
                        P.op(act, R(EQ), R(EK), lambda e: e.activation(out=EK[:], in_=EQ[:], func=AF.Exp, scale=-1.0 / 16))
                        yield
                        P.op(act, R(EQ), R(EQ), lambda e: e.activation(out=EQ[:], in_=EQ[:], func=AF.Exp, scale=1.0 / 16))
                        yield
                        P.op(dve, R(qT, EQ), R(qloc), lambda e: e.scalar_tensor_tensor(
                            out=qloc[:], in0=qT[:], scalar=0.125, in1=EQ[:], op0=ALU.mult, op1=ALU.mult))
                        yield
                        for hp in range(2):
                            P.op(dve, R(kT, EK, rowm), R(kl[hp]), lambda e, hp=hp: e.scalar_tensor_tensor(
                                out=kl[hp][:], in0=kT[:], scalar=rowm[:, hp:hp + 1], in1=EK[:], op0=ALU.mult, op1=ALU.mult))
                            yield
                        P.op(dve, R(kT, EK), R(klf), lambda e: e.tensor_tensor(out=klf[:], in0=kT[:], in1=EK[:], op=ALU.mult))
                        yield
                        P.op(act, R(Bc), R(dsm), lambda e, Bv=Bv: e.activation(out=dsm[:, 0, :], in_=Bv[:, :, 127], func=AF.Exp, scale=1.0 / 16))
                        yield
                        P.op(act, R(Bc), R(dsm), lambda e, Bv=Bv: e.activation(out=dsm[:, 1, :], in_=Bv[:, :, 63], func=AF.Exp, scale=1.0 / 16))
                        yield
                        P.op(dve, R(Bc), R(dsm), lambda e, Bv=Bv: e.tensor_tensor(out=dsm[:, 5, :], in0=Bv[:, :, 127], in1=Bv[:, :, 63], op=ALU.subtract))
                        yield
                        P.op(act, R(dsm), R(dsm), lambda e: e.activation(out=dsm[:, 2, :], in_=dsm[:, 5, :], func=AF.Exp, scale=1.0 / 16))
                        yield
                        for hp in range(2):
                            P.op(dve, R(dsm, rowm), R(dsm), lambda e, hp=hp: e.tensor_scalar(
                                out=dsm[:, 3 + hp, :], in0=dsm[:, 2, :], scalar1=rowm[:, hp:hp + 1], scalar2=None, op0=ALU.mult))
                            yield
                        def ftr(e):
                            ins = None
                            for t in range(4):
                                ins = e.transpose(PT[:, t * 128:(t + 1) * 128], klf[:, t * 128:(t + 1) * 128], identB[:])
                            return ins
                        P.op(pe, R(klf, identB), R(PT), ftr)
                        yield
                        P.op(act, R(PT), R(kltok), lambda e: e.copy(out=kltok[:].rearrange("p c t -> p (c t)"), in_=PT[:, 0:512]))
                        yield
                        bO = [nxt("Y", PY), nxt("Y", PY)]
                        S_ = Sst[p]
                        for t in range(4):
                            ts = slice(t * 128, (t + 1) * 128)
                            bU = PA[1]

                            def fu(e, t=t, bU=bU, p=p):
                                e.matmul(bU[:, 0:128], lhsT=kltok[:, t, :], rhs=vtok[:, t, (2 * p) * 128:(2 * p + 1) * 128], start=True, stop=True)
                                return e.matmul(bU[:, 128:256], lhsT=kltok[:, t, :], rhs=vtok[:, t, (2 * p + 1) * 128:(2 * p + 2) * 128], start=True, stop=True)
                            P.op(pe, R(kltok, vtok), R(bU), fu)
                            yield
                            bS = PB[1]

                            def fs(e, ts=ts, bS=bS):
                                e.matmul(bS[:, 0:128], lhsT=kl[0][:, ts], rhs=qloc[:, ts], start=True, stop=True)
                                return e.matmul(bS[:, 128:256], lhsT=kl[1][:, ts], rhs=qloc[:, ts], start=True, stop=True)
                            P.op(pe, R(kl[0], kl[1], qloc), R(bS), fs)
                            yield
                            for hp in range(2):
                                P.op(dve, R(bS, maskT), R(scm[hp]), lambda e, hp=hp, bS=bS: e.tensor_tensor(
                                    out=scm[hp][:], in0=bS[:, hp * 128:(hp + 1) * 128], in1=maskT[:], op=ALU.mult))
                                yield
                                P.op(dve, R(S_, dsm, rowm), R(Sb[hp]), lambda e, hp=hp, t=t, S_=S_: e.tensor_scalar(
                                    out=Sb[hp][:], in0=S_[:], scalar1=dsm[:, 1, t:t + 1], scalar2=rowm[:, hp:hp + 1], op0=ALU.mult, op1=ALU.mult))
                                yield
                            for hp in range(2):
                                h = 2 * p + hp

                                def fo(e, hp=hp, h=h, t=t, ts=ts):
                                    e.matmul(bO[hp][:, ts], lhsT=vtok[:, t, h * 128:(h + 1) * 128], rhs=scm[hp][:], start=True, stop=False)
                                    return e.matmul(bO[hp][:, ts], lhsT=Sb[hp][:], rhs=qloc[:, ts], start=False, stop=True)
                                P.op(pe, R(vtok, scm[hp], Sb[hp], qloc), R(bO[hp]), fo)
                                yield
                            P.op(dve, R(bU, dsm), R(T1[0]), lambda e, t=t, bU=bU: e.tensor_scalar(
                                out=T1[0][:], in0=bU[:, 0:128], scalar1=dsm[:, 3, t:t + 1], scalar2=None, op0=ALU.mult))
                            yield
                            P.op(dve, R(bU, dsm), R(T1[1]), lambda e, t=t, bU=bU: e.tensor_scalar(
                                out=T1[1][:], in0=bU[:, 128:256], scalar1=dsm[:, 4, t:t + 1], scalar2=None, op0=ALU.mult))
                            yield
                            P.op(dve, R(S_, dsm, T1[0]), R(S_), lambda e, t=t, S_=S_: e.scalar_tensor_tensor(
                                out=S_[:], in0=S_[:], scalar=dsm[:, 0, t:t + 1], in1=T1[0][:], op0=ALU.mult, op1=ALU.add))
                            yield
                            P.op(dve, R(S_, T1[1]), R(S_), lambda e, S_=S_: e.tensor_tensor(out=S_[:], in0=S_[:], in1=T1[1][:], op=ALU.add))
                            yield
                        for hp in range(2):
                            h = 2 * p + hp
                            b_ = bO[hp]
                            evac_copy(oT[:], oT.r, b_[:, :], b_.r)
                            yield
                            P.op(act, R(b_), R(sqo), lambda e, b_=b_: e.activation(out=sqo[:], in_=b_[:, :], func=AF.Square))
                            yield
                            P.op(pe, R(onesB, sqo), R(PM), lambda e: e.matmul(PM[:, :], lhsT=onesB[:], rhs=sqo[:], start=True, stop=True))
                            yield
                            P.op(act, R(PM), R(rso), lambda e: e.activation(out=rso[:], in_=PM[:, :], func=AF.Ln, scale=1.0 / 128, bias=EPS))
                            yield
                            P.op(act, R(rso), R(rso), lambda e: e.activation(out=rso[:], in_=rso[:], func=AF.Exp, scale=-0.5))
                            yield
                            P.op(dve, R(oT, rso), R(oT), lambda e: e.tensor_tensor(out=oT[:], in0=oT[:], in1=rso[:], op=ALU.mult))
                            yield
                            P.op(dve, R(oT, vec, gsil), R(mixo), lambda e, h=h: e.scalar_tensor_tensor(
                                out=mixo[:, 4 + h, :], in0=oT[:], scalar=glag, in1=gsil[:, h, :], op0=ALU.mult, op1=ALU.mult))
                            yield
                def compAB(compA=compA, compB=compB):
                    gens = [compA(), compB()]
                    while gens:
                        for g_ in list(gens):
                            try:
                                next(g_)
                            except StopIteration:
                                gens.remove(g_)
                add_step(ld_in([("rgx", 0), ("rgg", 512), ("qk", 1024), ("v", 1536), ("g", 2048)]), compAB)

                wo = {}

                def ldC(l=l, wo=wo, blk=blk):
                    for i in range(2):
                        s = nxt("FS", FS)
                        wo[i] = s
                        load_slot(s, (l, "out", i), blk, lambda e, s=s, i=i: e.dma_start(
                            out=s[:], in_=w_out_d[l, i * 512:(i + 1) * 512, :].rearrange("(j p) d -> p j d", p=128)), False)

                def compC(l=l, wo=wo, gtm=gtm, s1f=s1f, shf=shf, moe=moe):
                    for dc in range(8):
                        bY = nxt("Y", PY)

                        def f(e, dc=dc, bY=bY):
                            ins = None
                            for m in range(8):
                                ins = e.matmul(bY[:, :], lhsT=wo[m // 4][:, m % 4, dc * 128:(dc + 1) * 128], rhs=mixo[:, m, :], start=(m == 0), stop=(m == 7))
                            return ins
                        P.op(pe, R(wo[0], wo[1], mixo), R(bY), f)
                        resid(bY, dc, gtm)
                    norm_mod(s1f, shf)
                    if moe:
                        def fr(e):
                            ins = None
                            for t in range(4):
                                for k in range(8):
                                    ins = e.matmul(PM[:, t * NE:(t + 1) * NE], lhsT=hT[:, k, t * 128:(t + 1) * 128], rhs=rwb[:, k, :], start=(k == 0), stop=(k == 7))
                            return ins
                        P.op(pe, R(hT, rwb), R(PM), fr)
                        P.op(dve, R(PM), R(LG), lambda e: e.tensor_copy(out=LG[:].rearrange("p c t -> p (c t)"), in_=PM[:, 0:4 * NE]))
                        bc = lambda ap: ap.to_broadcast([128, 4, NE])
                        P.op(dve, R(LG), R(sm4), lambda e: e.tensor_reduce(out=sm4[:, :, 0:1], in_=LG[:], axis=AX.X, op=ALU.max))
                        P.op(dve, R(LG, sm4), R(L2), lambda e: e.tensor_tensor(out=L2[:], in0=LG[:], in1=bc(sm4[:, :, 0:1]), op=ALU.is_equal))
                        P.op(dve, R(L2, LG), R(L2), lambda e: e.scalar_tensor_tensor(out=L2[:], in0=L2[:], scalar=-1e30, in1=LG[:], op0=ALU.mult, op1=ALU.add))
                        P.op(dve, R(L2), R(sm4), lambda e: e.tensor_reduce(out=sm4[:, :, 1:2], in_=L2[:], axis=AX.X, op=ALU.max))
                        P.op(dve, R(LG, sm4), R(L2), lambda e: e.tensor_tensor(out=L2[:], in0=LG[:], in1=bc(sm4[:, :, 1:2]), op=ALU.is_ge))
                        P.op(dve, R(LG, sm4), R(GT), lambda e: e.tensor_tensor(out=GT[:], in0=LG[:], in1=bc(sm4[:, :, 0:1]), op=ALU.subtract))
                        P.op(act, R(GT), R(GT), lambda e: e.activation(out=GT[:], in_=GT[:], func=AF.Exp))
                        P.op(dve, R(GT, L2), R(GT), lambda e: e.tensor_tensor(out=GT[:], in0=GT[:], in1=L2[:], op=ALU.mult))
                        P.op(dve, R(GT), R(sm4), lambda e: e.tensor_reduce(out=sm4[:, :, 2:3], in_=GT[:], axis=AX.X, op=ALU.add))
                        P.op(dve, R(sm4), R(sm4), lambda e: e.reciprocal(out=sm4[:, :, 3:4], in_=sm4[:, :, 2:3]))
                        P.op(dve, R(GT, sm4), R(GT), lambda e: e.tensor_tensor(out=GT[:], in0=GT[:], in1=bc(sm4[:, :, 3:4]), op=ALU.mult))

                        def ft(e):
                            ins = None
                            for t in range(4):
                                ins = e.transpose(PM[0:NE, t * 128:(t + 1) * 128], GT[:, t, :], identF[:])
                            return ins
                        P.op(pe, R(GT, identF), R(PM), ft)
                        P.op(dve, R(PM), R(GTT), lambda e: e.tensor_copy(out=GTT[:], in_=PM[0:NE, :]))
                add_step(ldC, compC)

                if moe:
                    experts = [(mw1_d[jl, ex], mw3_d[jl, ex], mw2_d[jl, ex], ex) for ex in range(NE)]
                else:
                    experts = [(fw1_d[jl], fw3_d[jl], fw2_d[jl], None)]
                for (w1a, w3a, w2a, ex) in experts:
                    for gi, (f0, g) in enumerate(GROUPS):
                        ws = {}

                        def ldF(w1a=w1a, w3a=w3a, w2a=w2a, f0=f0, g=g, ws=ws, l=l, ex=ex, gi=gi, blk=blk):
                            c0, c1 = f0 * 128, (f0 + g) * 128
                            for nm, src in (("w1", w1a), ("w3", w3a)):
                                s = nxt("KS", KS)
                                ws[nm] = s
                                load_slot(s, (l, ex, gi, nm), blk, lambda e, s=s, src=src: e.dma_start(
                                    out=s[:, :, 0:g * 128], in_=src[:, c0:c1].rearrange("(k p) f -> p k f", p=128)), True)
                            s = nxt("FS", FS)
                            ws["w2"] = s
                            load_slot(s, (l, ex, gi, "w2"), blk, lambda e, s=s: e.dma_start(
                                out=s[:, 0:g, :], in_=w2a[c0:c1, :].rearrange("(j p) d -> p j d", p=128)), False)

                        def compF(ex=ex, gi=gi, g=g, ws=ws, gtf=gtf):
                            if ex is not None and gi == 0:
                                bA = nxt("A", PA)
                                P.op(pe, R(selE, GTT), R(bA), lambda e, bA=bA: e.matmul(bA[:, :], lhsT=selE[0:NE, ex, :], rhs=GTT[0:NE, :], start=True, stop=True))
                                evac_copy(gbc[:], gbc.r, bA[:, :], bA.r)
                                for k in range(8):
                                    P.op(dve, R(hT, gbc), R(hgT), lambda e, k=k: e.tensor_tensor(
                                        out=hgT[:, k, :], in0=hT[:, k, :], in1=gbc[:], op=ALU.mult))
                            h3 = hT if ex is None else hgT
                            gt_ = nxt("gT", gT)
                            pre = {}
                            if ex is not None and gi == 0:
                                for fi in range(g):
                                    bA = nxt("A", PA)
                                    proj(ws["w1"], fi * 128, 128, bA)
                                    s_ = nxt("sl", sl)
                                    P.op(act, R(bA), R(s_), lambda e, bA=bA, s_=s_: e.activation(out=s_[:], in_=bA[:, :], func=AF.Silu))
                                    pre[fi] = s_
                            for fi in range(g):
                                if fi not in pre:
                                    bA = nxt("A", PA)
                                    proj(ws["w1"], fi * 128, 128, bA)
                                bB = nxt("B", PB)

                                def f3(e, fi=fi, bB=bB):
                                    ins = None
                                    for k in range(8):
                                        ins = e.matmul(bB[:, :], lhsT=ws["w3"][:, k, fi * 128:(fi + 1) * 128], rhs=h3[:, k, :], start=(k == 0), stop=(k == 7))
                                    return ins
                                P.op(pe, R(ws["w3"], h3), R(bB), f3)
                                if fi in pre:
                                    s_ = pre[fi]
                                else:
                                    s_ = nxt("sl", sl)
                                    P.op(act, R(bA), R(s_), lambda e, bA=bA, s_=s_: e.activation(out=s_[:], in_=bA[:, :], func=AF.Silu))
                                P.op(dve, R(s_, bB), R(gt_), lambda e, fi=fi, bB=bB, s_=s_, gt_=gt_: e.tensor_tensor(
                                    out=gt_[:, fi, :], in0=s_[:], in1=bB[:, :], op=ALU.mult))
                            for dc in range(8):
                                bY = nxt("Y3", PY + [PM])

                                def f2(e, dc=dc, bY=bY, gt_=gt_):
                                    ins = None
                                    for fi in range(g):
                                        ins = e.matmul(bY[:, :], lhsT=ws["w2"][:, fi, dc * 128:(dc + 1) * 128], rhs=gt_[:, fi, :], start=(fi == 0), stop=(fi == g - 1))
                                    return ins
                                P.op(pe, R(ws["w2"], gt_), R(bY), f2)
                                resid(bY, dc, gtf)
                        add_step(ldF, compF)

                def compZ(l=l, blk=blk, last_l=last_l):
                    if not last_l:
                        P.dma(sp, xsr[blk], xT.r, lambda e: e.dma_start(out=xs_d[blk], in_=xT[:]))
                    else:
                        for c in range(8):
                            P.op(act, R(xT), R(hgT), lambda e, c=c: e.activation(out=hgT[:, c, :], in_=xT[:, c, :], func=AF.Square))

                        def f(e):
                            ins = None
                            for c in range(8):
                                ins = e.matmul(PM[:, :], lhsT=onesB[:], rhs=hgT[:, c, :], start=(c == 0), stop=(c == 7))
                            return ins
                        P.op(pe, R(onesB, hgT), R(PM), f)
                        P.op(act, R(PM), R(rstd), lambda e: e.activation(out=rstd[:], in_=PM[:, :], func=AF.Ln, scale=1.0 / D, bias=EPS))
                        P.op(act, R(rstd), R(rstd), lambda e: e.activation(out=rstd[:], in_=rstd[:], func=AF.Exp, scale=-0.5))
                        for c in range(8):
                            P.op(dve, R(xT, rstd, vec), R(xT), lambda e, c=c: e.scalar_tensor_tensor(
                                out=xT[:, c, :], in0=xT[:, c, :], scalar=vec[:, 8 + c:9 + c], in1=rstd[:], op0=ALU.mult, op1=ALU.mult))
                        for t in range(4):
                            xo = nxt("xin", xin)
                            for half in range(2):
                                bY = nxt("Y", PY)

                                def tr(e, t=t, half=half, bY=bY):
                                    ins = None
                                    for cc in range(4):
                                        c = half * 4 + cc
                                        ins = e.transpose(bY[:, cc * 128:(cc + 1) * 128], xT[:, c, t * 128:(t + 1) * 128], identF[:])
                                    return ins
                                P.op(pe, R(xT, identF), R(bY), tr)
                                evac_copy(xo[:, half * 512:(half + 1) * 512], xo.r, bY[:, :], bY.r, eng=(act if half == 0 else dve))
                            r0 = blk * NT + t * 128
                            P.dma(sp, outr, xo.r, lambda e, xo=xo, r0=r0: e.dma_start(out=out_d[r0:r0 + 128, :], in_=xo[:]))
                add_step(None, compZ)

        xsr = [P.res("xs%d" % b) for b in range(NB)]
        outr = P.res("out")
        if steps[0][0]:
            steps[0][0]()
        for i, (ld, comp) in enumerate(steps):
            if i + 1 < len(steps) and steps[i + 1][0]:
                steps[i + 1][0]()
            comp()
        P.wait_all(sp, [outr])
        sp.e.wait_ge(outr.dsem["hw"], outr.dcnt["hw"])
    return nc


def _col(v):
    v = np.asarray(v, np.float32).reshape(-1, 128)
    return v.T


def _pack_vecs(inp, b):
    cols = [_col(inp["c"][b]), _col(inp["final_g"])]
    for l in range(DEPTH):
        cols += [_col(inp["norm_mix_g"][l]), _col(inp["norm_ffn_g"][l])]
        cw = inp["rg_conv_w"][l]
        cols += [_col(cw[k]) for k in range(4)]
        cols += [_col(inp["rg_conv_b"][l]), _col(inp["rg_ba"][l]), _col(inp["rg_bx"][l]), _col(inp["rg_lambda"][l]),
                 _col(inp["gla_bg"][l]), _col(inp["gla_norm_g"][l]), _col(inp["ada_b"][l])]
    v = np.ascontiguousarray(np.concatenate(cols, axis=1), dtype=np.float32)
    assert v.shape == (128, NV), v.shape
    return v


_NC_CACHE = {}


def kernel(**inputs):
    inp = {k: np.asarray(v) for k, v in inputs.items()}
    if "nc" not in _NC_CACHE:
        _NC_CACHE["nc"] = build()
    nc = _NC_CACHE["nc"]
    shared = {k: np.ascontiguousarray(inp[k], dtype=np.float32) for k in
              ("ada_w", "w_in", "rg_wa", "rg_wx", "gla_wg2", "w_out", "ffn_w1", "ffn_w3", "ffn_w2",
               "router_w", "moe_w1", "moe_w3", "moe_w2")}
    B = inp["x"].shape[0]
    in_maps = []
    for b in range(B):
        m = dict(shared)
        m["x"] = np.ascontiguousarray(inp["x"][b], dtype=np.float32)
        m["vecs"] = _pack_vecs(inp, b)
        in_maps.append(m)
    res = run_bass_kernel_spmd(nc, in_maps, core_ids=list(range(B)))
    out = np.stack([np.asarray(res.results[b]["out"], dtype=np.float32) for b in range(B)], axis=0)
    return out
```

```python
import numpy as np
from contextlib import ExitStack
import concourse.bass as bass
import concourse.mybir as mybir
from concourse.bass_utils import run_bass_kernel_spmd

F32 = mybir.dt.float32
BF16 = mybir.dt.bfloat16
AF = mybir.ActivationFunctionType
ALU = mybir.AluOpType
AX = mybir.AxisListType

D = 1024
S = 4096
NT = 512
NB = S // NT
DFF = 2816
NE = 8
DIN = 2576
DEPTH = 4
EPS = 1e-6
LV = 99
NV = 16 + DEPTH * LV
GROUPS = [(0, 4), (4, 4), (8, 4), (12, 4), (16, 4), (20, 2)]


class Res:
    __slots__ = ("name", "w", "r", "dsem", "dcnt", "children", "parent")

    def __init__(self, name):
        self.name = name
        self.w = None
        self.r = {}
        self.dsem = {}
        self.dcnt = {}
        self.children = []
        self.parent = None


class Eng:
    def __init__(self, name, e, sem):
        self.name, self.e, self.sem, self.cnt, self.known = name, e, sem, 0, {}


class _FirstWait:
    def __init__(self, e, wait):
        self.e, self.wait, self.done = e, wait, wait is None

    def _w(self, ins):
        if not self.done:
            ins._wait_ge(self.wait[0], self.wait[1])
            self.done = True
        return ins

    def matmul(self, *a, **k):
        return self._w(self.e.matmul(*a, **k))

    def transpose(self, *a, **k):
        return self._w(self.e.transpose(*a, **k))


class Prog:
    def __init__(self, nc, es):
        self.nc, self.es = nc, es
        self.pe = Eng("pe", nc.tensor, es.enter_context(nc.semaphore("s_pe")))
        self.act = Eng("act", nc.scalar, es.enter_context(nc.semaphore("s_act")))
        self.dve = Eng("dve", nc.vector, es.enter_context(nc.semaphore("s_dve")))
        self.pool = Eng("pool", nc.gpsimd, es.enter_context(nc.semaphore("s_pool")))
        self.sp = Eng("sp", nc.sync, es.enter_context(nc.semaphore("s_sp")))
        self.nres = 0

    def res(self, name):
        return Res(name)

    def _need(self, eng, reads, writes):
        def expand(rs):
            out = []
            for r in rs:
                out.append(r)
                out.extend(r.children)
                if r.parent is not None:
                    out.append(r.parent)
            return out

        rd, wr = {}, {}

        def add(dct, tok):
            if tok is None:
                return
            s, v = tok
            if dct.get(id(s), (None, 0))[1] < v:
                dct[id(s)] = (s, v)

        for r in expand(reads):
            add(rd, r.w)
        for w in expand(writes):
            add(wr, w.w)
            for s, v in w.r.values():
                add(wr, (s, v))
        need_r, need_w = [], []
        for k, (s, v) in rd.items():
            if eng is self.pe and s is self.pe.sem:
                continue
            if eng.known.get(k, 0) < v:
                need_r.append((s, v))
        for k, (s, v) in wr.items():
            if eng is self.pe and s is self.pe.sem:
                continue
            if k in rd and rd[k][1] >= v:
                continue
            if eng.known.get(k, 0) < v:
                need_w.append((s, v))
        return need_r, need_w

    def _deps(self, eng, reads, writes):
        need_r, need_w = self._need(eng, reads, writes)
        for s, v in need_r + need_w:
            if eng.known.get(id(s), 0) < v:
                eng.e.wait_ge(s, v)
                eng.known[id(s)] = v

    def op(self, eng, reads, writes, fn):
        need_r, need_w = self._need(eng, reads, writes)
        embed = None
        if eng is self.pe:
            standalone = need_r + need_w[:-1]
            if need_w:
                embed = need_w[-1]
        else:
            allw = need_r + need_w
            standalone = allw[:-1]
            if allw:
                embed = allw[-1]
        for s, v in standalone:
            if eng.known.get(id(s), 0) < v:
                eng.e.wait_ge(s, v)
                eng.known[id(s)] = v
        if eng is self.pe:
            prox = _FirstWait(eng.e, embed)
            ins = fn(prox)
            if embed is not None and not prox.done:
                raise RuntimeError("embedded wait not consumed")
        else:
            ins = fn(eng.e)
            if embed is not None:
                ins._wait_ge(embed[0], embed[1])
        if embed is not None:
            eng.known[id(embed[0])] = max(eng.known.get(id(embed[0]), 0), embed[1])
        eng.cnt += 1
        ins.then_inc(eng.sem, 1)
        tok = (eng.sem, eng.cnt)
        for r in reads:
            r.r[id(eng.sem)] = tok
        for w in writes:
            w.w = tok
            w.r = {}
        return ins

    def dma(self, q, out_res, in_res, fn):
        self._deps(q, [in_res], [out_res])
        kq = "sw" if q is self.pool else "hw"
        if kq not in out_res.dsem:
            self.nres += 1
            out_res.dsem[kq] = self.es.enter_context(self.nc.semaphore("d%d" % self.nres))
            out_res.dcnt[kq] = 0
        ins = fn(q.e)
        out_res.dcnt[kq] += 16
        dsem = out_res.dsem[kq]
        ins.then_inc(dsem, 16)
        tok = (dsem, out_res.dcnt[kq])
        in_res.r[id(dsem)] = tok
        out_res.w = tok
        out_res.r = {}
        return ins

    def wait_all(self, q, ress):
        self._deps(q, ress, [])


def build(nlayers=DEPTH):
    nc = bass.Bass("TRN2", target_bir_lowering=False)

    def din(name, shape):
        return nc.dram_tensor(name, shape, F32, kind="ExternalInput").ap()

    x_d = din("x", [S, D])
    vec_d = din("vecs", [128, NV])
    ada_w_d = din("ada_w", [DEPTH, D, 6 * D])
    w_in_d = din("w_in", [DEPTH, D, DIN])
    wa_d = din("rg_wa", [DEPTH, 8, 64, 64])
    wx_d = din("rg_wx", [DEPTH, 8, 64, 64])
    wg2_d = din("gla_wg2", [DEPTH, 16, 256])
    w_out_d = din("w_out", [DEPTH, D, D])
    fw1_d = din("ffn_w1", [2, D, DFF])
    fw3_d = din("ffn_w3", [2, D, DFF])
    fw2_d = din("ffn_w2", [2, DFF, D])
    rw_d = din("router_w", [2, D, NE])
    mw1_d = din("moe_w1", [2, NE, D, DFF])
    mw3_d = din("moe_w3", [2, NE, D, DFF])
    mw2_d = din("moe_w2", [2, NE, DFF, D])
    out_d = nc.dram_tensor("out", [S, D], F32, kind="ExternalOutput").ap()
    xs_d = nc.dram_tensor("xs", [NB, 128, 8, NT], F32, kind="Internal").ap()
    wsc_l = [nc.dram_tensor("wsc%d" % l, [7 + (144 if l % 2 == 1 else 18), 128, 4096], BF16, kind="Internal").ap() for l in range(DEPTH)]

    with ExitStack() as es:
        P = Prog(nc, es)
        pe, act, dve, pool, sp = P.pe, P.act, P.dve, P.pool, P.sp

        class T:
            def __init__(self, name, shape, dt, psum=False):
                if psum:
                    self.t = es.enter_context(nc.psum_tensor(name, shape, dt))
                else:
                    self.t = es.enter_context(nc.sbuf_tensor(name, shape, dt))
                self.r = P.res(name)

            def __getitem__(self, k):
                return self.t[k]

        def sb(name, shape, dt=F32):
            return T(name, shape, dt)

        dummy = P.res("dram_in")

        identF = sb("identF", [128, 128])
        identB = sb("identB", [128, 128], BF16)
        onesB = sb("onesB", [128, 128], BF16)
        maskT = sb("maskT", [128, 128])
        rowm = sb("rowm", [128, 2])
        resetm = sb("resetm", [128, NT])
        selE = sb("selE", [8, NE, 128])
        vec = sb("vec", [128, NV])
        cact2 = sb("cact2", [128, 8, 2])
        modall = sb("modall", [128, DEPTH, 48])
        lvec = sb("lvec", [128, 40])
        WaBD = sb("WaBD", [128, 4, 128], BF16)
        WxBD = sb("WxBD", [128, 4, 128], BF16)
        wg2b = sb("wg2b", [16, 256], BF16)
        flw = sb("flw", [128, 8, 16], BF16)
        rwb = sb("rwb", [128, 8, NE], BF16)
        halo = sb("halo", [128, 4, 3])
        hst = sb("hst", [128, 4])
        Sst = [sb("Sst%d" % p, [128, 128]) for p in range(2)]
        xT = sb("xT", [128, 8, NT])
        xTc = [P.res("xTc%d" % c) for c in range(8)]
        for r_ in xTc:
            r_.parent = xT.r
        xT.r.children = list(xTc)
        tmpY = [sb("tmpY%d" % i, [128, NT]) for i in range(2)]
        hT = sb("hT", [128, 8, NT], BF16)
        hgT = sb("hgT", [128, 8, NT], BF16)
        rstd = sb("rstd", [128, NT])
        tmpx = [sb("tmpx%d" % i, [128, NT]) for i in range(1)]
        xin = [sb("xin%d" % i, [128, D]) for i in range(2)]
        KS = [sb("KS%d" % i, [128, 8, 512], BF16) for i in range(5)]
        FS = [sb("FS%d" % i, [128, 4, D], BF16) for i in range(3)]
        gT = [sb("gT%d" % i, [128, 4, NT], BF16) for i in range(2)]
        gTf = []
        for g_ in gT:
            ch = [P.res(g_.r.name + "_f%d" % i) for i in range(4)]
            for r_ in ch:
                r_.parent = g_.r
            g_.r.children = ch
            gTf.append(ch)
        sl = [sb("sl%d" % i, [128, NT]) for i in range(4)]
        rgxh = sb("rgxh", [128, NT + 3])
        Ub = sb("Ub", [128, NT], BF16)
        mU, mR, mI, mA, mM, mGG, mG2 = [sb("m%s" % n, [128, NT]) for n in ("U", "R", "I", "A", "M", "GG", "G2")]
        mixo = sb("mixo", [128, 8, NT], BF16)
        flowT = sb("flowT", [16, NT], BF16)
        vtok = sb("vtok", [128, 4, 512], BF16)
        gsil = sb("gsil", [128, 4, NT])
        qT, kT, LF, Bc, EQ, EK = [sb("g%s" % n, [128, NT]) for n in ("q", "k", "LF", "Bc", "EQ", "EK")]
        qloc = sb("qloc", [128, NT], BF16)
        kl = [sb("kl%d" % i, [128, NT], BF16) for i in range(2)]
        klf = sb("klf", [128, NT], BF16)
        kltok = sb("kltok", [128, 4, 128], BF16)
        dsm = sb("dsm", [128, 6, 4])
        Sb = [sb("Sb%d" % i, [128, 128], BF16) for i in range(2)]
        scm = [sb("scm%d" % i, [128, 128], BF16) for i in range(2)]
        T1 = [sb("T1_%d" % i, [128, 128]) for i in range(2)]
        oT = sb("oT", [128, NT])
        sqo = sb("sqo", [128, NT], BF16)
        rso = sb("rso", [128, NT])
        LG = sb("LG", [128, 4, NE])
        L2 = sb("L2", [128, 4, NE])
        GT = sb("GT", [128, 4, NE])
        sm4 = sb("sm4", [128, 4, 4])
        GTT = sb("GTT", [8, NT])
        gbc = sb("gbc", [128, NT], BF16)
        PA = [T("psA%d" % i, [128, 512], F32, psum=True) for i in range(2)]
        PB = [T("psB%d" % i, [128, 512], F32, psum=True) for i in range(2)]
        PY = [T("psY%d" % i, [128, 512], F32, psum=True) for i in range(2)]
        PM = T("psM", [128, 512], F32, psum=True)
        PT = T("psT", [128, 1024], BF16, psum=True)
        ctr = {"A": 0, "B": 0, "Y": 0, "KS": 0, "FS": 0, "gT": 0, "sl": 0, "tmpx": 0, "xin": 0, "tmpY": 0, "Y3": 0}

        def nxt(kind, arr):
            i = ctr[kind]
            ctr[kind] = i + 1
            return arr[i % len(arr)]

        def R(*ts):
            return [t.r for t in ts]

        imgs = {}
        wbres = [P.res("wb%d" % i) for i in range(8)]
        wbctr = [0]

        def load_slot(slot, key, blk, src_fn, kmajor):
            pat = "p (k f) -> p k f" if kmajor else "p (j d) -> p j d"
            kw = {"k": 8} if kmajor else {"j": 4}
            wsc_d = wsc_l[key[0]]
            if blk == 0:
                idx = sum(1 for kk in imgs if kk[0] == key[0])
                P.dma(pool, slot.r, dummy, src_fn)
                wr = wbres[wbctr[0] % 8]
                wbctr[0] += 1
                P.dma(sp, wr, slot.r, lambda e: e.dma_start(out=wsc_d[idx].rearrange(pat, **kw), in_=slot[:]))
                ir = P.res("img%d" % idx)
                ir.w = wr.w
                imgs[key] = (idx, ir)
            else:
                idx, ir = imgs[key]
                P.dma(sp, slot.r, ir, lambda e: e.dma_start(out=slot[:], in_=wsc_d[idx].rearrange(pat, **kw)))

        P.op(pool, [], R(identF), lambda e: e.memset(identF[:], 1.0))
        P.op(pool, R(identF), R(identF), lambda e: e.affine_select(out=identF[:], in_=identF[:], pattern=[[1, 128]], compare_op=ALU.is_equal, fill=0.0, base=0, channel_multiplier=-1))
        P.op(pool, [], R(maskT), lambda e: e.memset(maskT[:], 1.0))
        P.op(pool, R(maskT), R(maskT), lambda e: e.affine_select(out=maskT[:], in_=maskT[:], pattern=[[1, 128]], compare_op=ALU.is_ge, fill=0.0, base=0, channel_multiplier=-1))
        P.op(pool, [], R(rowm), lambda e: e.memset(rowm[:], 1.0))
        P.op(pool, R(rowm), R(rowm), lambda e: e.affine_select(out=rowm[:, 0:1], in_=rowm[:, 0:1], pattern=[[0, 1]], compare_op=ALU.is_ge, fill=0.0, base=63, channel_multiplier=-1))
        P.op(pool, R(rowm), R(rowm), lambda e: e.affine_select(out=rowm[:, 1:2], in_=rowm[:, 1:2], pattern=[[0, 1]], compare_op=ALU.is_ge, fill=0.0, base=-64, channel_multiplier=1))
        P.op(pool, [], R(selE), lambda e: e.memset(selE[:], 1.0))
        P.op(pool, R(selE), R(selE), lambda e: e.affine_select(out=selE[:], in_=selE[:], pattern=[[-1, NE], [0, 128]], compare_op=ALU.is_equal, fill=0.0, base=0, channel_multiplier=1))
        P.op(pool, [], R(onesB), lambda e: e.memset(onesB[:], 1.0))
        P.op(pool, [], R(resetm), lambda e: e.memset(resetm[:], 1.0))
        for t in range(NT // 128):
            P.op(pool, R(resetm), R(resetm), lambda e, t=t: e.memset(resetm[:, t * 128:t * 128 + 1], 0.0))
        P.op(pool, R(identF), R(identB), lambda e: e.tensor_copy(out=identB[:], in_=identF[:]))

        P.dma(sp, vec.r, dummy, lambda e: e.dma_start(out=vec[:], in_=vec_d[:, :]))
        P.op(act, R(vec), R(cact2), lambda e: e.activation(out=cact2[:, :, 0], in_=vec[:, 0:8], func=AF.Silu))
        P.op(act, R(vec), R(cact2), lambda e: e.activation(out=cact2[:, :, 1], in_=vec[:, 0:8], func=AF.Silu))

        for l in range(nlayers):
            vb = 16 + l * LV
            for pc in range(12):
                P.dma(sp, xT.r, dummy, lambda e, l=l, pc=pc: e.dma_start(
                    out=xT[:], in_=ada_w_d[l, :, pc * 512:(pc + 1) * 512].rearrange("(k p) f -> p k f", p=128)))

                def mm(e, pc=pc):
                    ins = None
                    for jj in range(4):
                        j = pc * 4 + jj
                        for k in range(8):
                            ins = e.matmul(PM[:, 2 * j:2 * j + 2], lhsT=xT[:, k, jj * 128:(jj + 1) * 128], rhs=cact2[:, k, :], start=(k == 0), stop=(k == 7))
                    return ins
                P.op(pe, R(xT, cact2), R(PM), mm)
            pmv = PM[:, 0:96].rearrange("p (j t) -> p j t", t=2)
            P.op(dve, R(PM, vec), R(modall), lambda e, l=l, vb=vb, pmv=pmv: e.tensor_tensor(
                out=modall[:, l, :], in0=pmv[:, :, 0], in1=vec[:, vb + 51:vb + 99], op=ALU.add))

        def evac_copy(dst_ap, dst_res, src_ap, src_res, eng=None):
            eng = eng or act
            if eng is act:
                P.op(act, [src_res], [dst_res], lambda e: e.copy(out=dst_ap, in_=src_ap))
            else:
                P.op(dve, [src_res], [dst_res], lambda e: e.tensor_copy(out=dst_ap, in_=src_ap))

        def proj(wslot, c0, width, bank, rhs_of_k=None, m_out=128):
            def f(e):
                ins = None
                for k in range(8):
                    ins = e.matmul(bank[0:width, :], lhsT=wslot[:, k, c0:c0 + width], rhs=hT[:, k, :], start=(k == 0), stop=(k == 7))
                return ins
            P.op(pe, R(wslot, hT), R(bank), f)

        def norm_mod(s1_ap, sh_ap):
            for c in range(8):
                P.op(act, R(xT), R(hgT), lambda e, c=c: e.activation(out=hgT[:, c, :], in_=xT[:, c, :], func=AF.Square))

            def f(e):
                ins = None
                for c in range(8):
                    ins = e.matmul(PM[:, :], lhsT=onesB[:], rhs=hgT[:, c, :], start=(c == 0), stop=(c == 7))
                return ins
            P.op(pe, R(onesB, hgT), R(PM), f)
            P.op(act, R(PM), R(rstd), lambda e: e.activation(out=rstd[:], in_=PM[:, :], func=AF.Ln, scale=1.0 / D, bias=EPS))
            P.op(act, R(rstd), R(rstd), lambda e: e.activation(out=rstd[:], in_=rstd[:], func=AF.Exp, scale=-0.5))
            for c in range(8):
                tx = nxt("tmpx", tmpx + tmpY)
                P.op(dve, R(xT, rstd, lvec), R(tx), lambda e, c=c, tx=tx: e.scalar_tensor_tensor(
                    out=tx[:], in0=xT[:, c, :], scalar=s1_ap[:, c:c + 1], in1=rstd[:], op0=ALU.mult, op1=ALU.mult))
                P.op(act, R(tx, modall), R(hT), lambda e, c=c, tx=tx: e.activation(
                    out=hT[:, c, :], in_=tx[:], func=AF.Identity, bias=sh_ap[:, c:c + 1]))

        def resid(bank, dc, gt_ap):
            if dc % 2 == 0:
                P.op(dve, [bank.r, modall.r, xTc[dc]], [xTc[dc]], lambda e: e.scalar_tensor_tensor(
                    out=xT[:, dc, :], in0=bank[:, :], scalar=gt_ap[:, dc:dc + 1], in1=xT[:, dc, :], op0=ALU.mult, op1=ALU.add))
            else:
                ty = nxt("tmpY", tmpY)
                P.op(act, [bank.r, modall.r], [ty.r], lambda e: e.activation(out=ty[:], in_=bank[:, :], func=AF.Identity, scale=gt_ap[:, dc:dc + 1]))
                P.op(pool, [ty.r, xTc[dc]], [xTc[dc]], lambda e: e.tensor_tensor(out=xT[:, dc, :], in0=xT[:, dc, :], in1=ty[:], op=ALU.add))

        steps = []

        def add_step(loads, compute):
            steps.append((loads, compute))

        for l in range(nlayers):
            vb = 16 + l * LV
            moe = (l % 2 == 1)
            jl = l // 2
            mod = lambda j0, l=l: modall[:, l, j0:j0 + 8]
            shm, gtm, shf, gtf = mod(0), mod(16), mod(24), mod(40)
            s1m, s1f, spv, sp2v = lvec[:, 0:8], lvec[:, 8:16], lvec[:, 16:20], lvec[:, 20:24]
            cw = lambda k, c, vb=vb: vec[:, vb + 16 + k * 4 + c:vb + 16 + k * 4 + c + 1]
            cb = lambda c, vb=vb: vec[:, vb + 32 + c:vb + 33 + c]
            ba = lambda c, vb=vb: vec[:, vb + 36 + c:vb + 37 + c]
            bx = lambda c, vb=vb: vec[:, vb + 40 + c:vb + 41 + c]
            bg = lambda p, vb=vb: vec[:, vb + 48 + p:vb + 49 + p]
            glag = vec[:, vb + 50:vb + 51]

            def layer_setup(l=l, vb=vb, moe=moe, jl=jl):
                P.op(dve, R(modall, vec), R(lvec), lambda e: e.scalar_tensor_tensor(
                    out=lvec[:, 0:8], in0=modall[:, l, 8:16], scalar=1.0, in1=vec[:, vb:vb + 8], op0=ALU.add, op1=ALU.mult))
                P.op(dve, R(modall, vec), R(lvec), lambda e: e.scalar_tensor_tensor(
                    out=lvec[:, 8:16], in0=modall[:, l, 32:40], scalar=1.0, in1=vec[:, vb + 8:vb + 16], op0=ALU.add, op1=ALU.mult))
                P.op(act, R(vec), R(lvec), lambda e: e.activation(out=lvec[:, 24:28], in_=vec[:, vb + 44:vb + 48], func=AF.Exp, scale=-1.0))
                P.op(act, R(lvec), R(lvec), lambda e: e.activation(out=lvec[:, 24:28], in_=lvec[:, 24:28], func=AF.Ln, bias=1.0))
                P.op(dve, R(lvec), R(lvec), lambda e: e.tensor_scalar(out=lvec[:, 16:20], in0=lvec[:, 24:28], scalar1=-8.0, scalar2=None, op0=ALU.mult))
                P.op(dve, R(lvec), R(lvec), lambda e: e.tensor_scalar(out=lvec[:, 20:24], in0=lvec[:, 24:28], scalar1=-16.0, scalar2=None, op0=ALU.mult))
                P.op(dve, [], R(WaBD), lambda e: e.memset(WaBD[:], 0.0))
                P.op(dve, [], R(WxBD), lambda e: e.memset(WxBD[:], 0.0))
                for n in range(8):
                    pb = (n % 2) * 64
                    P.dma(pool, WaBD.r, dummy, lambda e, n=n, pb=pb: e.dma_start(out=WaBD[pb:pb + 64, n // 2, pb:pb + 64], in_=wa_d[l, n, :, :]))
                    P.dma(pool, WxBD.r, dummy, lambda e, n=n, pb=pb: e.dma_start(out=WxBD[pb:pb + 64, n // 2, pb:pb + 64], in_=wx_d[l, n, :, :]))
                P.dma(pool, wg2b.r, dummy, lambda e: e.dma_start(out=wg2b[:], in_=wg2_d[l, :, :]))
                P.dma(pool, flw.r, dummy, lambda e: e.dma_start(out=flw[:], in_=w_in_d[l, :, 2560:2576].rearrange("(k p) f -> p k f", p=128)))
                if moe:
                    P.dma(pool, rwb.r, dummy, lambda e: e.dma_start(out=rwb[:], in_=rw_d[jl, :, :].rearrange("(k p) f -> p k f", p=128)))
                P.op(dve, [], R(halo), lambda e: e.memset(halo[:], 0.0))
                P.op(dve, [], R(hst), lambda e: e.memset(hst[:], 0.0))
                for p in range(2):
                    P.op(dve, [], R(Sst[p]), lambda e, p=p: e.memset(Sst[p][:], 0.0))

            for blk in range(NB):
                first_l, last_l = (l == 0), (l == nlayers - 1)
                slots = {}

                def ld_in(names, l=l, slots=slots, blk=blk):
                    def f():
                        for nm, c0 in names:
                            s = nxt("KS", KS)
                            slots[nm] = s
                            load_slot(s, (l, "in", nm), blk, lambda e, s=s, c0=c0: e.dma_start(
                                out=s[:], in_=w_in_d[l, :, c0:c0 + 512].rearrange("(k p) f -> p k f", p=128)), True)
                    return f

                def compA(l=l, blk=blk, slots=slots, first_l=first_l, shm=shm, s1m=s1m, cw=cw, cb=cb, ba=ba, bx=bx, spv=spv, sp2v=sp2v, layer_setup=layer_setup):
                    if blk == 0:
                        layer_setup()
                    if first_l:
                        for t in range(4):
                            xi = nxt("xin", xin)
                            r0 = blk * NT + t * 128
                            P.dma(sp, xi.r, dummy, lambda e, xi=xi, r0=r0: e.dma_start(out=xi[:], in_=x_d[r0:r0 + 128, :]))
                            for half in range(2):
                                bT = nxt("Y", PY)

                                def tr(e, xi=xi, half=half, bT=bT):
                                    ins = None
                                    for cc in range(4):
                                        c = half * 4 + cc
                                        ins = e.transpose(bT[:, cc * 128:(cc + 1) * 128], xi[:, c * 128:(c + 1) * 128], identF[:])
                                    return ins
                                P.op(pe, R(xi, identF), R(bT), tr)
                                if half == 0:
                                    P.op(dve, R(bT), R(xT), lambda e, t=t, half=half, bT=bT: e.tensor_copy(
                                        out=xT[:, half * 4:half * 4 + 4, t * 128:(t + 1) * 128], in_=bT[:, :].rearrange("p (c t) -> p c t", t=128)))
                                else:
                                    P.op(act, R(bT), R(xT), lambda e, t=t, half=half, bT=bT: e.copy(
                                        out=xT[:, half * 4:half * 4 + 4, t * 128:(t + 1) * 128], in_=bT[:, :].rearrange("p (c t) -> p c t", t=128)))
                    else:
                        P.dma(sp, xT.r, xsr[blk], lambda e: e.dma_start(out=xT[:], in_=xs_d[blk]))
                    norm_mod(s1m, shm)
                    P0, P1 = slots["rgx"], slots["rgg"]
                    for c in range(4):
                        bA = PA[0]
                        proj(P0, c * 128, 128, bA)
                        yield
                        P.op(dve, R(halo), R(rgxh), lambda e, c=c: e.tensor_copy(out=rgxh[:, 0:3], in_=halo[:, c, :]))
                        yield
                        evac_copy(rgxh[:, 3:NT + 3], rgxh.r, bA[:, :], bA.r)
                        yield
                        bB = PB[0]
                        proj(P1, c * 128, 128, bB)
                        yield
                        evac_copy(mGG[:], mGG.r, bB[:, :], bB.r)
                        yield
                        P.op(dve, R(rgxh, vec), R(mU), lambda e, c=c: e.tensor_scalar(
                            out=mU[:], in0=rgxh[:, 3:NT + 3], scalar1=cw(3, c), scalar2=cb(c), op0=ALU.mult, op1=ALU.add))
                        yield
                        for k in range(3):
                            P.op(dve, R(rgxh, vec, mU), R(mU), lambda e, c=c, k=k: e.scalar_tensor_tensor(
                                out=mU[:], in0=rgxh[:, k:k + NT], scalar=cw(k, c), in1=mU[:], op0=ALU.mult, op1=ALU.add))
                            yield
                        P.op(dve, R(rgxh), R(halo), lambda e, c=c: e.tensor_copy(out=halo[:, c, :], in_=rgxh[:, NT:NT + 3]))
                        yield
                        P.op(act, R(mU), R(Ub), lambda e: e.copy(out=Ub[:], in_=mU[:]))
                        yield
                        bA = PA[0]
                        P.op(pe, R(WaBD, Ub), R(bA), lambda e, c=c, bA=bA: e.matmul(bA[:, :], lhsT=WaBD[:, c, :], rhs=Ub[:], start=True, stop=True))
                        yield
                        bB = PB[0]
                        P.op(pe, R(WxBD, Ub), R(bB), lambda e, c=c, bB=bB: e.matmul(bB[:, :], lhsT=WxBD[:, c, :], rhs=Ub[:], start=True, stop=True))
                        yield
                        P.op(act, R(bA, vec), R(mR), lambda e, c=c, bA=bA: e.activation(out=mR[:], in_=bA[:, :], func=AF.Sigmoid, bias=ba(c)))
                        yield
                        P.op(act, R(bB, vec), R(mI), lambda e, c=c, bB=bB: e.activation(out=mI[:], in_=bB[:, :], func=AF.Sigmoid, bias=bx(c)))
                        yield
                        P.op(act, R(mR, lvec), R(mA), lambda e, c=c: e.activation(out=mA[:], in_=mR[:], func=AF.Exp, scale=spv[:, c:c + 1]))
                        yield
                        P.op(act, R(mR, lvec), R(mM), lambda e, c=c: e.activation(out=mM[:], in_=mR[:], func=AF.Exp, scale=sp2v[:, c:c + 1]))
                        yield
                        P.op(act, R(mM), R(mM), lambda e: e.activation(out=mM[:], in_=mM[:], func=AF.Sqrt, scale=-1.0, bias=1.0))
                        yield
                        if blk == 0:
                            P.op(dve, [], R(mM), lambda e: e.memset(mM[:, 0:1], 1.0))
                            yield
                        P.op(dve, R(mM, mI), R(mM), lambda e: e.tensor_tensor(out=mM[:], in0=mM[:], in1=mI[:], op=ALU.mult))
                        yield
                        P.op(dve, R(mM, mU), R(mM), lambda e: e.tensor_tensor(out=mM[:], in0=mM[:], in1=mU[:], op=ALU.mult))
                        yield
                        P.op(dve, R(mA, mM, hst), R(mR), lambda e, c=c: e.tensor_tensor_scan(
                            out=mR[:], data0=mA[:], data1=mM[:], initial=hst[:, c:c + 1], op0=ALU.mult, op1=ALU.add))
                        yield
                        P.op(dve, R(mR), R(hst), lambda e, c=c: e.tensor_copy(out=hst[:, c:c + 1], in_=mR[:, NT - 1:NT]))
                        yield
                        P.op(act, R(mGG), R(mG2), lambda e: e.activation(out=mG2[:], in_=mGG[:], func=AF.Square))
                        yield
                        P.op(dve, R(mG2), R(mG2), lambda e: e.tensor_scalar(out=mG2[:], in0=mG2[:], scalar1=0.044715, scalar2=1.0, op0=ALU.mult, op1=ALU.add))
                        yield
                        P.op(dve, R(mG2, mGG), R(mG2), lambda e: e.tensor_tensor(out=mG2[:], in0=mG2[:], in1=mGG[:], op=ALU.mult))
                        yield
                        P.op(act, R(mG2), R(mG2), lambda e: e.activation(out=mG2[:], in_=mG2[:], func=AF.Sigmoid, scale=1.5957691216057308))
                        yield
                        P.op(dve, R(mG2, mGG), R(mG2), lambda e: e.tensor_tensor(out=mG2[:], in0=mG2[:], in1=mGG[:], op=ALU.mult))
                        yield
                        P.op(dve, R(mR, mG2), R(mixo), lambda e, c=c: e.tensor_tensor(out=mixo[:, c, :], in0=mR[:], in1=mG2[:], op=ALU.mult))
                        yield

                def compB(l=l, blk=blk, slots=slots, bg=bg, glag=glag):
                    P2, P3, P4 = slots["qk"], slots["v"], slots["g"]
                    bA = PA[1]

                    def ff(e, bA=bA):
                        ins = None
                        for k in range(8):
                            ins = e.matmul(bA[0:16, :], lhsT=flw[:, k, :], rhs=hT[:, k, :], start=(k == 0), stop=(k == 7))
                        return ins
                    P.op(pe, R(flw, hT), R(bA), ff)
                    yield
                    evac_copy(flowT[:], flowT.r, bA[0:16, :], bA.r)
                    yield
                    for t in range(4):
                        bB = PB[1]

                        def fv(e, t=t, bB=bB):
                            ins = None
                            for k in range(8):
                                ins = e.matmul(bB[:, :], lhsT=hT[:, k, t * 128:(t + 1) * 128], rhs=P3[:, k, :], start=(k == 0), stop=(k == 7))
                            return ins
                        P.op(pe, R(P3, hT), R(bB), fv)
                        yield
                        evac_copy(vtok[:, t, :], vtok.r, bB[:, :], bB.r, eng=(act if t % 2 == 0 else dve))
                        yield
                    for hd in range(4):
                        bA = PA[1]
                        proj(P4, hd * 128, 128, bA)
                        yield
                        P.op(act, R(bA), R(gsil), lambda e, hd=hd, bA=bA: e.activation(out=gsil[:, hd, :], in_=bA[:, :], func=AF.Silu))
                        yield
                    for p in range(2):
                        bA = PA[1]
                        proj(P2, p * 128, 128, bA)
                        yield
                        evac_copy(qT[:], qT.r, bA[:, :], bA.r)
                        yield
                        bB = PB[1]
                        proj(P2, 256 + p * 128, 128, bB)
                        yield
                        evac_copy(kT[:], kT.r, bB[:, :], bB.r, eng=dve)
                        yield
                        bA = PA[1]
                        P.op(pe, R(wg2b, flowT), R(bA), lambda e, p=p, bA=bA: e.matmul(
                            bA[:, :], lhsT=wg2b[0:16, p * 128:(p + 1) * 128], rhs=flowT[0:16, :], start=True, stop=True))
                        yield
                        P.op(act, R(bA, vec), R(LF), lambda e, p=p, bA=bA: e.activation(out=LF[:], in_=bA[:, :], func=AF.Sigmoid, bias=bg(p)))
                        yield
                        P.op(act, R(LF), R(LF), lambda e: e.activation(out=LF[:], in_=LF[:], func=AF.Ln))
                        yield
                        P.op(dve, R(resetm, LF), R(Bc), lambda e: e.tensor_tensor_scan(
                            out=Bc[:], data0=resetm[:], data1=LF[:], initial=0.0, op0=ALU.mult, op1=ALU.add))
                        yield
                        Bv = Bc[:].rearrange("p (c t) -> p c t", t=128)
                        P.op(dve, R(Bc), R(EQ), lambda e, Bv=Bv: e.tensor_tensor(
                            out=EQ[:].rearrange("p (c t) -> p c t", t=128), in0=Bv, in1=Bv[:, :, 63:64].to_broadcast([128, 4, 128]), op=ALU.subtract))
                        yield
                        P.op(act, R(EQ), R(EK), lambda e: e.activation(out=EK[:], in_=EQ[:], func=AF.Exp, scale=-1.0 / 16))
                        yield
                        P.op(act, R(EQ), R(EQ), lambda e: e.activation(out=EQ[:], in_=EQ[:], func=AF.Exp, scale=1.0 / 16))
                        yield
                        P.op(dve, R(qT, EQ), R(qloc), lambda e: e.scalar_tensor_tensor(
                            out=qloc[:], in0=qT[:], scalar=0.125, in1=EQ[:], op0=ALU.mult, op1=ALU.mult))
                        yield
                        for hp in range(2):
                            P.op(dve, R(kT, EK, rowm), R(kl[hp]), lambda e, hp=hp: e.scalar_tensor_tensor(
                                out=kl[hp][:], in0=kT[:], scalar=rowm[:, hp:hp + 1], in1=EK[:], op0=ALU.mult, op1=ALU.mult))
                            yield
                        P.op(dve, R(kT, EK), R(klf), lambda e: e.tensor_tensor(out=klf[:], in0=kT[:], in1=EK[:], op=ALU.mult))
                        yield
                        P.op(act, R(Bc), R(dsm), lambda e, Bv=Bv: e.activation(out=dsm[:, 0, :], in_=Bv[:, :, 127], func=AF.Exp, scale=1.0 / 16))
                        yield
                        P.op(act, R(Bc), R(dsm), lambda e, Bv=Bv: e.activation(out=dsm[:, 1, :], in_=Bv[:, :, 63], func=AF.Exp, scale=1.0 / 16))
                        yield
                        P.op(dve, R(Bc), R(dsm), lambda e, Bv=Bv: e.tensor_tensor(out=dsm[:, 5, :], in0=Bv[:, :, 127], in1=Bv[:, :, 63], op=ALU.subtract))
                        yield
                        P.op(act, R(dsm), R(dsm), lambda e: e.activation(out=dsm[:, 2, :], in_=dsm[:, 5, :], func=AF.Exp, scale=1.0 / 16))
                        yield
                        for hp in range(2):
                            P.op(dve, R(dsm, rowm), R(dsm), lambda e, hp=hp: e.tensor_scalar(
                                out=dsm[:, 3 + hp, :], in0=dsm[:, 2, :], scalar1=rowm[:, hp:hp + 1], scalar2=None, op0=ALU.mult))
                            yield
                        def ftr(e):
                            ins = None
                            for t in range(4):
                                ins = e.transpose(PT[:, t * 128:(t + 1) * 128], klf[:, t * 128:(t + 1) * 128], identB[:])
                            return ins
                        P.op(pe, R(klf, identB), R(PT), ftr)
                        yield
                        P.op(act, R(PT), R(kltok), lambda e: e.copy(out=kltok[:].rearrange("p c t -> p (c t)"), in_=PT[:, 0:512]))
                        yield
                        bO = [nxt("Y", PY), nxt("Y", PY)]
                        S_ = Sst[p]
                        for t in range(4):
                            ts = slice(t * 128, (t + 1) * 128)
                            bU = PA[1]

                            def fu(e, t=t, bU=bU, p=p):
                                e.matmul(bU[:, 0:128], lhsT=kltok[:, t, :], rhs=vtok[:, t, (2 * p) * 128:(2 * p + 1) * 128], start=True, stop=True)
                                return e.matmul(bU[:, 128:256], lhsT=kltok[:, t, :], rhs=vtok[:, t, (2 * p + 1) * 128:(2 * p + 2) * 128], start=True, stop=True)
                            P.op(pe, R(kltok, vtok), R(bU), fu)
                            yield
                            bS = PB[1]

                            def fs(e, ts=ts, bS=bS):
                                e.matmul(bS[:, 0:128], lhsT=kl[0][:, ts], rhs=qloc[:, ts], start=True, stop=True)
                                return e.matmul(bS[:, 128:256], lhsT=kl[1][:, ts], rhs=qloc[:, ts], start=True, stop=True)
                            P.op(pe, R(kl[0], kl[1], qloc), R(bS), fs)
                            yield
                            for hp in range(2):
                                P.op(dve, R(bS, maskT), R(scm[hp]), lambda e, hp=hp, bS=bS: e.tensor_tensor(
                                    out=scm[hp][:], in0=bS[:, hp * 128:(hp + 1) * 128], in1=maskT[:], op=ALU.mult))
                                yield
                                P.op(dve, R(S_, dsm, rowm), R(Sb[hp]), lambda e, hp=hp, t=t, S_=S_: e.tensor_scalar(
                                    out=Sb[hp][:], in0=S_[:], scalar1=dsm[:, 1, t:t + 1], scalar2=rowm[:, hp:hp + 1], op0=ALU.mult, op1=ALU.mult))
                                yield
                            for hp in range(2):
                                h = 2 * p + hp

                                def fo(e, hp=hp, h=h, t=t, ts=ts):
                                    e.matmul(bO[hp][:, ts], lhsT=vtok[:, t, h * 128:(h + 1) * 128], rhs=scm[hp][:], start=True, stop=False)
                                    return e.matmul(bO[hp][:, ts], lhsT=Sb[hp][:], rhs=qloc[:, ts], start=False, stop=True)
                                P.op(pe, R(vtok, scm[hp], Sb[hp], qloc), R(bO[hp]), fo)
                                yield
                            P.op(dve, R(bU, dsm), R(T1[0]), lambda e, t=t, bU=bU: e.tensor_scalar(
                                out=T1[0][:], in0=bU[:, 0:128], scalar1=dsm[:, 3, t:t + 1], scalar2=None, op0=ALU.mult))
                            yield
                            P.op(dve, R(bU, dsm), R(T1[1]), lambda e, t=t, bU=bU: e.tensor_scalar(
                                out=T1[1][:], in0=bU[:, 128:256], scalar1=dsm[:, 4, t:t + 1], scalar2=None, op0=ALU.mult))
                            yield
                            P.op(dve, R(S_, dsm, T1[0]), R(S_), lambda e, t=t, S_=S_: e.scalar_tensor_tensor(
                                out=S_[:], in0=S_[:], scalar=dsm[:, 0, t:t + 1], in1=T1[0][:], op0=ALU.mult, op1=ALU.add))
                            yield
                            P.op(dve, R(S_, T1[1]), R(S_), lambda e, S_=S_: e.tensor_tensor(out=S_[:], in0=S_[:], in1=T1[1][:], op=ALU.add))
                            yield
                        for hp in range(2):
                            h = 2 * p + hp
                            b_ = bO[hp]
                            evac_copy(oT[:], oT.r, b_[:, :], b_.r)
                            yield
                            P.op(act, R(b_), R(sqo), lambda e, b_=b_: e.activation(out=sqo[:], in_=b_[:, :], func=AF.Square))
                            yield
                            P.op(pe, R(onesB, sqo), R(PM), lambda e: e.matmul(PM[:, :], lhsT=onesB[:], rhs=sqo[:], start=True, stop=True))
                            yield
                            P.op(act, R(PM), R(rso), lambda e: e.activation(out=rso[:], in_=PM[:, :], func=AF.Ln, scale=1.0 / 128, bias=EPS))
                            yield
                            P.op(act, R(rso), R(rso), lambda e: e.activation(out=rso[:], in_=rso[:], func=AF.Exp, scale=-0.5))
                            yield
                            P.op(dve, R(oT, rso), R(oT), lambda e: e.tensor_tensor(out=oT[:], in0=oT[:], in1=rso[:], op=ALU.mult))
                            yield
                            P.op(dve, R(oT, vec, gsil), R(mixo), lambda e, h=h: e.scalar_tensor_tensor(
                                out=mixo[:, 4 + h, :], in0=oT[:], scalar=glag, in1=gsil[:, h, :], op0=ALU.mult, op1=ALU.mult))
                            yield
                def compAB(compA=compA, compB=compB):
                    gens = [compA(), compB()]
                    while gens:
                        for g_ in list(gens):
                            try:
                                next(g_)
                            except StopIteration:
                                gens.remove(g_)
                add_step(ld_in([("rgx", 0), ("rgg", 512), ("qk", 1024), ("v", 1536), ("g", 2048)]), compAB)

                wo = {}

                def ldC(l=l, wo=wo, blk=blk):
                    for i in range(2):
                        s = nxt("FS", FS)
                        wo[i] = s
                        load_slot(s, (l, "out", i), blk, lambda e, s=s, i=i: e.dma_start(
                            out=s[:], in_=w_out_d[l, i * 512:(i + 1) * 512, :].rearrange("(j p) d -> p j d", p=128)), False)

                def compC(l=l, wo=wo, gtm=gtm, s1f=s1f, shf=shf, moe=moe):
                    for dc in range(8):
                        bY = nxt("Y", PY)

                        def f(e, dc=dc, bY=bY):
                            ins = None
                            for m in range(8):
                                ins = e.matmul(bY[:, :], lhsT=wo[m // 4][:, m % 4, dc * 128:(dc + 1) * 128], rhs=mixo[:, m, :], start=(m == 0), stop=(m == 7))
                            return ins
                        P.op(pe, R(wo[0], wo[1], mixo), R(bY), f)
                        resid(bY, dc, gtm)
                    norm_mod(s1f, shf)
                    if moe:
                        def fr(e):
                            ins = None
                            for t in range(4):
                                for k in range(8):
                                    ins = e.matmul(PM[:, t * NE:(t + 1) * NE], lhsT=hT[:, k, t * 128:(t + 1) * 128], rhs=rwb[:, k, :], start=(k == 0), stop=(k == 7))
                            return ins
                        P.op(pe, R(hT, rwb), R(PM), fr)
                        P.op(dve, R(PM), R(LG), lambda e: e.tensor_copy(out=LG[:].rearrange("p c t -> p (c t)"), in_=PM[:, 0:4 * NE]))
                        bc = lambda ap: ap.to_broadcast([128, 4, NE])
                        P.op(dve, R(LG), R(sm4), lambda e: e.tensor_reduce(out=sm4[:, :, 0:1], in_=LG[:], axis=AX.X, op=ALU.max))
                        P.op(dve, R(LG, sm4), R(L2), lambda e: e.tensor_tensor(out=L2[:], in0=LG[:], in1=bc(sm4[:, :, 0:1]), op=ALU.is_equal))
                        P.op(dve, R(L2, LG), R(L2), lambda e: e.scalar_tensor_tensor(out=L2[:], in0=L2[:], scalar=-1e30, in1=LG[:], op0=ALU.mult, op1=ALU.add))
                        P.op(dve, R(L2), R(sm4), lambda e: e.tensor_reduce(out=sm4[:, :, 1:2], in_=L2[:], axis=AX.X, op=ALU.max))
                        P.op(dve, R(LG, sm4), R(L2), lambda e: e.tensor_tensor(out=L2[:], in0=LG[:], in1=bc(sm4[:, :, 1:2]), op=ALU.is_ge))
                        P.op(dve, R(LG, sm4), R(GT), lambda e: e.tensor_tensor(out=GT[:], in0=LG[:], in1=bc(sm4[:, :, 0:1]), op=ALU.subtract))
                        P.op(act, R(GT), R(GT), lambda e: e.activation(out=GT[:], in_=GT[:], func=AF.Exp))
                        P.op(dve, R(GT, L2), R(GT), lambda e: e.tensor_tensor(out=GT[:], in0=GT[:], in1=L2[:], op=ALU.mult))
                        P.op(dve, R(GT), R(sm4), lambda e: e.tensor_reduce(out=sm4[:, :, 2:3], in_=GT[:], axis=AX.X, op=ALU.add))
                        P.op(dve, R(sm4), R(sm4), lambda e: e.reciprocal(out=sm4[:, :, 3:4], in_=sm4[:, :, 2:3]))
                        P.op(dve, R(GT, sm4), R(GT), lambda e: e.tensor_tensor(out=GT[:], in0=GT[:], in1=bc(sm4[:, :, 3:4]), op=ALU.mult))

                        def ft(e):
                            ins = None
                            for t in range(4):
                                ins = e.transpose(PM[0:NE, t * 128:(t + 1) * 128], GT[:, t, :], identF[:])
                            return ins
                        P.op(pe, R(GT, identF), R(PM), ft)
                        P.op(dve, R(PM), R(GTT), lambda e: e.tensor_copy(out=GTT[:], in_=PM[0:NE, :]))
                add_step(ldC, compC)

                if moe:
                    experts = [(mw1_d[jl, ex], mw3_d[jl, ex], mw2_d[jl, ex], ex) for ex in range(NE)]
                else:
                    experts = [(fw1_d[jl], fw3_d[jl], fw2_d[jl], None)]
                for (w1a, w3a, w2a, ex) in experts:
                    for gi, (f0, g) in enumerate(GROUPS):
                        ws = {}

                        def ldF(w1a=w1a, w3a=w3a, w2a=w2a, f0=f0, g=g, ws=ws, l=l, ex=ex, gi=gi, blk=blk):
                            c0, c1 = f0 * 128, (f0 + g) * 128
                            for nm, src in (("w1", w1a), ("w3", w3a)):
                                s = nxt("KS", KS)
                                ws[nm] = s
                                load_slot(s, (l, ex, gi, nm), blk, lambda e, s=s, src=src: e.dma_start(
                                    out=s[:, :, 0:g * 128], in_=src[:, c0:c1].rearrange("(k p) f -> p k f", p=128)), True)
                            s = nxt("FS", FS)
                            ws["w2"] = s
                            load_slot(s, (l, ex, gi, "w2"), blk, lambda e, s=s: e.dma_start(
                                out=s[:, 0:g, :], in_=w2a[c0:c1, :].rearrange("(j p) d -> p j d", p=128)), False)

                        def compF(ex=ex, gi=gi, g=g, ws=ws, gtf=gtf):
                            if ex is not None and gi == 0:
                                bA = nxt("A", PA)
                                P.op(pe, R(selE, GTT), R(bA), lambda e, bA=bA: e.matmul(bA[:, :], lhsT=selE[0:NE, ex, :], rhs=GTT[0:NE, :], start=True, stop=True))
                                evac_copy(gbc[:], gbc.r, bA[:, :], bA.r)
                                for k in range(8):
                                    P.op(dve, R(hT, gbc), R(hgT), lambda e, k=k: e.tensor_tensor(
                                        out=hgT[:, k, :], in0=hT[:, k, :], in1=gbc[:], op=ALU.mult))
                            h3 = hT if ex is None else hgT
                            gt_ = nxt("gT", gT)
                            gch = gTf[gT.index(gt_)]
                            pre = {}
                            if ex is not None and gi == 0:
                                for fi in range(g):
                                    bA = nxt("A", PA)
                                    proj(ws["w1"], fi * 128, 128, bA)
                                    s_ = nxt("sl", sl)
                                    P.op(act, R(bA), R(s_), lambda e, bA=bA, s_=s_: e.activation(out=s_[:], in_=bA[:, :], func=AF.Silu))
                                    pre[fi] = s_
                            for fi in range(g):
                                if fi not in pre:
                                    bA = nxt("A", PA)
                                    proj(ws["w1"], fi * 128, 128, bA)
                                bB = nxt("B", PB)

                                def f3(e, fi=fi, bB=bB):
                                    ins = None
                                    for k in range(8):
                                        ins = e.matmul(bB[:, :], lhsT=ws["w3"][:, k, fi * 128:(fi + 1) * 128], rhs=h3[:, k, :], start=(k == 0), stop=(k == 7))
                                    return ins
                                P.op(pe, R(ws["w3"], h3), R(bB), f3)
                                if fi in pre:
                                    s_ = pre[fi]
                                else:
                                    s_ = nxt("sl", sl)
                                    P.op(act, R(bA), R(s_), lambda e, bA=bA, s_=s_: e.activation(out=s_[:], in_=bA[:, :], func=AF.Silu))
                                P.op(dve, R(s_, bB), [gch[fi]], lambda e, fi=fi, bB=bB, s_=s_, gt_=gt_: e.tensor_tensor(
                                    out=gt_[:, fi, :], in0=s_[:], in1=bB[:, :], op=ALU.mult))
                            for dc in range(8):
                                bY = nxt("Y3", PY + [PM])

                                def f2a(e, dc=dc, bY=bY, gt_=gt_):
                                    ins = None
                                    for fi in range(g - 1):
                                        ins = e.matmul(bY[:, :], lhsT=ws["w2"][:, fi, dc * 128:(dc + 1) * 128], rhs=gt_[:, fi, :], start=(fi == 0), stop=False)
                                    return ins

                                def f2b(e, dc=dc, bY=bY, gt_=gt_):
                                    fi = g - 1
                                    return e.matmul(bY[:, :], lhsT=ws["w2"][:, fi, dc * 128:(dc + 1) * 128], rhs=gt_[:, fi, :], start=False, stop=True)
                                if dc == 0:
                                    P.op(pe, [ws["w2"].r] + gch[0:g - 1], R(bY), f2a)
                                    P.op(pe, [ws["w2"].r, gch[g - 1]], R(bY), f2b)
                                else:
                                    def f2(e, dc=dc, bY=bY, gt_=gt_):
                                        ins = None
                                        for fi in range(g):
                                            ins = e.matmul(bY[:, :], lhsT=ws["w2"][:, fi, dc * 128:(dc + 1) * 128], rhs=gt_[:, fi, :], start=(fi == 0), stop=(fi == g - 1))
                                        return ins
                                    P.op(pe, [ws["w2"].r] + gch[0:g], R(bY), f2)
                                resid(bY, dc, gtf)
                        add_step(ldF, compF)

                def compZ(l=l, blk=blk, last_l=last_l):
                    if not last_l:
                        P.dma(sp, xsr[blk], xT.r, lambda e: e.dma_start(out=xs_d[blk], in_=xT[:]))
                    else:
                        for c in range(8):
                            P.op(act, R(xT), R(hgT), lambda e, c=c: e.activation(out=hgT[:, c, :], in_=xT[:, c, :], func=AF.Square))

                        def f(e):
                            ins = None
                            for c in range(8):
                                ins = e.matmul(PM[:, :], lhsT=onesB[:], rhs=hgT[:, c, :], start=(c == 0), stop=(c == 7))
                            return ins
                        P.op(pe, R(onesB, hgT), R(PM), f)
                        P.op(act, R(PM), R(rstd), lambda e: e.activation(out=rstd[:], in_=PM[:, :], func=AF.Ln, scale=1.0 / D, bias=EPS))
                        P.op(act, R(rstd), R(rstd), lambda e: e.activation(out=rstd[:], in_=rstd[:], func=AF.Exp, scale=-0.5))
                        for c in range(8):
                            P.op(dve, R(xT, rstd, vec), R(xT), lambda e, c=c: e.scalar_tensor_tensor(
                                out=xT[:, c, :], in0=xT[:, c, :], scalar=vec[:, 8 + c:9 + c], in1=rstd[:], op0=ALU.mult, op1=ALU.mult))
                        for t in range(4):
                            xo = nxt("xin", xin)
                            for half in range(2):
                                bY = nxt("Y", PY)

                                def tr(e, t=t, half=half, bY=bY):
                                    ins = None
                                    for cc in range(4):
                                        c = half * 4 + cc
                                        ins = e.transpose(bY[:, cc * 128:(cc + 1) * 128], xT[:, c, t * 128:(t + 1) * 128], identF[:])
                                    return ins
                                P.op(pe, R(xT, identF), R(bY), tr)
                                evac_copy(xo[:, half * 512:(half + 1) * 512], xo.r, bY[:, :], bY.r, eng=(act if half == 0 else dve))
                            r0 = blk * NT + t * 128
                            P.dma(sp, outr, xo.r, lambda e, xo=xo, r0=r0: e.dma_start(out=out_d[r0:r0 + 128, :], in_=xo[:]))
                add_step(None, compZ)

        xsr = [P.res("xs%d" % b) for b in range(NB)]
        outr = P.res("out")
        if steps[0][0]:
            steps[0][0]()
        for i, (ld, comp) in enumerate(steps):
            if i + 1 < len(steps) and steps[i + 1][0]:
                steps[i + 1][0]()
            comp()
        P.wait_all(sp, [outr])
        sp.e.wait_ge(outr.dsem["hw"], outr.dcnt["hw"])
    return nc


def _col(v):
    v = np.asarray(v, np.float32).reshape(-1, 128)
    return v.T


def _pack_vecs(inp, b):
    cols = [_col(inp["c"][b]), _col(inp["final_g"])]
    for l in range(DEPTH):
        cols += [_col(inp["norm_mix_g"][l]), _col(inp["norm_ffn_g"][l])]
        cw = inp["rg_conv_w"][l]
        cols += [_col(cw[k]) for k in range(4)]
        cols += [_col(inp["rg_conv_b"][l]), _col(inp["rg_ba"][l]), _col(inp["rg_bx"][l]), _col(inp["rg_lambda"][l]),
                 _col(inp["gla_bg"][l]), _col(inp["gla_norm_g"][l]), _col(inp["ada_b"][l])]
    v = np.ascontiguousarray(np.concatenate(cols, axis=1), dtype=np.float32)
    assert v.shape == (128, NV), v.shape
    return v


_NC_CACHE = {}


def kernel(**inputs):
    inp = {k: np.asarray(v) for k, v in inputs.items()}
    if "nc" not in _NC_CACHE:
        _NC_CACHE["nc"] = build()
    nc = _NC_CACHE["nc"]
    shared = {k: np.ascontiguousarray(inp[k], dtype=np.float32) for k in
              ("ada_w", "w_in", "rg_wa", "rg_wx", "gla_wg2", "w_out", "ffn_w1", "ffn_w3", "ffn_w2",
               "router_w", "moe_w1", "moe_w3", "moe_w2")}
    B = inp["x"].shape[0]
    in_maps = []
    for b in range(B):
        m = dict(shared)
        m["x"] = np.ascontiguousarray(inp["x"][b], dtype=np.float32)
        m["vecs"] = _pack_vecs(inp, b)
        in_maps.append(m)
    res = run_bass_kernel_spmd(nc, in_maps, core_ids=list(range(B)))
    out = np.stack([np.asarray(res.results[b]["out"], dtype=np.float32) for b in range(B)], axis=0)
    return out
```
